# Optimizing a Trainium2 kernel written in Bass

```python
import math
import jax
import jax.numpy as jnp
from jax import lax
import numpy as np

D_MODEL = 1024
BATCH = 8
SEQ = 4096
DEPTH = 2

GRID_W = 64
CTX_LEN = 256
ATTN_BLOCK = 128
ROPE_BASE = 10000.0
RMS_EPS = 1e-6

DIFF_HEADS = 4
DIFF_HEAD_DIM = 64
NA_HEADS = 8
NA_HEAD_DIM = 64
NA_WIN_ROWS_MAX = 8
NA_WIN_COLS = 16
MLA_HEADS = 8
MLA_Q_RANK = 256
MLA_KV_RANK = 128
MLA_NOPE_DIM = 64
MLA_ROPE_DIM = 32
MLA_V_DIM = 64
RET_HEADS = 4
RET_QK_DIM = 64
RET_V_DIM = 128
RET_CHUNK = 128
N_EXPERTS = 16
EC_CAPACITY_FACTOR = 2
EXPERT_FF = 1024

DIFF_WIDTH = DIFF_HEADS * 2 * DIFF_HEAD_DIM
NA_WIDTH = NA_HEADS * NA_HEAD_DIM
EVEN_IN = 3 * DIFF_WIDTH + 3 * NA_WIDTH
EVEN_OUT = DIFF_WIDTH + NA_WIDTH
MLA_QK_DIM = MLA_NOPE_DIM + MLA_ROPE_DIM
MLA_WIDTH = MLA_HEADS * MLA_V_DIM
RET_QK_WIDTH = RET_HEADS * RET_QK_DIM
RET_V_WIDTH = RET_HEADS * RET_V_DIM
ODD_IN = MLA_Q_RANK + MLA_KV_RANK + MLA_ROPE_DIM + 2 * RET_QK_WIDTH + 2 * RET_V_WIDTH
ODD_OUT = MLA_WIDTH + RET_V_WIDTH
N_EVEN = (DEPTH + 1) // 2
N_ODD = DEPTH // 2

kernel_name = 'hybrid_diffusion_diffattn_natten_mla_retention_ecmoe'


def rms_norm(x, gain=None):
    xf = x.astype(jnp.float32)
    y = xf * lax.rsqrt(jnp.mean(xf * xf, axis=-1, keepdims=True) + RMS_EPS)
    if gain is not None:
        y = y * gain.astype(jnp.float32)
    return y.astype(x.dtype)


def split_cols(p, widths):
    idx, acc = [], 0
    for w in widths[:-1]:
        acc += w
        idx.append(acc)
    return jnp.split(p, idx, axis=-1)


def axial_angles(n_tokens, rot_dim):
    t = jnp.arange(n_tokens)
    rows = (t // GRID_W).astype(jnp.float32)
    cols = (t % GRID_W).astype(jnp.float32)
    m = rot_dim // 4
    freqs = ROPE_BASE ** (-jnp.arange(m, dtype=jnp.float32) / m)
    return rows[:, None] * freqs, cols[:, None] * freqs


def rope_half(x, ang):
    a, b = jnp.split(x, 2, axis=-1)
    cos = jnp.cos(ang).astype(x.dtype)
    sin = jnp.sin(ang).astype(x.dtype)
    return jnp.concatenate([a * cos - b * sin, b * cos + a * sin], axis=-1)


def apply_axial_rope(x, ang_r, ang_c):
    shape = ang_r.shape[:1] + (1,) * (x.ndim - 3) + ang_r.shape[1:]
    xr, xc = jnp.split(x, 2, axis=-1)
    return jnp.concatenate([rope_half(xr, ang_r.reshape(shape)), rope_half(xc, ang_c.reshape(shape))], axis=-1)


def sweep_query_blocks(fn, queries):
    B, L = queries[0].shape[:2]
    nb = L // ATTN_BLOCK
    blocks = tuple(jnp.moveaxis(q.reshape((B, nb, ATTN_BLOCK) + q.shape[2:]), 1, 0) for q in queries)
    out = jnp.moveaxis(lax.map(fn, blocks), 0, 1)
    return out.reshape((B, L) + out.shape[3:])


def softmax_attention(q, k, v, q_rope=None, k_rope=None):
    d = q.shape[-1] + (0 if q_rope is None else q_rope.shape[-1])
    scale = d ** -0.5

    def block(qs):
        s = jnp.einsum('bqhd,bkhd->bhqk', qs[0], k).astype(jnp.float32)
        if q_rope is not None:
            s = s + jnp.einsum('bqhr,bkr->bhqk', qs[1], k_rope).astype(jnp.float32)
        p = jax.nn.softmax(s * scale, axis=-1).astype(v.dtype)
        return jnp.einsum('bhqk,bkhe->bqhe', p, v)

    queries = (q,) if q_rope is None else (q, q_rope)
    return sweep_query_blocks(block, queries)


def diff_attention(q, k, v, lam):
    scale = DIFF_HEAD_DIM ** -0.5

    def block(qs):
        s = jnp.einsum('bqhcd,bkhcd->bhcqk', qs[0], k).astype(jnp.float32) * scale
        p = jax.nn.softmax(s, axis=-1)
        a = p[:, :, 0] - lam * p[:, :, 1]
        return jnp.einsum('bhqk,bkhe->bqhe', a.astype(v.dtype), v)

    return sweep_query_blocks(block, (q,))


def neighbourhood_attention(q, k, v, k_ctx, v_ctx, rpb):
    B, L, H, d = q.shape
    rows = L // GRID_W
    wr = min(NA_WIN_ROWS_MAX, rows)
    wc = NA_WIN_COLS
    scale = d ** -0.5
    qg = q.reshape(B, rows, GRID_W, H, d)
    kg = k.reshape(B, rows, GRID_W, H, d)
    vg = v.reshape(B, rows, GRID_W, H, d)
    cols = jnp.arange(GRID_W)
    col_start = jnp.clip(cols - wc // 2, 0, GRID_W - wc)
    col_idx = col_start[:, None] + jnp.arange(wc)[None, :]
    col_bias = rpb[:, :, col_idx - cols[:, None] + (NA_WIN_COLS - 1)]

    def row_block(r):
        rs = jnp.clip(r - wr // 2, 0, rows - wr)
        q_row = lax.dynamic_index_in_dim(qg, r, axis=1, keepdims=False)
        k_win = lax.dynamic_slice_in_dim(kg, rs, wr, axis=1)[:, :, col_idx]
        v_win = lax.dynamic_slice_in_dim(vg, rs, wr, axis=1)[:, :, col_idx]
        row_off = rs + jnp.arange(wr) - r + (NA_WIN_ROWS_MAX - 1)
        bias = jnp.transpose(jnp.take(col_bias, row_off, axis=1), (0, 2, 1, 3))
        s_loc = jnp.einsum('bqhd,brqwhd->bhqrw', q_row, k_win).astype(jnp.float32) * scale + bias[None].astype(jnp.float32)
        s_ctx = jnp.einsum('bqhd,bkhd->bhqk', q_row, k_ctx).astype(jnp.float32) * scale
        s = jnp.concatenate([s_loc.reshape(B, H, GRID_W, wr * wc), s_ctx], axis=-1)
        p = jax.nn.softmax(s, axis=-1).astype(v.dtype)
        p_loc = p[..., :wr * wc].reshape(B, H, GRID_W, wr, wc)
        p_ctx = p[..., wr * wc:]
        return jnp.einsum('bhqrw,brqwhd->bqhd', p_loc, v_win) + jnp.einsum('bhqk,bkhd->bqhd', p_ctx, v_ctx)

    out = lax.map(row_block, jnp.arange(rows))
    return jnp.moveaxis(out, 0, 1).reshape(B, L, H, d)


def retention_chunkwise(q, k, v, log_gamma, s0, include_diag):
    B, H, L, _ = q.shape
    dv = v.shape[-1]
    C = RET_CHUNK
    n = L // C
    pos = jnp.arange(C, dtype=jnp.float32)
    rel = pos[:, None] - pos[None, :]
    mask = (rel >= 0) if include_diag else (rel > 0)
    decay_in = jnp.where(mask, jnp.exp(log_gamma[:, None, None] * jnp.where(mask, rel, 0.0)), 0.0)
    decay_q = jnp.exp(log_gamma[:, None] * (pos + 1.0))[..., None]
    decay_k = jnp.exp(log_gamma[:, None] * (C - 1.0 - pos))[..., None]
    decay_chunk = jnp.exp(log_gamma * C)[:, None, None]

    def to_chunks(t):
        return jnp.moveaxis(t.reshape(B, H, n, C, t.shape[-1]), 2, 0)

    def step(state, blk):
        qb, kb, vb = blk
        inner = jnp.einsum('bhij,bhjv->bhiv', jnp.einsum('bhid,bhjd->bhij', qb, kb) * decay_in, vb)
        cross = jnp.einsum('bhid,bhdv->bhiv', qb * decay_q, state)
        new_state = decay_chunk * state + jnp.einsum('bhjd,bhjv->bhdv', kb * decay_k, vb)
        return new_state, inner + cross

    _, out = lax.scan(step, s0, (to_chunks(q), to_chunks(k), to_chunks(v)))
    return jnp.moveaxis(out, 0, 2).reshape(B, H, L, dv)


def retention_final_state(k, v, log_gamma):
    L = k.shape[2]
    w = jnp.exp(log_gamma[:, None] * (L - 1.0 - jnp.arange(L, dtype=jnp.float32)))
    return jnp.einsum('hl,bhld,bhlv->bhdv', w, k, v)


def retention_bidir(q, k, v, q_c, k_c, v_c, decay_logit, need_ctx):
    def to_bhld(t):
        return jnp.moveaxis(t, 2, 1).astype(jnp.float32)

    def flip(t):
        return jnp.flip(t, axis=2)

    kscale = RET_QK_DIM ** -0.5
    q, v, q_c, v_c = to_bhld(q), to_bhld(v), to_bhld(q_c), to_bhld(v_c)
    k, k_c = to_bhld(k) * kscale, to_bhld(k_c) * kscale
    log_g = jax.nn.log_sigmoid(decay_logit.astype(jnp.float32))
    lg_f, lg_b = log_g[0], log_g[1]
    s_f = retention_final_state(k_c, v_c, lg_f)
    s_b = retention_final_state(flip(k_c), flip(v_c), lg_b)
    out = retention_chunkwise(q, k, v, lg_f, s_f, True) + flip(retention_chunkwise(flip(q), flip(k), flip(v), lg_b, s_b, False))
    out = jnp.moveaxis(out, 1, 2)
    if not need_ctx:
        return out, None
    zero = jnp.zeros_like(s_f)
    out_c = retention_chunkwise(q_c, k_c, v_c, lg_f, zero, True) + flip(retention_chunkwise(flip(q_c), flip(k_c), flip(v_c), lg_b, zero, False))
    return out, jnp.moveaxis(out_c, 1, 2)


def even_layer(h, hc, w_in, w_out, lam_params, subln, rpb, layer_idx, need_ctx):
    B, L, _ = h.shape
    Lc = hc.shape[1]
    widths = (DIFF_WIDTH,) * 3 + (NA_WIDTH,) * 3
    aq, ak, av, nq, nk, nv = split_cols(h @ w_in, widths)
    aqc, akc, avc, nqc, nkc, nvc = split_cols(hc @ w_in, widths)

    def dh(t):
        return t.reshape(t.shape[:2] + (DIFF_HEADS, 2, DIFF_HEAD_DIM))

    def dvh(t):
        return t.reshape(t.shape[:2] + (DIFF_HEADS, 2 * DIFF_HEAD_DIM))

    def nh(t):
        return t.reshape(t.shape[:2] + (NA_HEADS, NA_HEAD_DIM))

    ang_r, ang_c = axial_angles(L, DIFF_HEAD_DIM)
    lam_init = 0.8 - 0.6 * math.exp(-0.3 * layer_idx)
    lq1, lk1, lq2, lk2 = lam_params
    lam = jnp.exp(jnp.sum(lq1 * lk1)) - jnp.exp(jnp.sum(lq2 * lk2)) + lam_init

    k_all = jnp.concatenate([apply_axial_rope(dh(ak), ang_r, ang_c), dh(akc)], axis=1)
    v_all = jnp.concatenate([dvh(av), dvh(avc)], axis=1)
    a = diff_attention(apply_axial_rope(dh(aq), ang_r, ang_c), k_all, v_all, lam)
    a = (rms_norm(a, subln) * (1.0 - lam_init)).reshape(B, L, DIFF_WIDTH)
    nb = neighbourhood_attention(nh(nq), nh(nk), nh(nv), nh(nkc), nh(nvc), rpb).reshape(B, L, NA_WIDTH)
    y = jnp.concatenate([a, nb], axis=-1) @ w_out
    if not need_ctx:
        return y, None
    ac = diff_attention(dh(aqc), dh(akc), dvh(avc), lam)
    ac = (rms_norm(ac, subln) * (1.0 - lam_init)).reshape(B, Lc, DIFF_WIDTH)
    nc = softmax_attention(nh(nqc), nh(nkc), nh(nvc)).reshape(B, Lc, NA_WIDTH)
    yc = jnp.concatenate([ac, nc], axis=-1) @ w_out
    return y, yc


def odd_layer(h, hc, w_in, w_out, q_norm, w_uq, kv_norm, w_ukv, decay_logit, need_ctx):
    B, L, _ = h.shape
    Lc = hc.shape[1]
    widths = (MLA_Q_RANK, MLA_KV_RANK, MLA_ROPE_DIM, RET_QK_WIDTH, RET_QK_WIDTH, RET_V_WIDTH, RET_V_WIDTH)
    cq, ckv, kr, rq, rk, rv, rg = split_cols(h @ w_in, widths)
    cqc, ckvc, krc, rqc, rkc, rvc, rgc = split_cols(hc @ w_in, widths)

    def mla_up(cq_, ckv_):
        n = cq_.shape[:2]
        qh = (rms_norm(cq_, q_norm) @ w_uq).reshape(n + (MLA_HEADS, MLA_QK_DIM))
        kvh = (rms_norm(ckv_, kv_norm) @ w_ukv).reshape(n + (MLA_HEADS, MLA_NOPE_DIM + MLA_V_DIM))
        return qh[..., :MLA_NOPE_DIM], qh[..., MLA_NOPE_DIM:], kvh[..., :MLA_NOPE_DIM], kvh[..., MLA_NOPE_DIM:]

    def rh(t, d):
        return t.reshape(t.shape[:2] + (RET_HEADS, d))

    qn, qr, kn, vm = mla_up(cq, ckv)
    qnc, qrc, knc, vmc = mla_up(cqc, ckvc)
    ang_r, ang_c = axial_angles(L, MLA_ROPE_DIM)
    k_rope_all = jnp.concatenate([apply_axial_rope(kr, ang_r, ang_c), krc], axis=1)
    m = softmax_attention(qn, jnp.concatenate([kn, knc], axis=1), jnp.concatenate([vm, vmc], axis=1),
                          apply_axial_rope(qr, ang_r, ang_c), k_rope_all).reshape(B, L, MLA_WIDTH)
    r, r_c = retention_bidir(rh(rq, RET_QK_DIM), rh(rk, RET_QK_DIM), rh(rv, RET_V_DIM),
                             rh(rqc, RET_QK_DIM), rh(rkc, RET_QK_DIM), rh(rvc, RET_V_DIM), decay_logit, need_ctx)
    r = rms_norm(r.astype(h.dtype)).reshape(B, L, RET_V_WIDTH) * jax.nn.silu(rg)
    y = jnp.concatenate([m, r], axis=-1) @ w_out
    if not need_ctx:
        return y, None
    mc = softmax_attention(qnc, knc, vmc, qrc, krc).reshape(B, Lc, MLA_WIDTH)
    rc = rms_norm(r_c.astype(hc.dtype)).reshape(B, Lc, RET_V_WIDTH) * jax.nn.silu(rgc)
    yc = jnp.concatenate([mc, rc], axis=-1) @ w_out
    return y, yc


def expert_choice_ffn(h, w_router, w1, w3, w2):
    B, T, _ = h.shape
    cap = (EC_CAPACITY_FACTOR * T) // N_EXPERTS
    aff = jax.nn.softmax((h @ w_router).astype(jnp.float32), axis=-1)
    gate, idx = lax.top_k(jnp.swapaxes(aff, 1, 2), cap)
    bidx = jnp.arange(B)[:, None, None]
    xe = h[bidx, idx]
    hid = jax.nn.silu(jnp.einsum('becd,edf->becf', xe, w1)) * jnp.einsum('becd,edf->becf', xe, w3)
    ye = jnp.einsum('becf,efd->becd', hid, w2) * gate[..., None].astype(h.dtype)
    return jnp.zeros_like(h).at[bidx, idx].add(ye)


def setup_inputs(seed: int = 0) -> dict:
    key = jax.random.key(seed)
    ks = jax.random.split(key, 25)
    f32 = jnp.float32

    def nrm(k, shape, scale):
        return jax.random.normal(k, shape, f32) * scale

    def gain(k, shape):
        return 1.0 + 0.02 * jax.random.normal(k, shape, f32)

    decay_init = jnp.log(2.0 ** (5.0 + jnp.arange(RET_HEADS, dtype=f32)) - 1.0)
    return {
        'x': nrm(ks[0], (BATCH, SEQ, D_MODEL), 1.0),
        'c': nrm(ks[1], (BATCH, D_MODEL), 1.0),
        'ctx': nrm(ks[2], (BATCH, CTX_LEN, D_MODEL), 1.0),
        'c_ctx': nrm(ks[3], (D_MODEL,), 1.0),
        'ada_w': nrm(ks[4], (DEPTH, D_MODEL, 6 * D_MODEL), 0.5 * D_MODEL ** -0.5),
        'ada_b': nrm(ks[5], (DEPTH, 6 * D_MODEL), 0.01),
        'norm_mix': gain(ks[6], (DEPTH, D_MODEL)),
        'norm_ffn': gain(ks[7], (DEPTH, D_MODEL)),
        'final_norm': gain(ks[8], (D_MODEL,)),
        'even_w_in': nrm(ks[9], (N_EVEN, D_MODEL, EVEN_IN), D_MODEL ** -0.5),
        'even_w_out': nrm(ks[10], (N_EVEN, EVEN_OUT, D_MODEL), EVEN_OUT ** -0.5),
        'diff_lambda': nrm(ks[11], (N_EVEN, 4, DIFF_HEAD_DIM), 0.1),
        'diff_subln': gain(ks[12], (N_EVEN, 2 * DIFF_HEAD_DIM)),
        'na_rpb': nrm(ks[13], (N_EVEN, NA_HEADS, 2 * NA_WIN_ROWS_MAX - 1, 2 * NA_WIN_COLS - 1), 0.02),
        'odd_w_in': nrm(ks[14], (N_ODD, D_MODEL, ODD_IN), D_MODEL ** -0.5),
        'odd_w_out': nrm(ks[15], (N_ODD, ODD_OUT, D_MODEL), ODD_OUT ** -0.5),
        'mla_q_norm': gain(ks[16], (N_ODD, MLA_Q_RANK)),
        'mla_w_uq': nrm(ks[17], (N_ODD, MLA_Q_RANK, MLA_HEADS * MLA_QK_DIM), MLA_Q_RANK ** -0.5),
        'mla_kv_norm': gain(ks[18], (N_ODD, MLA_KV_RANK)),
        'mla_w_ukv': nrm(ks[19], (N_ODD, MLA_KV_RANK, MLA_HEADS * (MLA_NOPE_DIM + MLA_V_DIM)), MLA_KV_RANK ** -0.5),
        'ret_decay_logit': jnp.broadcast_to(decay_init, (N_ODD, 2, RET_HEADS)) + nrm(ks[20], (N_ODD, 2, RET_HEADS), 0.05),
        'moe_router': nrm(ks[21], (DEPTH, D_MODEL, N_EXPERTS), D_MODEL ** -0.5),
        'moe_w1': nrm(ks[22], (DEPTH, N_EXPERTS, D_MODEL, EXPERT_FF), D_MODEL ** -0.5),
        'moe_w3': nrm(ks[23], (DEPTH, N_EXPERTS, D_MODEL, EXPERT_FF), D_MODEL ** -0.5),
        'moe_w2': nrm(ks[24], (DEPTH, N_EXPERTS, EXPERT_FF, D_MODEL), EXPERT_FF ** -0.5),
    }


def reference(x, c, ctx, c_ctx, ada_w, ada_b, norm_mix, norm_ffn, final_norm, even_w_in, even_w_out,
              diff_lambda, diff_subln, na_rpb, odd_w_in, odd_w_out, mla_q_norm, mla_w_uq, mla_kv_norm,
              mla_w_ukv, ret_decay_logit, moe_router, moe_w1, moe_w3, moe_w2):
    xc = ctx
    silu_c = jax.nn.silu(c)
    silu_cc = jax.nn.silu(c_ctx)
    for l in range(DEPTH):
        need_ctx = l < DEPTH - 1
        mods = jnp.split(silu_c @ ada_w[l] + ada_b[l], 6, axis=-1)
        sh1, sc1, g1, sh2, sc2, g2 = [m[:, None, :] for m in mods]
        sh1c, sc1c, g1c, sh2c, sc2c, g2c = jnp.split(silu_cc @ ada_w[l] + ada_b[l], 6, axis=-1)
        h = rms_norm(x, norm_mix[l]) * (1.0 + sc1) + sh1
        hc = rms_norm(xc, norm_mix[l]) * (1.0 + sc1c) + sh1c
        i = l // 2
        if l % 2 == 0:
            y, yc = even_layer(h, hc, even_w_in[i], even_w_out[i], diff_lambda[i], diff_subln[i], na_rpb[i], l, need_ctx)
        else:
            y, yc = odd_layer(h, hc, odd_w_in[i], odd_w_out[i], mla_q_norm[i], mla_w_uq[i], mla_kv_norm[i],
                              mla_w_ukv[i], ret_decay_logit[i], need_ctx)
        x = x + g1 * y
        h2 = rms_norm(x, norm_ffn[l]) * (1.0 + sc2) + sh2
        x = x + g2 * expert_choice_ffn(h2, moe_router[l], moe_w1[l], moe_w3[l], moe_w2[l])
        if need_ctx:
            xc = xc + g1c * yc
            hc2 = rms_norm(xc, norm_ffn[l]) * (1.0 + sc2c) + sh2c
            xc = xc + g2c * expert_choice_ffn(hc2, moe_router[l], moe_w1[l], moe_w3[l], moe_w2[l])
    return rms_norm(x, final_norm)
```

```python
import os
import numpy as np
from contextlib import ExitStack
import concourse.bass as bass
import concourse.mybir as mybir
from concourse.bass_utils import run_bass_kernel_spmd

F32 = mybir.dt.float32
BF16 = mybir.dt.bfloat16
I32 = mybir.dt.int32
U32 = mybir.dt.uint32
AF = mybir.ActivationFunctionType
ALU = mybir.AluOpType

D = 1024
L = 4096
LC = 256
LT = L + LC
NT = LT // 128
NEG = -30000.0


class View:
    __slots__ = ("ap", "tt", "reg")

    def __init__(self, ap, tt, reg):
        self.ap = ap
        self.tt = tt
        self.reg = reg

    def with_ap(self, ap):
        return View(ap, self.tt, self.reg)


class TT:
    def __init__(self, name, base_ap, shape, space):
        self.name = name
        self.base = base_ap
        self.shape = tuple(shape)
        self.space = space
        self.recs = []

    def __getitem__(self, idx):
        if not isinstance(idx, tuple):
            idx = (idx,)
        idx = tuple(idx) + (slice(None),) * (len(self.shape) - len(idx))
        reg = []
        for i, s in enumerate(idx):
            n = self.shape[i]
            if isinstance(s, int):
                assert 0 <= s < n, (self.name, idx, self.shape)
                reg.append((s, s + 1))
            else:
                lo = 0 if s.start is None else s.start
                hi = n if s.stop is None else s.stop
                assert 0 <= lo < hi <= n, (self.name, idx, self.shape)
                reg.append((lo, hi))
        return View(self.base[idx], self, tuple(reg))

    def all(self):
        return self[tuple(slice(None) for _ in self.shape)]


def _overlap(a, b):
    for (l1, h1), (l2, h2) in zip(a, b):
        if h1 <= l2 or h2 <= l1:
            return False
    return True


def _contains(a, b):
    for (l1, h1), (l2, h2) in zip(a, b):
        if l2 < l1 or h2 > h1:
            return False
    return True


class Prog:
    def __init__(self, nc, n_dma_sems=10):
        self.nc = nc
        self.eobj = {"pe": nc.tensor, "act": nc.scalar, "dve": nc.vector, "pool": nc.gpsimd, "sp": nc.sync}
        self.es = ExitStack()
        self.cnt = {e: 0 for e in self.eobj}
        self.ninst = {e: 0 for e in self.eobj}
        self.seen = {e: {} for e in self.eobj}
        self.esem = {}
        for e in ("pe", "act", "dve", "pool"):
            self.esem[e] = self.es.enter_context(nc.semaphore("es_" + e))
        self.dsem = {}
        self.dtot = {}
        self.dnext = {}
        self.semobj = {}
        for e in ("sp", "act", "pool"):
            self.dsem[e] = [self.es.enter_context(nc.semaphore("ds_%s%d" % (e, i))) for i in range(n_dma_sems)]
            self.dnext[e] = 0
            for s in self.dsem[e]:
                self.dtot[id(s)] = 0
                self.semobj[id(s)] = s
        for e, s in self.esem.items():
            self.semobj[id(s)] = s
        self.scopes = []

    def push(self):
        st = ExitStack()
        self.scopes.append(st)
        return st

    def pop(self):
        self.barrier()
        self.scopes.pop().close()

    def _stack(self):
        return self.scopes[-1] if self.scopes else self.es

    def sbuf(self, name, shape, dt):
        self.uid = getattr(self, "uid", 0) + 1
        name = "%s_%d" % (name, self.uid)
        h = self._stack().enter_context(self.nc.sbuf_tensor(name, list(shape), dt))
        return TT(name, h[tuple(slice(None) for _ in shape)], shape, "sbuf")

    def psum(self, name, shape, dt):
        self.uid = getattr(self, "uid", 0) + 1
        name = "%s_%d" % (name, self.uid)
        h = self._stack().enter_context(self.nc.psum_tensor(name, list(shape), dt))
        return TT(name, h[tuple(slice(None) for _ in shape)], shape, "psum")

    def dram(self, name, shape, dt, kind=None):
        if kind is None:
            h = self.nc.dram_tensor(name, list(shape), dt)
        else:
            h = self.nc.dram_tensor(name, list(shape), dt, kind=kind)
        return TT(name, h.ap(), shape, "dram")

    def _deps(self, reads, writes):
        deps = {}
        for v in reads:
            for r in v.tt.recs:
                if r[1] and _overlap(r[0], v.reg):
                    if deps.get(r[2], 0) < r[3]:
                        deps[r[2]] = r[3]
        for v in writes:
            for r in v.tt.recs:
                if _overlap(r[0], v.reg):
                    if deps.get(r[2], 0) < r[3]:
                        deps[r[2]] = r[3]
        return deps

    def _record(self, reads, writes, sk, val):
        for v in writes:
            recs = v.tt.recs
            recs[:] = [r for r in recs if not _contains(v.reg, r[0])]
            recs.append([v.reg, True, sk, val])
        for v in reads:
            recs = v.tt.recs
            for r in recs:
                if (not r[1]) and r[2] == sk and r[0] == v.reg:
                    if r[3] < val:
                        r[3] = val
                    break
            else:
                recs.append([v.reg, False, sk, val])

    def _psum_deps(self, eng, views, deps, val=None):
        mysk = id(self.esem[eng]) if eng in self.esem else None
        for v in views:
            if v.tt.space != "psum":
                continue
            last = v.tt.__dict__.setdefault("last", {})
            if val is None:
                for sk, lv in last.items():
                    if sk != mysk and deps.get(sk, 0) < lv:
                        deps[sk] = lv
            else:
                last[mysk] = val

    def _emit(self, eng, deps, fn, sk, inc):
        e = self.eobj[eng]
        seen = self.seen[eng]
        for wsk, wv in deps.items():
            if seen.get(wsk, 0) < wv:
                seen[wsk] = wv
                e.wait_ge(self.semobj[wsk], wv)
        fn(e).then_inc(self.semobj[sk], inc)
        self.ninst[eng] += 1

    def op(self, eng, fn, reads=(), writes=()):
        deps = self._deps(reads, writes)
        self._psum_deps(eng, list(reads) + list(writes), deps)
        self.cnt[eng] += 1
        sk = id(self.esem[eng])
        self._emit(eng, deps, fn, sk, 1)
        self._record(reads, writes, sk, self.cnt[eng])
        self._psum_deps(eng, list(reads) + list(writes), None, val=self.cnt[eng])

    def dma(self, eng, out, in_, fn=None, extra_reads=()):
        reads = [in_] + list(extra_reads)
        deps = self._deps(reads, [out])
        pool = self.dsem[eng]
        s = pool[self.dnext[eng] % len(pool)]
        self.dnext[eng] += 1
        sk = id(s)
        if self.dtot[sk] > 0 and deps.get(sk, 0) < self.dtot[sk]:
            deps[sk] = self.dtot[sk]
        self.dtot[sk] += 16
        if fn is None:
            oap, iap = out.ap, in_.ap
            fn = lambda e: e.dma_start(out=oap, in_=iap)
        self._emit(eng, deps, fn, sk, 16)
        self._record(reads, [out], sk, self.dtot[sk])

    def barrier(self):
        allev = {}
        for e in ("pe", "act", "dve", "pool"):
            if self.cnt[e] > 0:
                allev[id(self.esem[e])] = self.cnt[e]
        for sk, tot in self.dtot.items():
            if tot > 0:
                allev[sk] = tot
        for eng, e in self.eobj.items():
            seen = self.seen[eng]
            for wsk, wv in allev.items():
                if seen.get(wsk, 0) < wv:
                    seen[wsk] = wv
                    e.wait_ge(self.semobj[wsk], wv)

    def finish(self):
        self.barrier()
        while self.scopes:
            self.scopes.pop().close()
        self.es.close()

    def act(self, out, in_, func, scale=1.0, bias=None, accum=None, eng="act"):
        rd = [in_]
        wr = [out]
        kw = {}
        if isinstance(scale, View):
            rd.append(scale)
            kw["scale"] = scale.ap
        else:
            kw["scale"] = float(scale)
        if bias is not None:
            if isinstance(bias, View):
                rd.append(bias)
                kw["bias"] = bias.ap
            else:
                kw["bias"] = float(bias)
        if accum is not None:
            wr.append(accum)
            kw["accum_out"] = accum.ap
        oap, iap = out.ap, in_.ap
        self.op("act", lambda e: e.activation(out=oap, in_=iap, func=func, **kw), rd, wr)

    def ts(self, eng, out, in0, s1, s2=None, op0=ALU.mult, op1=None, accum=None):
        rd = [in0]
        wr = [out]
        a1 = s1
        if isinstance(s1, View):
            rd.append(s1)
            a1 = s1.ap
        a2 = s2
        if isinstance(s2, View):
            rd.append(s2)
            a2 = s2.ap
        kw = {}
        if op1 is not None:
            kw["op1"] = op1
        if accum is not None:
            kw["accum_out"] = accum.ap
            wr.append(accum)
        oap, iap = out.ap, in0.ap
        self.op(eng, lambda e: e.tensor_scalar(out=oap, in0=iap, scalar1=a1, scalar2=a2, op0=op0, **kw), rd, wr)

    def tt(self, eng, out, in0, in1, op):
        oap, a, b = out.ap, in0.ap, in1.ap
        self.op(eng, lambda e: e.tensor_tensor(out=oap, in0=a, in1=b, op=op), [in0, in1], [out])

    def stt(self, out, in0, scalar, in1, op0, op1, accum=None):
        rd = [in0, in1]
        wr = [out]
        sc = scalar
        if isinstance(scalar, View):
            rd.append(scalar)
            sc = scalar.ap
        kw = {}
        if accum is not None:
            kw["accum_out"] = accum.ap
            wr.append(accum)
        oap, a, b = out.ap, in0.ap, in1.ap
        self.op("dve", lambda e: e.scalar_tensor_tensor(out=oap, in0=a, scalar=sc, in1=b, op0=op0, op1=op1, **kw), rd, wr)

    def copy(self, eng, out, in_):
        oap, iap = out.ap, in_.ap
        if eng == "act":
            self.op("act", lambda e: e.activation(out=oap, in_=iap, func=AF.Copy), [in_], [out])
        else:
            self.op(eng, lambda e: e.tensor_copy(out=oap, in_=iap), [in_], [out])

    def memset(self, eng, out, val):
        oap = out.ap
        self.op(eng, lambda e: e.memset(oap, val), [], [out])

    def recip(self, out, in_):
        oap, iap = out.ap, in_.ap
        self.op("dve", lambda e: e.reciprocal(out=oap, in_=iap), [in_], [out])

    def mm(self, out, pairs, start=True, skip=False):
        rd = []
        for a, b in pairs:
            rd.append(a)
            rd.append(b)
        oap = out.ap
        aps = [(a.ap, b.ap) for a, b in pairs]
        n = len(aps)

        def fn(e):
            ins = None
            for i, (la, ra) in enumerate(aps):
                st = start and i == 0
                if skip:
                    ins = e.matmul(oap, lhsT=la, rhs=ra, start=st, stop=(i == n - 1), skip_group_check=True)
                else:
                    ins = e.matmul(oap, lhsT=la, rhs=ra, start=st, stop=(i == n - 1))
            return ins
        self.op("pe", fn, rd, [out])

    def transposes(self, items, ident, extra_writes=()):
        rd = [ident] + [b for _, b in items]
        wr = [a for a, _ in items]
        aps = [(a.ap, b.ap) for a, b in items]
        iap = ident.ap

        def fn(e):
            ins = None
            for oa, ia in aps:
                ins = e.transpose(out=oa, in_=ia, identity=iap)
            return ins
        self.op("pe", fn, rd, wr)


def bview(tt_dram, row_idx, lo, hi, reg_tt=None):
    v = tt_dram[row_idx + (slice(lo, hi),)] if isinstance(row_idx, tuple) else tt_dram[row_idx, lo:hi]
    return v.with_ap(v.ap.partition_broadcast(128))


def _rope_tables():
    f32 = np.float32
    tok = np.arange(L)
    rows = (tok // 64).astype(f32)
    cols = (tok % 64).astype(f32)
    fr = (np.float32(10000.0) ** (-np.arange(16, dtype=f32) / np.float32(16))).astype(f32)
    cs = np.zeros((2, 128, L), f32)
    for p in range(128):
        d = p % 64
        pos = rows if d < 32 else cols
        ang = (pos * fr[d % 16]).astype(f32)
        cs[0, p] = np.cos(ang)
        sg = -1.0 if (d % 32) < 16 else 1.0
        cs[1, p] = sg * np.sin(ang)
    fr8 = (np.float32(10000.0) ** (-np.arange(8, dtype=f32) / np.float32(8))).astype(f32)
    cm = np.zeros((2, 128, L), f32)
    for d in range(32):
        pos = rows if d < 16 else cols
        ang = (pos * fr8[d % 8]).astype(f32)
        cm[0, 64 + d] = np.cos(ang)
        sg = -1.0 if (d % 16) < 8 else 1.0
        cm[1, 64 + d] = sg * np.sin(ang)
    return cs, cm


def _na_tiles():
    pats = {}
    per_j = []
    for j in range(32):
        if 2 <= j <= 29:
            kts = list(range(j - 2, j + 3))
            ids = [kt - j + 2 for kt in kts]
        else:
            kts = [0, 1, 2, 3] if j < 2 else [28, 29, 30, 31]
            base = {0: 5, 1: 9, 30: 13, 31: 17}[j]
            ids = [base + i for i in range(4)]
        for kt, pid in zip(kts, ids):
            if pid not in pats:
                pats[pid] = (j, kt)
        per_j.append(list(zip(kts, ids)))
    return per_j, pats


def _na_bias(rpb):
    per_j, pats = _na_tiles()
    out = np.full((21, 128, 8, 128), NEG, np.float32)
    kl = np.arange(128)
    ql = np.arange(128)
    for pid, (j, kt) in pats.items():
        kr = 2 * kt + kl // 64
        kc = kl % 64
        qr = 2 * j + ql // 64
        qc = ql % 64
        rs = np.clip(qr - 4, 0, 56)
        cst = np.clip(qc - 8, 0, 48)
        inwin = ((kr[:, None] >= rs[None, :]) & (kr[:, None] < rs[None, :] + 8) &
                 (kc[:, None] >= cst[None, :]) & (kc[:, None] < cst[None, :] + 16))
        ro = np.clip(kr[:, None] - qr[None, :] + 7, 0, 14)
        co = np.clip(kc[:, None] - qc[None, :] + 15, 0, 30)
        for h in range(8):
            g = rpb[h][ro, co]
            out[pid, :, (h % 2) * 4 + h // 2, :] = np.where(inwin, g, np.float32(NEG))
    return out


def host_prep(inp):
    f32 = np.float32
    sh = {}
    cs, cm = _rope_tables()
    sh["rope_d"] = cs
    sh["rope_m"] = cm
    sh["ident"] = np.eye(128, dtype=f32)
    sh["na_bias"] = _na_bias(np.asarray(inp["na_rpb"][0]))
    pos = np.arange(128, dtype=f32)
    ramps = np.zeros((128, 4, 128), f32)
    ramps[:64, 0, :] = pos[None, :] + 1.0
    ramps[64:, 0, :] = 128.0 - pos[None, :]
    ramps[:, 1, :] = np.maximum(pos[None, :] - pos[:, None], 0.0)
    ramps[:, 2, :] = np.maximum(pos[:, None] - pos[None, :], 0.0)
    ramps[:, 3, 0] = 127.0 - pos
    ramps[:, 3, 1] = pos
    ramps[:, 3, 2] = 128.0
    sh["ramps"] = ramps
    sh["dummy_idx"] = np.ascontiguousarray(np.broadcast_to((LT + np.arange(128, dtype=np.int32))[:, None], (128, 16)))
    w = np.asarray(inp["even_w_in"][0])
    d = np.arange(64)
    sw = np.where((d % 32) < 16, d + 16, d - 16)
    idx = np.arange(512).reshape(4, 2, 64)[:, :, sw].reshape(-1)
    sh["w_in0"] = np.ascontiguousarray(np.concatenate([w, w[:, 0:512][:, idx], w[:, 512:1024][:, idx]], axis=1))
    sh["w_out0"] = np.asarray(inp["even_w_out"][0])
    w1 = np.asarray(inp["odd_w_in"][0])
    cq, ckv, kr = w1[:, 0:256], w1[:, 256:384], w1[:, 384:416]
    rq, rk, rv, rg = w1[:, 416:672], w1[:, 672:928], w1[:, 928:1440], w1[:, 1440:1952]
    d32 = np.arange(32)
    sw32 = np.where((d32 % 16) < 8, d32 + 8, d32 - 8)
    z64 = np.zeros((D, 64), f32)
    z32 = np.zeros((D, 32), f32)
    kr_pad = np.concatenate([z64, kr, z32], axis=1)
    kr_sw_pad = np.concatenate([z64, kr[:, sw32], z32], axis=1)
    rq_dup = np.concatenate([np.concatenate([rq[:, h * 64:(h + 1) * 64]] * 2, axis=1) for h in range(4)], axis=1)
    rk_dup = np.concatenate([np.concatenate([rk[:, h * 64:(h + 1) * 64]] * 2, axis=1) for h in range(4)], axis=1)
    sh["w_in1"] = np.ascontiguousarray(np.concatenate([cq, ckv, rk, rv, rg, kr_pad, kr_sw_pad, rq_dup, rk_dup], axis=1))
    sh["w_out1"] = np.asarray(inp["odd_w_out"][0])
    wuq = np.asarray(inp["mla_w_uq"][0])
    ci = np.arange(768).reshape(8, 96).copy()
    ci[:, 64:] = ci[:, 64:][:, sw32]
    sh["w_uq"] = np.ascontiguousarray(np.concatenate([wuq, wuq[:, ci.reshape(-1)]], axis=1))
    wukv = np.asarray(inp["mla_w_ukv"][0]).reshape(128, 8, 128)
    sh["w_ukv"] = np.ascontiguousarray(np.concatenate([wukv[:, :, :64].reshape(128, 512), wukv[:, :, 64:].reshape(128, 512)], axis=1))
    for k in ("ada_w", "ada_b", "norm_mix", "norm_ffn", "final_norm", "diff_lambda", "diff_subln", "mla_q_norm",
              "mla_kv_norm", "ret_decay_logit", "moe_router", "moe_w1", "moe_w3", "moe_w2"):
        sh[k] = np.ascontiguousarray(np.asarray(inp[k], dtype=f32))
    sh["final_norm"] = sh["final_norm"].reshape(1, D)
    sh["diff_lambda"] = sh["diff_lambda"].reshape(1, 256)
    sh["ret_decay_logit"] = sh["ret_decay_logit"].reshape(1, 8)
    per = []
    c_ctx = np.asarray(inp["c_ctx"], f32)
    for b in range(inp["x"].shape[0]):
        cc = np.stack([np.asarray(inp["c"][b], f32).reshape(8, 128).T, c_ctx.reshape(8, 128).T], axis=-1)
        xa = np.concatenate([np.asarray(inp["x"][b], f32), np.asarray(inp["ctx"][b], f32)], axis=0)
        per.append({"xin": np.ascontiguousarray(xa), "cc": np.ascontiguousarray(cc)})
    return sh, per


def build(stop_after=None, debug=False, only=None):
    nc = bass.Bass("TRN2", target_bir_lowering=False)
    P = Prog(nc)
    EXT = "ExternalOutput" if debug else None

    def din(name, shape, dt=F32):
        return P.dram(name, shape, dt, kind="ExternalInput")

    xin = din("xin", [LT, D])
    cc = din("cc", [128, 8, 2])
    rope_d = din("rope_d", [2, 128, L])
    rope_m = din("rope_m", [2, 128, L])
    ident_d = din("ident", [128, 128])
    na_bias = din("na_bias", [21, 128, 8, 128])
    ramps_d = din("ramps", [128, 4, 128])
    dummy_idx = din("dummy_idx", [128, 16], I32)
    w_in0 = din("w_in0", [D, 4096])
    w_out0 = din("w_out0", [D, D])
    w_in1 = din("w_in1", [D, 2944])
    w_out1 = din("w_out1", [D, D])
    w_uq = din("w_uq", [256, 1536])
    w_ukv = din("w_ukv", [128, 1024])
    ada_w = din("ada_w", [2, D, 6 * D])
    ada_b = din("ada_b", [2, 6 * D])
    norm_mix = din("norm_mix", [2, D])
    norm_ffn = din("norm_ffn", [2, D])
    final_norm = din("final_norm", [1, D])
    diff_lambda = din("diff_lambda", [1, 256])
    diff_subln = din("diff_subln", [1, 128])
    mla_q_norm = din("mla_q_norm", [1, 256])
    mla_kv_norm = din("mla_kv_norm", [1, 128])
    ret_decay = din("ret_decay_logit", [1, 8])
    moe_router = din("moe_router", [2, D, 16])
    moe_w = [din("moe_w1", [2, 16, D, D]), din("moe_w3", [2, 16, D, D]), din("moe_w2", [2, 16, D, D])]
    out_d = P.dram("out", [L, D], F32, kind="ExternalOutput")

    mods_d = P.dram("mods_d", [2, 2, 6 * D], F32, kind=EXT)
    x1_d = P.dram("x1_d", [LT + 128, D], F32, kind=EXT)
    h2_d = P.dram("h2_d", [LT + 128, D], BF16, kind=EXT)
    ao_d = P.dram("ao_d", [LT, D], BF16, kind=EXT)
    qTd = P.dram("qTd", [4, 128, LT], BF16, kind=EXT)
    kTd = P.dram("kTd", [4, 128, LT], BF16, kind=EXT)
    qTn = P.dram("qTn", [4, 128, LT], BF16, kind=EXT)
    kTn = P.dram("kTn", [4, 128, LT], BF16, kind=EXT)
    vd = P.dram("vd", [LT, 512], BF16, kind=EXT)
    vn = P.dram("vn", [LT, 512], BF16, kind=EXT)
    cqn_d = P.dram("cqn_d", [3, 128, LT], BF16, kind=EXT)
    krT_d = P.dram("krT_d", [128, LT], BF16, kind=EXT)
    rqT_d = P.dram("rqT_d", [4, 128, LT], BF16, kind=EXT)
    rkT_d = P.dram("rkT_d", [4, 128, LT], BF16, kind=EXT)
    rk_d = P.dram("rk_d", [LT, 256], BF16, kind=EXT)
    rv_d = P.dram("rv_d", [LT, 512], BF16, kind=EXT)
    sg_d = P.dram("sg_d", [LT, 512], BF16, kind=EXT)
    dbg_aff = P.dram("dbg_aff", [16, LT], F32, kind=EXT) if debug else None
    dbg_idx = P.dram("dbg_idx", [128, 5, 16], I32, kind=EXT) if debug else None

    identf = P.sbuf("identf", [128, 128], F32)
    identb = P.sbuf("identb", [128, 128], BF16)
    gateT = P.sbuf("gateT", [128, 5, 16], F32)
    idxT = P.sbuf("idxT", [128, 5, 16], I32)
    P.dma("sp", identf.all(), ident_d.all())
    P.copy("dve", identb.all(), identf.all())
    eps_t = P.sbuf("eps_t", [128, 1], F32)
    P.memset("dve", eps_t.all(), 1e-6)

    MOD = {"sh1": 0, "sc1": 1, "g1": 2, "sh2": 3, "sc2": 4, "g2": 5}

    def modrow(l, who, name):
        k = MOD[name]
        v = mods_d[l, who, k * D:(k + 1) * D]
        return v.with_ap(v.ap.partition_broadcast(128))

    def brow(tt_d, idx, n):
        v = tt_d[idx, 0:n]
        return v.with_ap(v.ap.partition_broadcast(128))

    def stop(name):
        return stop_after == name

    P.push()
    zt = P.sbuf("zt", [128, D], F32)
    ztb = P.sbuf("ztb", [128, D], BF16)
    P.memset("dve", zt.all(), 0.0)
    P.memset("pool", ztb.all(), 0.0)
    P.dma("sp", x1_d[LT:LT + 128, :], zt.all())
    P.dma("sp", h2_d[LT:LT + 128, :], ztb.all())
    P.pop()
    P.push()
    ccs = P.sbuf("ccs", [128, 8, 2], F32)
    scs = P.sbuf("scs", [128, 8, 2], F32)
    P.dma("sp", ccs.all(), cc.all())
    P.act(scs.all(), ccs.all(), AF.Silu)
    adw = [P.sbuf("adw%d" % i, [128, 8, 512], F32) for i in range(2)]
    modsb = P.sbuf("modsb", [2, 6 * D], F32)
    adb = P.sbuf("adb", [2, 6 * D], F32)
    psm = [P.psum("psm%d" % i, [128, 512], F32) for i in range(2)]
    for l in range(2):
        P.dma("sp", adb[0:1, :], ada_b[l:l + 1, :])
        P.dma("sp", adb[1:2, :], ada_b[l:l + 1, :])
        for nb in range(12):
            w = adw[nb % 2]
            src = ada_w[l, :, nb * 512:(nb + 1) * 512]
            P.dma("sp" if nb % 2 == 0 else "act", w.all(), src.with_ap(src.ap.rearrange("(c p) n -> p c n", p=128)))
            P.mm(psm[nb % 2][0:2, :], [(scs[:, c, :], w[:, c, :]) for c in range(8)])
            P.tt("dve", modsb[:, nb * 512:(nb + 1) * 512], psm[nb % 2][0:2, :], adb[:, nb * 512:(nb + 1) * 512], ALU.add)
        P.dma("sp", mods_d[l], modsb.all())
    P.pop()
    if stop("mods"):
        P.finish()
        return nc

    def make_sb(l, who, which, s_b, sh_b, t1, t2):
        nrm = norm_mix if which == 1 else norm_ffn
        P.dma("sp", t1.all(), modrow(l, who, "sc%d" % which))
        P.dma("sp", t2.all(), brow(nrm, l, D))
        P.stt(s_b.all(), t1.all(), 1.0, t2.all(), ALU.add, ALU.mult)
        P.dma("sp", sh_b.all(), modrow(l, who, "sh%d" % which))

    def rstd_of(ss, rstd, n):
        P.act(rstd, ss, AF.Sqrt, scale=1.0 / n, bias=eps_t[:, 0:1])
        P.recip(rstd, rstd)


    def cast_rot(i):
        return ("dve", "pool", "act")[i % 3]

    def phase_A0():
        P.push()
        wb = P.sbuf("w0b", [128, 8, 4096], BF16)
        wst = [P.sbuf("w0st%d" % i, [128, 8, 256], F32) for i in range(2)]
        for nb in range(16):
            src = w_in0[:, nb * 256:(nb + 1) * 256]
            P.dma("sp" if nb % 2 == 0 else "act", wst[nb % 2].all(), src.with_ap(src.ap.rearrange("(c p) n -> p c n", p=128)))
            P.copy(cast_rot(nb), wb[:, :, nb * 256:(nb + 1) * 256], wst[nb % 2].all())
        sb = [P.sbuf("sb%d" % i, [128, D], F32) for i in range(2)]
        shb = [P.sbuf("shb%d" % i, [128, D], F32) for i in range(2)]
        xt = [P.sbuf("xt%d" % i, [128, D], F32) for i in range(2)]
        tmp32 = P.sbuf("tmp32", [128, D], F32)
        junk = P.sbuf("junk", [128, D], BF16)
        make_sb(0, 0, 1, sb[0], shb[0], xt[0], xt[1])
        make_sb(0, 1, 1, sb[1], shb[1], xt[0], xt[1])
        hb = [P.sbuf("hb%d" % i, [128, D], BF16) for i in range(2)]
        hTs = [P.sbuf("hT%d" % i, [128, 8, 512], BF16) for i in range(2)]
        cst = P.sbuf("cst", [128, 2, 512], F32)
        fst = P.sbuf("fst", [128, 16, 512], BF16)
        vst = P.sbuf("vst", [128, 4, 1024], BF16)
        t1 = P.sbuf("rt1", [128, 512], F32)
        t2 = P.sbuf("rt2", [128, 512], F32)
        ss = P.sbuf("ss", [128, 1], F32)
        rstd = P.sbuf("rstd", [128, 1], F32)
        pT = [P.psum("pT%d" % i, [128, 1024], BF16) for i in range(2)]
        pa = [P.psum("pa%d" % i, [128, 512], F32) for i in range(2)]
        pb = [P.psum("pb%d" % i, [128, 512], F32) for i in range(2)]
        pv = [P.psum("pv%d" % i, [128, 512], F32) for i in range(2)]
        def S1(blk):
            ntile = 4 if blk < 8 else 2
            who = 0 if blk < 8 else 1
            hT = hTs[blk % 2]
            for ti in range(ntile):
                t = blk * 4 + ti
                x = xt[ti % 2]
                P.dma("sp", x.all(), xin[t * 128:(t + 1) * 128, :])
                P.act(junk.all(), x.all(), AF.Square, accum=ss.all())
                rstd_of(ss.all(), rstd.all(), D)
                P.stt(tmp32.all(), x.all(), rstd.all(), sb[who].all(), ALU.mult, ALU.mult)
                P.tt("pool", hb[ti % 2].all(), tmp32.all(), shb[who].all(), ALU.add)
                P.transposes([(pT[ti % 2][:, c * 128:(c + 1) * 128], hb[ti % 2][:, c * 128:(c + 1) * 128]) for c in range(8)], identb.all())
                pv_ = pT[ti % 2].all()
                P.copy("act", hT[:, :, ti * 128:(ti + 1) * 128], pv_.with_ap(pv_.ap.rearrange("p (c t) -> p c t", c=8)))

        def S2(blk):
            ntile = 4 if blk < 8 else 2
            ntok = ntile * 128
            tok0 = blk * 512
            hT = hTs[blk % 2]
            if blk < 8:
                P.dma("sp", cst[:, 0, :], rope_d[0, :, tok0:tok0 + 512])
                P.dma("sp", cst[:, 1, :], rope_d[1, :, tok0:tok0 + 512])
            for g in range(16):
                col0 = [0, 512, 1536, 2048][g // 4] + (g % 4) * 128
                ps = pa[g % 2]
                P.mm(ps[:, 0:ntok], [(wb[:, c, col0:col0 + 128], hT[:, c, 0:ntok]) for c in range(8)])
                if g < 8 and blk < 8:
                    sc0 = 3072 + (g // 4) * 512 + (g % 4) * 128
                    ps2 = pb[g % 2]
                    P.mm(ps2[:, 0:ntok], [(wb[:, c, sc0:sc0 + 128], hT[:, c, 0:ntok]) for c in range(8)])
                    P.tt("dve", t1[:, 0:ntok], ps[:, 0:ntok], cst[:, 0, 0:ntok], ALU.mult)
                    P.tt("dve", t2[:, 0:ntok], ps2[:, 0:ntok], cst[:, 1, 0:ntok], ALU.mult)
                    P.tt("pool", fst[:, g, 0:ntok], t1[:, 0:ntok], t2[:, 0:ntok], ALU.add)
                else:
                    P.copy("act", fst[:, g, 0:ntok], ps[:, 0:ntok])
            for gi, dst in enumerate((qTd, kTd, qTn, kTn)):
                dv = dst[:, :, tok0:tok0 + ntok]
                P.dma("pool", dv.with_ap(dv.ap.rearrange("h p t -> p h t")), fst[:, gi * 4:(gi + 1) * 4, 0:ntok])
            for ti in range(ntile):
                for vi, c0 in enumerate((1024, 2560)):
                    psv = pv[vi]
                    P.mm(psv.all(), [(hT[:, c, ti * 128:(ti + 1) * 128], wb[:, c, c0:c0 + 512]) for c in range(8)])
                    P.copy("dve" if vi == 0 else "act", vst[:, ti, vi * 512:(vi + 1) * 512], psv.all())
            for vi, dst in enumerate((vd, vn)):
                dv = dst[tok0:tok0 + ntok, :]
                P.dma("pool", dv.with_ap(dv.ap.rearrange("(t p) c -> p t c", p=128)), vst[:, 0:ntile, vi * 512:(vi + 1) * 512])

        S1(0)
        for blk in range(9):
            if blk + 1 < 9:
                S1(blk + 1)
            S2(blk)
        P.pop()

    def phase_B0():
        lam_init = 0.2
        P.push()
        QT = [P.sbuf("QTd%d" % i, [128, LT], BF16) for i in range(2)]
        KT = [P.sbuf("KTd%d" % i, [128, LT], BF16) for i in range(2)]
        V = [P.sbuf("Vd%d" % i, [128, NT, 130], BF16) for i in range(2)]
        for i in range(2):
            P.memset("dve", V[i][:, :, 128:130], 1.0)
        pt = [P.sbuf("pt%d" % i, [128, 2, 512], BF16) for i in range(3)]
        dl = P.sbuf("dl", [128, 256], F32)
        dtmp = P.sbuf("dtmp", [128, 128], F32)
        sm = P.sbuf("lsm", [128, 2], F32)
        lam = P.sbuf("lam", [128, 1], F32)
        nlam = P.sbuf("nlam", [128, 1], F32)
        gsub = P.sbuf("gsub", [128, 128], F32)
        P.dma("sp", dl.all(), brow(diff_lambda, 0, 256))
        P.tt("dve", dtmp[:, 0:64], dl[:, 0:64], dl[:, 64:128], ALU.mult)
        P.tt("dve", dtmp[:, 64:128], dl[:, 128:192], dl[:, 192:256], ALU.mult)
        P.act(dtmp[:, 0:64], dtmp[:, 0:64], AF.Identity, accum=sm[:, 0:1])
        P.act(dtmp[:, 64:128], dtmp[:, 64:128], AF.Identity, accum=sm[:, 1:2])
        P.act(sm.all(), sm.all(), AF.Exp)
        P.tt("dve", lam.all(), sm[:, 0:1], sm[:, 1:2], ALU.subtract)
        P.ts("dve", nlam.all(), lam.all(), lam_init, -1.0, ALU.add, ALU.mult)
        P.dma("sp", gsub.all(), brow(diff_subln, 0, 128))
        P.ts("dve", gsub.all(), gsub.all(), 1.0 - lam_init, None, ALU.mult)
        osb = [P.sbuf("osb%d" % i, [128, 4, 2, 129], F32) for i in range(2)]
        rl = P.sbuf("rl", [128, 4, 2, 1], F32)
        aall = P.sbuf("aall", [128, 4, 128], F32)
        atmp = P.sbuf("atmp", [128, 4, 128], F32)
        junk = P.sbuf("junkd", [128, 128], F32)
        ss = P.sbuf("ssd", [128, 4], F32)
        rs = P.sbuf("rsd", [128, 4], F32)
        aost = [P.sbuf("aost%d" % i, [128, 4, 128], BF16) for i in range(2)]
        psS = [P.psum("psS%d" % b, [128, 1024], F32) for b in range(2)]
        psO = [P.psum("psO%d" % i, [128, 512], F32) for i in range(3)]
        it = 0
        for h in range(4):
            b = h % 2
            P.dma("sp", QT[b].all(), qTd[h])
            P.dma("sp", KT[b].all(), kTd[h])
            sv = vd[:, h * 128:(h + 1) * 128]
            P.dma("sp", V[b][:, :, 0:128], sv.with_ap(sv.ap.rearrange("(t p) c -> p t c", p=128)))
            for qb in range(9):
                nq = 4 if qb < 8 else 2
                nqt = nq * 128
                q0 = qb * 512
                kts = list(range(NT)) if qb < 8 else [32, 33]
                first = [True, True, True]

                def qk_ex(ki):
                    kt = kts[ki]
                    s_ = psS[ki % 2]
                    for m in range(2):
                        P.mm(s_[:, m * 512:m * 512 + nqt], [(KT[b][m * 64:(m + 1) * 64, kt * 128:(kt + 1) * 128],
                                                             QT[b][m * 64:(m + 1) * 64, q0:q0 + nqt])])
                    p = pt[ki % 3]
                    sv_ = s_.all()
                    sv3 = sv_.with_ap(sv_.ap.rearrange("p (m q) -> p m q", m=2)[:, :, 0:nqt])
                    P.act(p[:, :, 0:nqt], sv3, AF.Exp, scale=0.125)

                def pv(ki):
                    kt = kts[ki]
                    p = pt[ki % 3]
                    for qi in range(nq):
                        for m in range(2):
                            a = qi * 2 + m
                            bank, off = a // 3, (a % 3) * 129
                            P.mm(psO[bank][:, off:off + 129], [(p[:, m, qi * 128:(qi + 1) * 128], V[b][:, kt, 0:129])],
                                 start=first[bank], skip=True)
                            first[bank] = False

                qk_ex(0)
                for ki in range(len(kts)):
                    if ki + 1 < len(kts):
                        qk_ex(ki + 1)
                    pv(ki)
                ao = aost[it % 2]
                ob_ = osb[it % 2]
                it += 1
                na = nq * 2
                for bank in range((na + 2) // 3):
                    n_in = min(3, na - bank * 3)
                    ov = ob_.all()
                    o2 = ov.with_ap(ov.ap.rearrange("p a m c -> p (a m c)")[:, bank * 387:bank * 387 + n_in * 129])
                    P.copy("dve", o2, psO[bank][:, 0:n_in * 129])
                P.recip(rl[:, 0:nq], ob_[:, 0:nq, :, 128:129])
                P.ts("dve", rl[:, 0:nq, 1, :], rl[:, 0:nq, 1, :], nlam.all(), None, ALU.mult)
                r0 = rl[:, 0:nq, 0, :]
                r1_ = rl[:, 0:nq, 1, :]
                P.tt("dve", aall[:, 0:nq, :], ob_[:, 0:nq, 0, 0:128], r0.with_ap(r0.ap.to_broadcast([128, nq, 128])), ALU.mult)
                P.tt("pool", atmp[:, 0:nq, :], ob_[:, 0:nq, 1, 0:128], r1_.with_ap(r1_.ap.to_broadcast([128, nq, 128])), ALU.mult)
                P.tt("dve", aall[:, 0:nq, :], aall[:, 0:nq, :], atmp[:, 0:nq, :], ALU.add)
                for qi in range(nq):
                    P.stt(junk.all(), aall[:, qi, :], 1.0, aall[:, qi, :], ALU.mult, ALU.mult, accum=ss[:, qi:qi + 1])
                P.act(rs[:, 0:nq], ss[:, 0:nq], AF.Sqrt, scale=1.0 / 128, bias=eps_t[:, 0:1])
                P.recip(rs[:, 0:nq], rs[:, 0:nq])
                for qi in range(nq):
                    P.stt(ao[:, qi, :], aall[:, qi, :], rs[:, qi:qi + 1], gsub.all(), ALU.mult, ALU.mult)
                dv = ao_d[q0:q0 + nqt, h * 128:(h + 1) * 128]
                P.dma("pool", dv.with_ap(dv.ap.rearrange("(t p) c -> p t c", p=128)), ao[:, 0:nq, :])
        P.pop()

    def phase_C0():
        P.push()
        QTn_ = P.sbuf("QTn", [128, 4, LT], BF16)
        KTn_ = P.sbuf("KTn", [128, 4, LT], BF16)
        Vn_ = P.sbuf("Vn", [128, NT, 8, 66], BF16)
        P.memset("dve", Vn_[:, :, :, 64:66], 1.0)
        for hp in range(4):
            P.dma("sp", QTn_[:, hp, :], qTn[hp])
            P.dma("act", KTn_[:, hp, :], kTn[hp])
        for h in range(8):
            sv = vn[:, h * 64:(h + 1) * 64]
            P.dma("sp" if h % 2 == 0 else "act", Vn_[:, :, h, 0:64], sv.with_ap(sv.ap.rearrange("(t p) c -> p t c", p=128)))
        breg = P.sbuf("nab", [128, 5, 8, 128], F32)
        bedge = P.sbuf("nae", [128, 4, 8, 128], F32)
        for i in range(5):
            P.dma("sp", breg[:, i, :, :], na_bias[i])
        ssb = [P.sbuf("nas%d" % i, [128, 8, 128], F32) for i in range(2)]
        ptn = [P.sbuf("napt%d" % i, [128, 8, 128], BF16) for i in range(3)]
        onst = [P.sbuf("naost%d" % i, [128, 512], BF16) for i in range(2)]
        rr = P.sbuf("narr", [128, 8], F32)
        psS = [[P.psum("nS%d%d" % (b, k), [128, 512], F32) for k in range(2)] for b in range(2)]
        psO = [[P.psum("nO%d%d" % (b, k), [128, 512], F32) for k in range(2)] for b in range(2)]
        per_j, _ = _na_tiles()
        _js = [int(v) for v in os.environ.get('C0_J', '').split(',') if v and v != 'none'] or list(range(NT))
        if os.environ.get('C0_J') == 'none':
            _js = []
        _nopv = os.environ.get('C0_NOPV') == '1'
        _noexp = os.environ.get('C0_NOEXP') == '1'
        for j in _js:
            if j < 32:
                tiles = per_j[j] + [(32, None), (33, None)]
            else:
                tiles = [(32, None), (33, None)]
            base = {0: 5, 1: 9, 30: 13, 31: 17}.get(j)
            if base is not None:
                for i in range(4):
                    P.dma("sp", bedge[:, i, :, :], na_bias[base + i])
            first = [True, True]
            ob = psO[j % 2]
            def qk_ex(ki):
                kt, pid = tiles[ki]
                s_ = psS[ki % 2]
                for h in range(8):
                    hp, hh = h // 2, h % 2
                    P.mm(s_[hh][:, hp * 128:(hp + 1) * 128],
                         [(KTn_[hh * 64:(hh + 1) * 64, hp, kt * 128:(kt + 1) * 128], QTn_[hh * 64:(hh + 1) * 64, hp, j * 128:(j + 1) * 128])])
                p = ptn[ki % 3]
                sbuf_s = ssb[ki % 2]
                for half in range(2):
                    pv_ = s_[half].all()
                    pv3 = pv_.with_ap(pv_.ap.rearrange("p (h q) -> p h q", h=4))
                    hs = slice(half * 4, half * 4 + 4)
                    if pid is not None:
                        bt = breg[:, pid, hs, :] if pid < 5 else bedge[:, pid - base, hs, :]
                        P.stt(sbuf_s[:, hs, :], pv3, 0.125, bt, ALU.mult, ALU.add)
                        P.act(p[:, hs, :], sbuf_s[:, hs, :], AF.Exp)
                    else:
                        P.act(p[:, hs, :], pv3, AF.Exp, scale=0.125)

            def pv(ki):
                kt, pid = tiles[ki]
                p = ptn[ki % 3]
                for h in range(8):
                    bk, off = h // 4, (h % 4) * 65
                    P.mm(ob[bk][:, off:off + 65], [(p[:, (h % 2) * 4 + h // 2, :], Vn_[:, kt, h, 0:65])], start=first[bk], skip=True)
                    first[bk] = False

            qk_ex(0)
            for ki in range(len(tiles)):
                if ki + 1 < len(tiles):
                    qk_ex(ki + 1)
                pv(ki)
            if _nopv:
                continue
            for h in range(8):
                bk, off = h // 4, (h % 4) * 65
                P.recip(rr[:, h:h + 1], ob[bk][:, off + 64:off + 65])
                P.ts("dve", onst[j % 2][:, h * 64:(h + 1) * 64], ob[bk][:, off:off + 64], rr[:, h:h + 1], None, ALU.mult)
            P.dma("pool", ao_d[j * 128:(j + 1) * 128, 512:1024], onst[j % 2].all())
        P.pop()

    def phase_D(l, ntiles, affT):
        w_out = w_out0 if l == 0 else w_out1
        xsrc = xin if l == 0 else x1_d
        P.push()
        wob = P.sbuf("wob", [128, 8, D], BF16)
        wst = [P.sbuf("wost%d" % i, [128, 8, 256], F32) for i in range(2)]
        for nb in range(4):
            src = w_out[:, nb * 256:(nb + 1) * 256]
            P.dma("sp" if nb % 2 == 0 else "act", wst[nb % 2].all(), src.with_ap(src.ap.rearrange("(c p) n -> p c n", p=128)))
            P.copy(cast_rot(nb), wob[:, :, nb * 256:(nb + 1) * 256], wst[nb % 2].all())
        nwho = 2 if l == 0 else 1
        xt = [P.sbuf("dxt%d" % i, [128, D], F32) for i in range(2)]
        tmp32 = P.sbuf("dtmp32", [128, D], F32)
        g1b = [P.sbuf("g1b%d" % i, [128, D], F32) for i in range(nwho)]
        s2b = [P.sbuf("s2b%d" % i, [128, D], F32) for i in range(nwho)]
        sh2b = [P.sbuf("sh2b%d" % i, [128, D], F32) for i in range(nwho)]
        for who in range(nwho):
            P.dma("sp", g1b[who].all(), modrow(l, who, "g1"))
            make_sb(l, who, 2, s2b[who], sh2b[who], xt[0], xt[1])
        wr = P.sbuf("wr", [128, 8, 16], F32)
        srcr = moe_router[l]
        P.dma("sp", wr.all(), srcr.with_ap(srcr.ap.rearrange("(c p) e -> p c e", p=128)))
        aot = [P.sbuf("aot%d" % i, [128, D], BF16) for i in range(2)]
        aoT = [P.sbuf("aoT%d" % i, [128, 8, 128], BF16) for i in range(2)]
        xn = [P.sbuf("dxn%d" % i, [128, D], F32) for i in range(2)]
        h2 = [P.sbuf("dh2%d" % i, [128, D], F32) for i in range(2)]
        h2b = [P.sbuf("dh2b%d" % i, [128, D], BF16) for i in range(2)]
        h2T = P.sbuf("dh2T", [128, 8, 128], F32)
        junk = P.sbuf("djunk", [128, D], BF16)
        ss = P.sbuf("dss", [128, 1], F32)
        rstd = P.sbuf("drstd", [128, 1], F32)
        mx = P.sbuf("dmx", [128, 1], F32)
        sme = P.sbuf("dsme", [128, 1], F32)
        ex = P.sbuf("dex", [128, 16], F32)
        aff = P.sbuf("daff", [128, 16], F32)
        pT = P.psum("dpT", [128, 1024], BF16)
        pY = [P.psum("dpY%d" % i, [128, 512], F32) for i in range(2)]
        pH = [P.psum("dpH%d" % i, [128, 512], F32) for i in range(2)]
        pR = P.psum("dpR", [128, 512], F32)
        pR2 = P.psum("dpR2", [128, 512], F32)
        def S1(t):
            who = 0 if t < 32 else 1
            k = t % 2
            P.dma("sp", aot[k].all(), ao_d[t * 128:(t + 1) * 128, :])
            P.dma("act", xt[k].all(), xsrc[t * 128:(t + 1) * 128, :])
            P.transposes([(pT[:, c * 128:(c + 1) * 128], aot[k][:, c * 128:(c + 1) * 128]) for c in range(8)], identb.all())
            pv_ = pT.all()
            P.copy("act", aoT[k].all(), pv_.with_ap(pv_.ap.rearrange("p (c t) -> p c t", c=8)))
            for half in range(2):
                hs = slice(half * 512, (half + 1) * 512)
                P.mm(pY[half].all(), [(aoT[k][:, c, :], wob[:, c, hs]) for c in range(8)])
                P.tt("dve", tmp32[:, hs], pY[half].all(), g1b[who][:, hs], ALU.mult)
            P.tt("pool", xn[k].all(), xt[k].all(), tmp32.all(), ALU.add)
            P.dma("pool", x1_d[t * 128:(t + 1) * 128, :], xn[k].all())
            P.act(junk.all(), xn[k].all(), AF.Square, accum=ss.all())
            rstd_of(ss.all(), rstd.all(), D)
            P.stt(tmp32.all(), xn[k].all(), rstd.all(), s2b[who].all(), ALU.mult, ALU.mult)
            P.tt("pool", h2[k].all(), tmp32.all(), sh2b[who].all(), ALU.add)
            P.copy("act", h2b[k].all(), h2[k].all())
            P.dma("pool", h2_d[t * 128:(t + 1) * 128, :], h2b[k].all())

        def S2(t):
            k = t % 2
            P.transposes([(pH[c // 4][:, (c % 4) * 128:(c % 4 + 1) * 128], h2[k][:, c * 128:(c + 1) * 128]) for c in range(8)], identf.all())
            for hh in range(2):
                pv_ = pH[hh].all()
                P.copy("dve" if hh == 0 else "act", h2T[:, hh * 4:(hh + 1) * 4, :], pv_.with_ap(pv_.ap.rearrange("p (c t) -> p c t", c=4)))
            P.mm(pR[:, 0:16], [(h2T[:, c, :], wr[:, c, :]) for c in range(8)])
            P.op("dve", lambda e: e.reduce_max(out=mx.all().ap, in_=pR[:, 0:16].ap, axis=mybir.AxisListType.X), [pR[:, 0:16]], [mx.all()])
            P.ts("dve", mx.all(), mx.all(), -1.0, None, ALU.mult)
            P.act(ex.all(), pR[:, 0:16], AF.Exp, bias=mx.all(), accum=sme.all())
            P.recip(sme.all(), sme.all())
            P.ts("dve", aff.all(), ex.all(), sme.all(), None, ALU.mult)
            P.transposes([(pR2[0:16, 0:128], aff.all())], identf.all())
            P.copy("dve", affT[:, t * 128:(t + 1) * 128], pR2[0:16, 0:128])

        S1(0)
        for t in range(ntiles):
            if t + 1 < ntiles:
                S1(t + 1)
            S2(t)
        if debug:
            P.dma("sp", dbg_aff[:, 0:ntiles * 128], affT[:, 0:ntiles * 128])
        P.pop()

    def phase_topk(l, affT):
        P.push()
        work = P.sbuf("tkw", [16, L], F32)
        vals = P.sbuf("tkv", [16, 512], F32)
        idxu = P.sbuf("tki", [16, 512], U32)
        idxf = P.sbuf("tkf", [16, 512], F32)
        pX = [P.psum("tkp%d" % i, [128, 512], F32) for i in range(2)]
        tf = P.sbuf("tktf", [128, 16], F32)

        def topk(src0, wk, v, iu, rounds):
            src = src0
            for r in range(rounds):
                v8 = v[:, r * 8:(r + 1) * 8]
                i8 = iu[:, r * 8:(r + 1) * 8]
                P.op("dve", lambda e, s=src, a=v8: e.max(out=a.ap, in_=s.ap), [src], [v8])
                P.op("dve", lambda e, s=src, a=v8, b=i8: e.max_index(out=b.ap, in_max=a.ap, in_values=s.ap), [src, v8], [i8])
                if r < rounds - 1:
                    P.op("dve", lambda e, s=src, a=v8, w=wk: e.match_replace(out=w.ap, in_to_replace=a.ap, in_values=s.ap, imm_value=-1.0),
                         [src, v8], [wk])
                    src = wk
        topk(affT[:, 0:L], work.all(), vals, idxu, 64)
        P.copy("dve", idxf.all(), idxu.all())
        for st in range(4):
            P.transposes([(pX[0][:, 0:16], vals[:, st * 128:(st + 1) * 128])], identf[0:16, 0:16])
            P.copy("dve", gateT[:, st, :], pX[0][:, 0:16])
            P.transposes([(pX[1][:, 0:16], idxf[:, st * 128:(st + 1) * 128])], identf[0:16, 0:16])
            P.copy("dve", tf.all(), pX[1][:, 0:16])
            P.copy("dve", idxT[:, st, :], tf.all())
        if l == 0:
            workc = P.sbuf("tkwc", [16, LC], F32)
            valsc = P.sbuf("tkvc", [16, 32], F32)
            idxcu = P.sbuf("tkic", [16, 32], U32)
            idxcf = P.sbuf("tkfc", [16, 32], F32)
            topk(affT[:, L:LT], workc.all(), valsc, idxcu, 4)
            P.copy("dve", idxcf.all(), idxcu.all())
            P.ts("dve", idxcf.all(), idxcf.all(), float(L), None, ALU.add)
            P.memset("dve", gateT[:, 4, :], 0.0)
            P.dma("sp", idxT[:, 4, :], dummy_idx.all())
            P.transposes([(pX[0][0:32, 0:16], valsc.all())], identf[0:16, 0:16])
            P.copy("dve", gateT[0:32, 4, :], pX[0][0:32, 0:16])
            P.transposes([(pX[1][0:32, 0:16], idxcf.all())], identf[0:16, 0:16])
            P.copy("dve", tf[0:32, :], pX[1][0:32, 0:16])
            P.copy("dve", idxT[0:32, 4, :], tf[0:32, :])
        if debug:
            P.dma("sp", dbg_idx.all(), idxT.all())
        P.pop()

    def phase_experts(l, NS):
        P.push()
        wbf = [[P.sbuf("ew%d_%d" % (wi, i), [128, 8, D], BF16) for i in range(2)] for wi in range(3)]
        wst = [P.sbuf("est%d" % i, [128, 2, 1024], F32) for i in range(3)]
        xes = [P.sbuf("xe%d" % i, [128, NS, D], BF16) for i in range(2)]
        xeT = P.sbuf("xeT", [128, 8, NS * 128], BF16)
        hidT = P.sbuf("hidT", [128, 8, NS * 128], BF16)
        s1 = P.sbuf("es1", [128, NS * 128], F32)
        yst = [P.sbuf("yst%d" % i, [128, D], F32) for i in range(3)]
        nwho = 2 if NS == 5 else 1
        g2b = [P.sbuf("g2b%d" % i, [128, D], F32) for i in range(nwho)]
        for who in range(nwho):
            P.dma("sp", g2b[who].all(), modrow(l, who, "g2"))
        pT1 = P.psum("epT", [128, 1024], BF16)
        pT = [pT1, pT1]
        h1As = [P.psum("eh1A%d" % i, [128, 512], F32) for i in range(2)]
        h3As = [P.psum("eh3A%d" % i, [128, 512], F32) for i in range(2)]
        hB = P.psum("ehB", [128, 512], F32)
        hB3 = hB
        pYe = [P.psum("epY%d" % i, [128, 512], F32) for i in range(2)]
        NSL = NS * 128
        state = {"n": 0}

        def wsteps(e):
            buf = e % 2
            for wi in range(3):
                for cb in range(4):
                    yield (wi, cb, buf, e)

        def do_step(step):
            wi, cb, buf, e = step
            i = state["n"]
            state["n"] += 1
            st = wst[i % 3]
            src = moe_w[wi][l, e, cb * 256:(cb + 1) * 256, :]
            P.dma("sp" if i % 2 == 0 else "act", st.all(), src.with_ap(src.ap.rearrange("(c p) n -> p c n", p=128)))
            P.copy(("act", "dve", "act")[i % 3], wbf[wi][buf][:, 2 * cb:2 * cb + 2, :], st.all())

        for step in wsteps(0):
            do_step(step)
        for e in range(16):
            buf = e % 2
            nxt = wsteps(e + 1) if e + 1 < 16 else iter(())

            def pre(n=1):
                for _ in range(n):
                    s = next(nxt, None)
                    if s is not None:
                        do_step(s)
            def gather(ee):
                xg = xes[ee % 2]
                for st in range(NS):
                    iv = idxT[:, st, ee:ee + 1]
                    P.dma("pool", xg[:, st, :], h2_d.all(),
                          fn=lambda en, o=xg[:, st, :].ap, i_=iv.ap: en.indirect_dma_start(
                              out=o, out_offset=None, in_=h2_d.all().ap, in_offset=bass.IndirectOffsetOnAxis(ap=i_, axis=0)),
                          extra_reads=[iv])
            if e == 0:
                gather(0)
            if e + 1 < 16:
                gather(e + 1)
            xe = xes[e % 2]
            for c in range(8):
                P.transposes([(pT[c % 2][:, st * 128:(st + 1) * 128], xe[:, st, c * 128:(c + 1) * 128]) for st in range(NS)], identb.all())
                P.copy("act" if c % 2 == 0 else "dve", xeT[:, c, :], pT[c % 2][:, 0:NSL])
            w1, w3, w2 = wbf[0][buf], wbf[1][buf], wbf[2][buf]
            for fc in range(8):
                fs = slice(fc * 128, (fc + 1) * 128)
                h1A, h3A = h1As[fc % 2], h3As[fc % 2]
                P.mm(h1A.all(), [(w1[:, c, fs], xeT[:, c, 0:512]) for c in range(8)])
                P.mm(h3A.all(), [(w3[:, c, fs], xeT[:, c, 0:512]) for c in range(8)])
                if NS == 5:
                    P.mm(hB[:, 0:128], [(w1[:, c, fs], xeT[:, c, 512:640]) for c in range(8)])
                    P.mm(hB3[:, 128:256], [(w3[:, c, fs], xeT[:, c, 512:640]) for c in range(8)])
                P.act(s1[:, 0:512], h1A.all(), AF.Silu)
                P.tt("dve", hidT[:, fc, 0:512], s1[:, 0:512], h3A.all(), ALU.mult)
                if NS == 5:
                    P.act(s1[:, 512:640], hB[:, 0:128], AF.Silu)
                    P.tt("dve", hidT[:, fc, 512:640], s1[:, 512:640], hB3[:, 128:256], ALU.mult)
                pre(1)
            for st in range(NS):
                y = yst[(e * NS + st) % 3]
                who = 1 if st == 4 else 0
                for half in range(2):
                    hs = slice(half * 512, (half + 1) * 512)
                    P.mm(pYe[half].all(), [(hidT[:, fc, st * 128:(st + 1) * 128], w2[:, fc, hs]) for fc in range(8)])
                    P.stt(y[:, hs], pYe[half].all(), gateT[:, st, e:e + 1], g2b[who][:, hs], ALU.mult, ALU.mult)
                iv = idxT[:, st, e:e + 1]
                P.dma("pool", x1_d.all(), y.all(),
                      fn=lambda en, y_=y.all().ap, i_=iv.ap: en.indirect_dma_start(
                          out=x1_d.all().ap, out_offset=bass.IndirectOffsetOnAxis(ap=i_, axis=0), in_=y_, in_offset=None,
                          compute_op=ALU.add),
                      extra_reads=[iv])
                pre(1)
            pre(12)
        P.pop()

    def phase_final():
        P.push()
        fnb = P.sbuf("fnb", [128, D], F32)
        P.dma("sp", fnb.all(), brow(final_norm, 0, D))
        xt = [P.sbuf("fxt%d" % i, [128, D], F32) for i in range(2)]
        ot = [P.sbuf("fot%d" % i, [128, D], F32) for i in range(2)]
        junk = P.sbuf("fjunk", [128, D], BF16)
        ss = P.sbuf("fss", [128, 1], F32)
        rstd = P.sbuf("frstd", [128, 1], F32)
        for t in range(32):
            k = t % 2
            P.dma("sp", xt[k].all(), x1_d[t * 128:(t + 1) * 128, :])
            P.act(junk.all(), xt[k].all(), AF.Square, accum=ss.all())
            rstd_of(ss.all(), rstd.all(), D)
            P.stt(ot[k].all(), xt[k].all(), rstd.all(), fnb.all(), ALU.mult, ALU.mult)
            P.dma("act", out_d[t * 128:(t + 1) * 128, :], ot[k].all())
        P.pop()

    def phase_A1():
        P.push()
        NW = 2944
        wb = P.sbuf("w1b", [128, 8, NW], BF16)
        wst = [P.sbuf("w1st%d" % i, [128, 8, 256], F32) for i in range(2)]
        nb = 0
        c0 = 0
        while c0 < NW:
            w_ = min(256, NW - c0)
            src = w_in1[:, c0:c0 + w_]
            P.dma("sp" if nb % 2 == 0 else "act", wst[nb % 2][:, :, 0:w_], src.with_ap(src.ap.rearrange("(c p) n -> p c n", p=128)))
            P.copy(cast_rot(nb), wb[:, :, c0:c0 + w_], wst[nb % 2][:, :, 0:w_])
            c0 += w_
            nb += 1
        sb = [P.sbuf("sb%d" % i, [128, D], F32) for i in range(2)]
        shb = [P.sbuf("shb%d" % i, [128, D], F32) for i in range(2)]
        xt = [P.sbuf("xt%d" % i, [128, D], F32) for i in range(2)]
        tmp32 = P.sbuf("tmp32", [128, D], F32)
        junk = P.sbuf("junk", [128, D], BF16)
        make_sb(1, 0, 1, sb[0], shb[0], xt[0], xt[1])
        make_sb(1, 1, 1, sb[1], shb[1], xt[0], xt[1])
        qnb = P.sbuf("qnb", [128, 384], F32)
        P.dma("sp", qnb[:, 0:256], brow(mla_q_norm, 0, 256))
        P.dma("sp", qnb[:, 256:384], brow(mla_kv_norm, 0, 128))
        hb = [P.sbuf("hb%d" % i, [128, D], BF16) for i in range(2)]
        hTs = [P.sbuf("hT%d" % i, [128, 8, 512], BF16) for i in range(2)]
        cst = P.sbuf("cst", [128, 2, 512], F32)
        fst = P.sbuf("fst", [128, 9, 512], BF16)
        cqst = P.sbuf("cqst", [128, 3, 512], BF16)
        cqn = P.sbuf("cqn", [128, 384], BF16)
        tkst = P.sbuf("tkst", [128, 4, 1280], BF16)
        t1 = P.sbuf("rt1", [128, 512], F32)
        t2 = P.sbuf("rt2", [128, 512], F32)
        ss = P.sbuf("ss", [128, 2], F32)
        rstd = P.sbuf("rstd", [128, 2], F32)
        pT = [P.psum("pT%d" % i, [128, 1024], BF16) for i in range(2)]
        pa = [P.psum("pa%d" % i, [128, 512], F32) for i in range(2)]
        pb = P.psum("pb", [128, 512], F32)
        pv = [P.psum("pv%d" % i, [128, 512], F32) for i in range(2)]
        pq = P.psum("pq", [128, 1024], BF16)
        def S1(blk):
            ntile = 4 if blk < 8 else 2
            who = 0 if blk < 8 else 1
            hT = hTs[blk % 2]
            for ti in range(ntile):
                t = blk * 4 + ti
                x = xt[ti % 2]
                P.dma("sp", x.all(), x1_d[t * 128:(t + 1) * 128, :])
                P.act(junk.all(), x.all(), AF.Square, accum=ss[:, 0:1])
                rstd_of(ss[:, 0:1], rstd[:, 0:1], D)
                P.stt(tmp32.all(), x.all(), rstd[:, 0:1], sb[who].all(), ALU.mult, ALU.mult)
                P.tt("pool", hb[ti % 2].all(), tmp32.all(), shb[who].all(), ALU.add)
                P.transposes([(pT[ti % 2][:, c * 128:(c + 1) * 128], hb[ti % 2][:, c * 128:(c + 1) * 128]) for c in range(8)], identb.all())
                pv_ = pT[ti % 2].all()
                P.copy("act", hT[:, :, ti * 128:(ti + 1) * 128], pv_.with_ap(pv_.ap.rearrange("p (c t) -> p c t", c=8)))

        def S2(blk):
            ntile = 4 if blk < 8 else 2
            ntok = ntile * 128
            tok0 = blk * 512
            hT = hTs[blk % 2]
            for ti in range(ntile):
                hts = slice(ti * 128, (ti + 1) * 128)
                P.mm(pv[0][:, 0:384], [(hT[:, c, hts], wb[:, c, 0:384]) for c in range(8)])
                P.act(junk[:, 0:256], pv[0][:, 0:256], AF.Square, accum=ss[:, 0:1])
                P.act(junk[:, 256:384], pv[0][:, 256:384], AF.Square, accum=ss[:, 1:2])
                rstd_of(ss[:, 0:1], rstd[:, 0:1], 256)
                rstd_of(ss[:, 1:2], rstd[:, 1:2], 128)
                P.stt(cqn[:, 0:256], pv[0][:, 0:256], rstd[:, 0:1], qnb[:, 0:256], ALU.mult, ALU.mult)
                P.stt(cqn[:, 256:384], pv[0][:, 256:384], rstd[:, 1:2], qnb[:, 256:384], ALU.mult, ALU.mult)
                P.transposes([(pq[:, c * 128:(c + 1) * 128], cqn[:, c * 128:(c + 1) * 128]) for c in range(3)], identb.all())
                pq_ = pq[:, 0:384]
                P.copy("dve", cqst[:, :, hts], pq_.with_ap(pq_.ap.rearrange("p (c t) -> p c t", c=3)))
                P.mm(pv[1][:, 0:256], [(hT[:, c, hts], wb[:, c, 384:640]) for c in range(8)])
                P.copy("dve", tkst[:, ti, 0:256], pv[1][:, 0:256])
                P.mm(pv[0].all(), [(hT[:, c, hts], wb[:, c, 640:1152]) for c in range(8)])
                P.copy("act", tkst[:, ti, 256:768], pv[0].all())
                P.mm(pv[1].all(), [(hT[:, c, hts], wb[:, c, 1152:1664]) for c in range(8)])
                P.act(tkst[:, ti, 768:1280], pv[1].all(), AF.Silu)
            if blk < 8:
                P.dma("sp", cst[:, 0, :], rope_m[0, :, tok0:tok0 + 512])
                P.dma("sp", cst[:, 1, :], rope_m[1, :, tok0:tok0 + 512])
            P.mm(pa[0][:, 0:ntok], [(wb[:, c, 1664:1792], hT[:, c, 0:ntok]) for c in range(8)])
            if blk < 8:
                P.mm(pb[:, 0:ntok], [(wb[:, c, 1792:1920], hT[:, c, 0:ntok]) for c in range(8)])
                P.tt("dve", t1[:, 0:ntok], pa[0][:, 0:ntok], cst[:, 0, 0:ntok], ALU.mult)
                P.tt("dve", t2[:, 0:ntok], pb[:, 0:ntok], cst[:, 1, 0:ntok], ALU.mult)
                P.tt("pool", fst[:, 0, 0:ntok], t1[:, 0:ntok], t2[:, 0:ntok], ALU.add)
            else:
                P.copy("act", fst[:, 0, 0:ntok], pa[0][:, 0:ntok])
            for g in range(8):
                col0 = 1920 + g * 128
                ps = pa[(g + 1) % 2]
                P.mm(ps[:, 0:ntok], [(wb[:, c, col0:col0 + 128], hT[:, c, 0:ntok]) for c in range(8)])
                P.copy("act" if g % 2 == 0 else "dve", fst[:, 1 + g, 0:ntok], ps[:, 0:ntok])
            P.dma("pool", krT_d[:, tok0:tok0 + ntok], fst[:, 0, 0:ntok])
            for gi, dst in enumerate((rqT_d, rkT_d)):
                dv = dst[:, :, tok0:tok0 + ntok]
                P.dma("pool", dv.with_ap(dv.ap.rearrange("h p t -> p h t")), fst[:, 1 + gi * 4:5 + gi * 4, 0:ntok])
            dv = cqn_d[:, :, tok0:tok0 + ntok]
            P.dma("pool", dv.with_ap(dv.ap.rearrange("c p t -> p c t")), cqst[:, :, 0:ntok])
            for dst, lo, hi in ((rk_d, 0, 256), (rv_d, 256, 768), (sg_d, 768, 1280)):
                dv = dst[tok0:tok0 + ntok, :]
                P.dma("pool", dv.with_ap(dv.ap.rearrange("(t p) c -> p t c", p=128)), tkst[:, 0:ntile, lo:hi])

        S1(0)
        for blk in range(9):
            if blk + 1 < 9:
                S1(blk + 1)
            S2(blk)
        P.pop()

    def phase_B1():
        P.push()
        cqnT = P.sbuf("cqnT", [128, 3, LT], BF16)
        for c in range(3):
            P.dma("sp" if c % 2 == 0 else "act", cqnT[:, c, :], cqn_d[c])
        wuqf = P.sbuf("wuqf", [128, 2, 1536], F32)
        wuq = P.sbuf("wuq", [128, 2, 1536], BF16)
        P.dma("sp", wuqf.all(), w_uq.all().with_ap(w_uq.base.rearrange("(c p) n -> p c n", p=128)))
        P.copy("dve", wuq.all(), wuqf.all())
        wukvf = P.sbuf("wukvf", [128, 1024], F32)
        wukv = P.sbuf("wukv", [128, 1024], BF16)
        P.dma("sp", wukvf.all(), w_ukv.all())
        P.copy("pool", wukv.all(), wukvf.all())
        Vm = P.sbuf("Vm", [128, NT, 8, 66], BF16)
        P.memset("dve", Vm[:, :, :, 64:66], 1.0)
        KT = P.sbuf("KTm", [96, LT], BF16)
        QT = [P.sbuf("QTm%d" % i, [96, LT], BF16) for i in range(2)]
        P.dma("sp", KT[64:96, :], krT_d[64:96, :])
        csm = P.sbuf("csm", [96, 2, L], F32)
        P.dma("sp", csm[64:96, 0, :], rope_m[0, 64:96, :])
        P.dma("act", csm[64:96, 1, :], rope_m[1, 64:96, :])
        ptm = [P.sbuf("ptm%d" % i, [128, 1024], BF16) for i in range(3)]
        ost = [P.sbuf("most%d" % i, [128, 4, 64], BF16) for i in range(2)]
        ones65 = P.sbuf("ones65", [65, 64], F32)
        P.memset("dve", ones65.all(), 0.0)
        P.memset("dve", ones65[64:65, :], 1.0)
        oT = P.sbuf("moT", [64, 512], F32)
        rl = P.sbuf("mrl", [65, 512], F32)
        P.memset("dve", rl.all(), 0.0)
        mT = P.sbuf("mmT", [64, 512], F32)
        t1 = P.sbuf("mt1", [96, 512], F32)
        t2 = P.sbuf("mt2", [96, 512], F32)
        psV = [P.psum("mpV%d" % i, [128, 512], F32) for i in range(2)]
        psS = [P.psum("mpS%d" % i, [128, 1024], F32) for i in range(2)]
        psO = [P.psum("mpO%d" % i, [128, 512], F32) for i in range(2)]
        class _Half:
            def __init__(self, tt_, lo):
                self.tt_, self.lo = tt_, lo

            def __getitem__(self, idx):
                r, c = idx
                c0 = 0 if c.start is None else c.start
                c1 = 512 if c.stop is None else c.stop
                return self.tt_[r, self.lo + c0:self.lo + c1]
        psQ = _Half(psS[0], 0)
        psQ2 = _Half(psS[0], 512)
        for t in range(NT):
            P.mm(psV[t % 2].all(), [(cqnT[:, 2, t * 128:(t + 1) * 128], wukv[:, 512:1024])])
            pv_ = psV[t % 2].all()
            P.copy("act" if t % 2 == 0 else "dve", Vm[:, t, :, 0:64], pv_.with_ap(pv_.ap.rearrange("p (h e) -> p h e", h=8)))
        sc = 96.0 ** -0.5
        for h in range(8):
            q_ = QT[h % 2]
            for blk in range(9):
                ntok = 512 if blk < 8 else 256
                tok0 = blk * 512
                ts_ = slice(tok0, tok0 + ntok)
                P.mm(psV[blk % 2][0:64, 0:ntok], [(wukv[:, h * 64:(h + 1) * 64], cqnT[:, 2, ts_])])
                P.copy("pool" if False else "dve", KT[0:64, ts_], psV[blk % 2][0:64, 0:ntok])
                P.mm(psQ[0:96, 0:ntok], [(wuq[:, c, h * 96:(h + 1) * 96], cqnT[:, c, ts_]) for c in range(2)])
                P.copy("act", q_[0:64, ts_], psQ[0:64, 0:ntok])
                if blk < 8:
                    P.mm(psQ2[0:96, 0:ntok], [(wuq[:, c, 768 + h * 96:768 + (h + 1) * 96], cqnT[:, c, ts_]) for c in range(2)])
                    P.tt("dve", t1[64:96, 0:ntok], psQ[64:96, 0:ntok], csm[64:96, 0, ts_], ALU.mult)
                    P.tt("dve", t2[64:96, 0:ntok], psQ2[64:96, 0:ntok], csm[64:96, 1, ts_], ALU.mult)
                    P.tt("pool", q_[64:96, ts_], t1[64:96, 0:ntok], t2[64:96, 0:ntok], ALU.add)
                else:
                    P.copy("act", q_[64:96, ts_], psQ[64:96, 0:ntok])
            for qb in range(8):
                q0 = qb * 512
                ob = psO[qb % 2]

                def qk_ex(pi):
                    s_ = psS[pi % 2]
                    for u in range(2):
                        kt = 2 * pi + u
                        P.mm(s_[:, u * 512:(u + 1) * 512], [(KT[0:96, kt * 128:(kt + 1) * 128], q_[0:96, q0:q0 + 512])])
                    P.act(ptm[pi % 3].all(), s_.all(), AF.Exp, scale=sc)

                def pv(pi):
                    p = ptm[pi % 3]
                    for u in range(2):
                        kt = 2 * pi + u
                        P.mm(ob[0:65, :], [(Vm[:, kt, h, 0:65], p[:, u * 512:(u + 1) * 512])], start=(kt == 0), skip=True)

                qk_ex(0)
                for pi in range(NT // 2):
                    if pi + 1 < NT // 2:
                        qk_ex(pi + 1)
                    pv(pi)
                o = ost[qb % 2]
                P.copy("dve", oT.all(), ob[0:64, :])
                P.recip(rl[64:65, :], ob[64:65, :])
                P.mm(psV[0][0:64, :], [(ones65[0:65, 0:64], rl[0:65, :])])
                P.tt("dve", mT.all(), oT.all(), psV[0][0:64, :], ALU.mult)
                P.transposes([(psV[1][:, qi * 64:(qi + 1) * 64], mT[:, qi * 128:(qi + 1) * 128]) for qi in range(4)], identf[0:64, 0:64])
                pv_ = psV[1][:, 0:256]
                P.copy("act", o.all(), pv_.with_ap(pv_.ap.rearrange("p (q e) -> p q e", q=4)))
                dv = ao_d[q0:q0 + 512, h * 64:(h + 1) * 64]
                P.dma("pool", dv.with_ap(dv.ap.rearrange("(t p) c -> p t c", p=128)), o.all())
        P.pop()

    def phase_C1():
        P.push()
        dl = P.sbuf("rdl", [128, 8], F32)
        lg = P.sbuf("rlg", [128, 8], F32)
        P.dma("sp", dl.all(), brow(ret_decay, 0, 8))
        P.act(lg.all(), dl.all(), AF.Exp, scale=-1.0)
        P.ts("dve", lg.all(), lg.all(), 1.0, None, ALU.add)
        P.act(lg.all(), lg.all(), AF.Ln)
        P.ts("dve", lg.all(), lg.all(), -1.0, None, ALU.mult)
        rmp = P.sbuf("rmp", [128, 4, 128], F32)
        P.dma("sp", rmp.all(), ramps_d.all())
        lgfb = P.sbuf("lgfb", [128, 1], F32)
        dq = P.sbuf("rdq", [128, 128], F32)
        dk = P.sbuf("rdk", [128, 2], F32)
        gC = P.sbuf("rgC", [128, 1], F32)
        DT = P.sbuf("rDT", [128, 128], F32)
        dtmp = P.sbuf("rdtmp", [128, 128], F32)
        qT = P.sbuf("rqT", [128, L], BF16)
        kT = P.sbuf("rkT", [64, L], BF16)
        ktok = P.sbuf("rktok", [128, NT, 64], BF16)
        vtok = P.sbuf("rvtok", [128, NT, 128], BF16)
        sg = P.sbuf("rsg", [128, 32, 128], BF16)
        U = P.sbuf("rU", [128, NT, 128], F32)
        S = P.sbuf("rS", [128, 32, 128], F32)
        Sb = P.sbuf("rSb", [128, 32, 128], BF16)
        Kd = [P.sbuf("rKd%d" % i, [128, 128], BF16) for i in range(2)]
        AT = [P.sbuf("rAT%d" % i, [128, 128], BF16) for i in range(2)]
        Qd = [P.sbuf("rQd%d" % i, [128, 128], BF16) for i in range(2)]
        r = [P.sbuf("rr%d" % i, [128, 128], F32) for i in range(2)]
        junk = P.sbuf("rjunk", [128, 128], F32)
        ss = P.sbuf("rss", [128, 1], F32)
        rs = P.sbuf("rrs", [128, 1], F32)
        ost = [P.sbuf("rost%d" % i, [128, 4, 128], BF16) for i in range(2)]
        psU = [P.psum("rpU%d" % i, [128, 512], F32) for i in range(2)]
        psS = [P.psum("rpS%d" % i, [128, 512], F32) for i in range(2)]
        psO = [P.psum("rpO%d" % i, [128, 512], F32) for i in range(2)]
        for h in range(4):
            P.copy("dve", lgfb[0:64, :], lg[0:64, h:h + 1])
            P.copy("dve", lgfb[64:128, :], lg[64:128, 4 + h:5 + h])
            P.act(dq.all(), rmp[:, 0, :], AF.Exp, scale=lgfb.all())
            P.act(dk[:, 0:1], rmp[:, 3, 0:1], AF.Exp, scale=lg[:, h:h + 1])
            P.act(dk[:, 1:2], rmp[:, 3, 1:2], AF.Exp, scale=lg[:, 4 + h:5 + h])
            P.act(gC.all(), rmp[:, 3, 2:3], AF.Exp, scale=lgfb.all())
            P.ts("dve", dtmp.all(), rmp[:, 1, :], lg[:, h:h + 1], None, ALU.mult)
            P.stt(dtmp.all(), rmp[:, 2, :], lg[:, 4 + h:5 + h], dtmp.all(), ALU.mult, ALU.add)
            P.act(DT.all(), dtmp.all(), AF.Exp)
            P.dma("sp", qT.all(), rqT_d[h, :, 0:L])
            P.dma("act", kT.all(), rkT_d[h, 0:64, 0:L])
            sv = rk_d[:, h * 64:(h + 1) * 64]
            P.dma("sp", ktok.all(), sv.with_ap(sv.ap.rearrange("(t p) c -> p t c", p=128)))
            sv = rv_d[:, h * 128:(h + 1) * 128]
            P.dma("act", vtok.all(), sv.with_ap(sv.ap.rearrange("(t p) c -> p t c", p=128)))
            sv = sg_d[0:L, h * 128:(h + 1) * 128]
            P.dma("sp", sg.all(), sv.with_ap(sv.ap.rearrange("(t p) c -> p t c", p=128)))
            for t in range(NT):
                kd = Kd[t % 2]
                P.ts("dve", kd[:, 0:64], ktok[:, t, :], dk[:, 0:1], None, ALU.mult)
                P.ts("pool", kd[:, 64:128], ktok[:, t, :], dk[:, 1:2], None, ALU.mult)
                P.mm(psU[t % 2][:, 0:128], [(kd.all(), vtok[:, t, :])])
                P.copy("act", U[:, t, :], psU[t % 2][:, 0:128])
            P.stt(S[0:64, 0, :], U[0:64, 32, :], gC[0:64, :], U[0:64, 33, :], ALU.mult, ALU.add)
            for n in range(31):
                P.stt(S[0:64, n + 1, :], S[0:64, n, :], gC[0:64, :], U[0:64, n, :], ALU.mult, ALU.add)
            P.stt(S[64:128, 31, :], U[64:128, 33, :], gC[64:128, :], U[64:128, 32, :], ALU.mult, ALU.add)
            for n in range(31, 0, -1):
                P.stt(S[64:128, n - 1, :], S[64:128, n, :], gC[64:128, :], U[64:128, n, :], ALU.mult, ALU.add)
            P.copy("pool", Sb.all(), S.all())
            def pre(n):
                ns = slice(n * 128, (n + 1) * 128)
                P.mm(psS[n % 2][:, 0:128], [(kT[0:64, ns], qT[0:64, ns])])
                P.tt("dve", AT[n % 2].all(), psS[n % 2][:, 0:128], DT.all(), ALU.mult)
                P.tt("pool", Qd[n % 2].all(), qT[:, ns], dq.all(), ALU.mult)
            pre(0)
            for n in range(32):
                ns = slice(n * 128, (n + 1) * 128)
                if n + 1 < 32:
                    pre(n + 1)
                P.mm(psO[n % 2][:, 0:128], [(AT[n % 2].all(), vtok[:, n, :]), (Qd[n % 2].all(), Sb[:, n, :])])
                rr_ = r[n % 2]
                P.act(rr_.all(), psO[n % 2][:, 0:128], AF.Copy, scale=0.125)
                P.act(junk.all(), rr_.all(), AF.Square, accum=ss.all())
                rstd_of(ss.all(), rs.all(), 128)
                o = ost[(n // 4) % 2]
                P.stt(o[:, n % 4, :], rr_.all(), rs.all(), sg[:, n, :], ALU.mult, ALU.mult)
                if n % 4 == 3:
                    q0 = (n - 3) * 128
                    dv = ao_d[q0:q0 + 512, 512 + h * 128:512 + (h + 1) * 128]
                    P.dma("pool", dv.with_ap(dv.ap.rearrange("(t p) c -> p t c", p=128)), o.all())
        P.pop()

    seq = [
        ("A0", lambda: phase_A0()),
        ("B0", lambda: phase_B0()),
        ("C0", lambda: phase_C0()),
        ("D0", None), ("T0", None), ("E0", None),
        ("A1", lambda: phase_A1()),
        ("B1", lambda: phase_B1()),
        ("C1", lambda: phase_C1()),
        ("D1", None), ("T1", None), ("E1", None),
        ("F", lambda: phase_final()),
    ]
    affT = None
    for name, fn in seq:
        if only is not None and name not in only:
            continue
        if name in ("D0", "D1"):
            l = 0 if name == "D0" else 1
            P.push()
            affT = P.sbuf("affT", [16, LT], F32)
            phase_D(l, NT if l == 0 else 32, affT)
        elif name in ("T0", "T1"):
            l = 0 if name == "T0" else 1
            phase_topk(l, affT)
            P.pop()
        elif name in ("E0", "E1"):
            l = 0 if name == "E0" else 1
            phase_experts(l, 5 if l == 0 else 4)
        else:
            fn()
        if stop(name):
            break
    if stop_after in ("D0", "D1"):
        P.pop()
    P.finish()
    P.stats = dict(P.ninst)
    return nc


_CACHE = {}


def kernel(**inputs):
    sh, per = host_prep(inputs)
    if "nc" not in _CACHE:
        _CACHE["nc"] = build()
    nc = _CACHE["nc"]
    n = len(per)
    in_maps = []
    for b in range(n):
        m = dict(sh)
        m.update(per[b])
        in_maps.append(m)
    res = run_bass_kernel_spmd(nc, in_maps, core_ids=list(range(n)))
    return np.stack([np.asarray(r["out"], dtype=np.float32) for r in res.results], axis=0)
```

```python
import os
import numpy as np
from contextlib import ExitStack
import concourse.bass as bass
import concourse.mybir as mybir
from concourse.bass_utils import run_bass_kernel_spmd

F32 = mybir.dt.float32
BF16 = mybir.dt.bfloat16
I32 = mybir.dt.int32
U32 = mybir.dt.uint32
AF = mybir.ActivationFunctionType
ALU = mybir.AluOpType

D = 1024
L = 4096
LC = 256
LT = L + LC
NT = LT // 128
NEG = -30000.0


class View:
    __slots__ = ("ap", "tt", "reg")

    def __init__(self, ap, tt, reg):
        self.ap = ap
        self.tt = tt
        self.reg = reg

    def with_ap(self, ap):
        return View(ap, self.tt, self.reg)


class TT:
    def __init__(self, name, base_ap, shape, space):
        self.name = name
        self.base = base_ap
        self.shape = tuple(shape)
        self.space = space
        self.recs = []

    def __getitem__(self, idx):
        if not isinstance(idx, tuple):
            idx = (idx,)
        idx = tuple(idx) + (slice(None),) * (len(self.shape) - len(idx))
        reg = []
        for i, s in enumerate(idx):
            n = self.shape[i]
            if isinstance(s, int):
                assert 0 <= s < n, (self.name, idx, self.shape)
                reg.append((s, s + 1))
            else:
                lo = 0 if s.start is None else s.start
                hi = n if s.stop is None else s.stop
                assert 0 <= lo < hi <= n, (self.name, idx, self.shape)
                reg.append((lo, hi))
        return View(self.base[idx], self, tuple(reg))

    def all(self):
        return self[tuple(slice(None) for _ in self.shape)]


def _overlap(a, b):
    for (l1, h1), (l2, h2) in zip(a, b):
        if h1 <= l2 or h2 <= l1:
            return False
    return True


def _contains(a, b):
    for (l1, h1), (l2, h2) in zip(a, b):
        if l2 < l1 or h2 > h1:
            return False
    return True


class Prog:
    def __init__(self, nc, n_dma_sems=10):
        self.nc = nc
        self.eobj = {"pe": nc.tensor, "act": nc.scalar, "dve": nc.vector, "pool": nc.gpsimd, "sp": nc.sync}
        self.es = ExitStack()
        self.cnt = {e: 0 for e in self.eobj}
        self.ninst = {e: 0 for e in self.eobj}
        self.seen = {e: {} for e in self.eobj}
        self.esem = {}
        for e in ("pe", "act", "dve", "pool"):
            self.esem[e] = self.es.enter_context(nc.semaphore("es_" + e))
        self.dsem = {}
        self.dtot = {}
        self.dnext = {}
        self.semobj = {}
        for e in ("sp", "act", "pool"):
            self.dsem[e] = [self.es.enter_context(nc.semaphore("ds_%s%d" % (e, i))) for i in range(n_dma_sems)]
            self.dnext[e] = 0
            for s in self.dsem[e]:
                self.dtot[id(s)] = 0
                self.semobj[id(s)] = s
        for e, s in self.esem.items():
            self.semobj[id(s)] = s
        self.scopes = []

    def push(self):
        st = ExitStack()
        self.scopes.append(st)
        return st

    def pop(self):
        self.barrier()
        self.scopes.pop().close()

    def _stack(self):
        return self.scopes[-1] if self.scopes else self.es

    def sbuf(self, name, shape, dt):
        self.uid = getattr(self, "uid", 0) + 1
        name = "%s_%d" % (name, self.uid)
        h = self._stack().enter_context(self.nc.sbuf_tensor(name, list(shape), dt))
        return TT(name, h[tuple(slice(None) for _ in shape)], shape, "sbuf")

    def psum(self, name, shape, dt):
        self.uid = getattr(self, "uid", 0) + 1
        name = "%s_%d" % (name, self.uid)
        h = self._stack().enter_context(self.nc.psum_tensor(name, list(shape), dt))
        return TT(name, h[tuple(slice(None) for _ in shape)], shape, "psum")

    def dram(self, name, shape, dt, kind=None):
        if kind is None:
            h = self.nc.dram_tensor(name, list(shape), dt)
        else:
            h = self.nc.dram_tensor(name, list(shape), dt, kind=kind)
        return TT(name, h.ap(), shape, "dram")

    def _deps(self, reads, writes):
        deps = {}
        for v in reads:
            for r in v.tt.recs:
                if r[1] and _overlap(r[0], v.reg):
                    if deps.get(r[2], 0) < r[3]:
                        deps[r[2]] = r[3]
        for v in writes:
            for r in v.tt.recs:
                if _overlap(r[0], v.reg):
                    if deps.get(r[2], 0) < r[3]:
                        deps[r[2]] = r[3]
        return deps

    def _record(self, reads, writes, sk, val):
        for v in writes:
            recs = v.tt.recs
            recs[:] = [r for r in recs if not _contains(v.reg, r[0])]
            recs.append([v.reg, True, sk, val])
        for v in reads:
            recs = v.tt.recs
            for r in recs:
                if (not r[1]) and r[2] == sk and r[0] == v.reg:
                    if r[3] < val:
                        r[3] = val
                    break
            else:
                recs.append([v.reg, False, sk, val])

    def _psum_deps(self, eng, views, deps, val=None):
        mysk = id(self.esem[eng]) if eng in self.esem else None
        for v in views:
            if v.tt.space != "psum":
                continue
            last = v.tt.__dict__.setdefault("last", {})
            if val is None:
                for sk, lv in last.items():
                    if sk != mysk and deps.get(sk, 0) < lv:
                        deps[sk] = lv
            else:
                last[mysk] = val

    def _emit(self, eng, deps, fn, sk, inc):
        e = self.eobj[eng]
        seen = self.seen[eng]
        for wsk, wv in deps.items():
            if seen.get(wsk, 0) < wv:
                seen[wsk] = wv
                e.wait_ge(self.semobj[wsk], wv)
        fn(e).then_inc(self.semobj[sk], inc)
        self.ninst[eng] += 1

    def op(self, eng, fn, reads=(), writes=()):
        deps = self._deps(reads, writes)
        self._psum_deps(eng, list(reads) + list(writes), deps)
        self.cnt[eng] += 1
        sk = id(self.esem[eng])
        self._emit(eng, deps, fn, sk, 1)
        self._record(reads, writes, sk, self.cnt[eng])
        self._psum_deps(eng, list(reads) + list(writes), None, val=self.cnt[eng])

    def dma(self, eng, out, in_, fn=None, extra_reads=()):
        reads = [in_] + list(extra_reads)
        deps = self._deps(reads, [out])
        pool = self.dsem[eng]
        s = pool[self.dnext[eng] % len(pool)]
        self.dnext[eng] += 1
        sk = id(s)
        if self.dtot[sk] > 0 and deps.get(sk, 0) < self.dtot[sk]:
            deps[sk] = self.dtot[sk]
        self.dtot[sk] += 16
        if fn is None:
            oap, iap = out.ap, in_.ap
            fn = lambda e: e.dma_start(out=oap, in_=iap)
        self._emit(eng, deps, fn, sk, 16)
        self._record(reads, [out], sk, self.dtot[sk])

    def barrier(self):
        allev = {}
        for e in ("pe", "act", "dve", "pool"):
            if self.cnt[e] > 0:
                allev[id(self.esem[e])] = self.cnt[e]
        for sk, tot in self.dtot.items():
            if tot > 0:
                allev[sk] = tot
        for eng, e in self.eobj.items():
            seen = self.seen[eng]
            for wsk, wv in allev.items():
                if seen.get(wsk, 0) < wv:
                    seen[wsk] = wv
                    e.wait_ge(self.semobj[wsk], wv)

    def finish(self):
        self.barrier()
        while self.scopes:
            self.scopes.pop().close()
        self.es.close()

    def act(self, out, in_, func, scale=1.0, bias=None, accum=None, eng="act"):
        rd = [in_]
        wr = [out]
        kw = {}
        if isinstance(scale, View):
            rd.append(scale)
            kw["scale"] = scale.ap
        else:
            kw["scale"] = float(scale)
        if bias is not None:
            if isinstance(bias, View):
                rd.append(bias)
                kw["bias"] = bias.ap
            else:
                kw["bias"] = float(bias)
        if accum is not None:
            wr.append(accum)
            kw["accum_out"] = accum.ap
        oap, iap = out.ap, in_.ap
        self.op("act", lambda e: e.activation(out=oap, in_=iap, func=func, **kw), rd, wr)

    def ts(self, eng, out, in0, s1, s2=None, op0=ALU.mult, op1=None, accum=None):
        rd = [in0]
        wr = [out]
        a1 = s1
        if isinstance(s1, View):
            rd.append(s1)
            a1 = s1.ap
        a2 = s2
        if isinstance(s2, View):
            rd.append(s2)
            a2 = s2.ap
        kw = {}
        if op1 is not None:
            kw["op1"] = op1
        if accum is not None:
            kw["accum_out"] = accum.ap
            wr.append(accum)
        oap, iap = out.ap, in0.ap
        self.op(eng, lambda e: e.tensor_scalar(out=oap, in0=iap, scalar1=a1, scalar2=a2, op0=op0, **kw), rd, wr)

    def tt(self, eng, out, in0, in1, op):
        oap, a, b = out.ap, in0.ap, in1.ap
        self.op(eng, lambda e: e.tensor_tensor(out=oap, in0=a, in1=b, op=op), [in0, in1], [out])

    def stt(self, out, in0, scalar, in1, op0, op1, accum=None):
        rd = [in0, in1]
        wr = [out]
        sc = scalar
        if isinstance(scalar, View):
            rd.append(scalar)
            sc = scalar.ap
        kw = {}
        if accum is not None:
            kw["accum_out"] = accum.ap
            wr.append(accum)
        oap, a, b = out.ap, in0.ap, in1.ap
        self.op("dve", lambda e: e.scalar_tensor_tensor(out=oap, in0=a, scalar=sc, in1=b, op0=op0, op1=op1, **kw), rd, wr)

    def copy(self, eng, out, in_):
        oap, iap = out.ap, in_.ap
        if eng == "act":
            self.op("act", lambda e: e.activation(out=oap, in_=iap, func=AF.Copy), [in_], [out])
        else:
            self.op(eng, lambda e: e.tensor_copy(out=oap, in_=iap), [in_], [out])

    def memset(self, eng, out, val):
        oap = out.ap
        self.op(eng, lambda e: e.memset(oap, val), [], [out])

    def recip(self, out, in_):
        oap, iap = out.ap, in_.ap
        self.op("dve", lambda e: e.reciprocal(out=oap, in_=iap), [in_], [out])

    def mm(self, out, pairs, start=True, skip=False):
        rd = []
        for a, b in pairs:
            rd.append(a)
            rd.append(b)
        oap = out.ap
        aps = [(a.ap, b.ap) for a, b in pairs]
        n = len(aps)

        def fn(e):
            ins = None
            for i, (la, ra) in enumerate(aps):
                st = start and i == 0
                if skip:
                    ins = e.matmul(oap, lhsT=la, rhs=ra, start=st, stop=(i == n - 1), skip_group_check=True)
                else:
                    ins = e.matmul(oap, lhsT=la, rhs=ra, start=st, stop=(i == n - 1))
            return ins
        self.op("pe", fn, rd, [out])

    def transposes(self, items, ident, extra_writes=()):
        rd = [ident] + [b for _, b in items]
        wr = [a for a, _ in items]
        aps = [(a.ap, b.ap) for a, b in items]
        iap = ident.ap

        def fn(e):
            ins = None
            for oa, ia in aps:
                ins = e.transpose(out=oa, in_=ia, identity=iap)
            return ins
        self.op("pe", fn, rd, wr)


def bview(tt_dram, row_idx, lo, hi, reg_tt=None):
    v = tt_dram[row_idx + (slice(lo, hi),)] if isinstance(row_idx, tuple) else tt_dram[row_idx, lo:hi]
    return v.with_ap(v.ap.partition_broadcast(128))


def _rope_tables():
    f32 = np.float32
    tok = np.arange(L)
    rows = (tok // 64).astype(f32)
    cols = (tok % 64).astype(f32)
    fr = (np.float32(10000.0) ** (-np.arange(16, dtype=f32) / np.float32(16))).astype(f32)
    cs = np.zeros((2, 128, L), f32)
    for p in range(128):
        d = p % 64
        pos = rows if d < 32 else cols
        ang = (pos * fr[d % 16]).astype(f32)
        cs[0, p] = np.cos(ang)
        sg = -1.0 if (d % 32) < 16 else 1.0
        cs[1, p] = sg * np.sin(ang)
    fr8 = (np.float32(10000.0) ** (-np.arange(8, dtype=f32) / np.float32(8))).astype(f32)
    cm = np.zeros((2, 128, L), f32)
    for d in range(32):
        pos = rows if d < 16 else cols
        ang = (pos * fr8[d % 8]).astype(f32)
        cm[0, 64 + d] = np.cos(ang)
        sg = -1.0 if (d % 16) < 8 else 1.0
        cm[1, 64 + d] = sg * np.sin(ang)
    return cs, cm


def _na_tiles():
    pats = {}
    per_j = []
    for j in range(32):
        if 2 <= j <= 29:
            kts = list(range(j - 2, j + 3))
            ids = [kt - j + 2 for kt in kts]
        else:
            kts = [0, 1, 2, 3] if j < 2 else [28, 29, 30, 31]
            base = {0: 5, 1: 9, 30: 13, 31: 17}[j]
            ids = [base + i for i in range(4)]
        for kt, pid in zip(kts, ids):
            if pid not in pats:
                pats[pid] = (j, kt)
        per_j.append(list(zip(kts, ids)))
    return per_j, pats


def _na_bias(rpb):
    per_j, pats = _na_tiles()
    out = np.full((21, 128, 8, 128), NEG, np.float32)
    kl = np.arange(128)
    ql = np.arange(128)
    for pid, (j, kt) in pats.items():
        kr = 2 * kt + kl // 64
        kc = kl % 64
        qr = 2 * j + ql // 64
        qc = ql % 64
        rs = np.clip(qr - 4, 0, 56)
        cst = np.clip(qc - 8, 0, 48)
        inwin = ((kr[:, None] >= rs[None, :]) & (kr[:, None] < rs[None, :] + 8) &
                 (kc[:, None] >= cst[None, :]) & (kc[:, None] < cst[None, :] + 16))
        ro = np.clip(kr[:, None] - qr[None, :] + 7, 0, 14)
        co = np.clip(kc[:, None] - qc[None, :] + 15, 0, 30)
        for h in range(8):
            g = rpb[h][ro, co]
            out[pid, :, (h % 2) * 4 + h // 2, :] = np.where(inwin, g, np.float32(NEG))
    return out


def host_prep(inp):
    f32 = np.float32
    sh = {}
    cs, cm = _rope_tables()
    sh["rope_d"] = cs
    sh["rope_m"] = cm
    sh["ident"] = np.eye(128, dtype=f32)
    sh["na_bias"] = _na_bias(np.asarray(inp["na_rpb"][0]))
    pos = np.arange(128, dtype=f32)
    ramps = np.zeros((128, 4, 128), f32)
    ramps[:64, 0, :] = pos[None, :] + 1.0
    ramps[64:, 0, :] = 128.0 - pos[None, :]
    ramps[:, 1, :] = np.maximum(pos[None, :] - pos[:, None], 0.0)
    ramps[:, 2, :] = np.maximum(pos[:, None] - pos[None, :], 0.0)
    ramps[:, 3, 0] = 127.0 - pos
    ramps[:, 3, 1] = pos
    ramps[:, 3, 2] = 128.0
    sh["ramps"] = ramps
    sh["dummy_idx"] = np.ascontiguousarray(np.broadcast_to((LT + np.arange(128, dtype=np.int32))[:, None], (128, 16)))
    w = np.asarray(inp["even_w_in"][0])
    d = np.arange(64)
    sw = np.where((d % 32) < 16, d + 16, d - 16)
    idx = np.arange(512).reshape(4, 2, 64)[:, :, sw].reshape(-1)
    sh["w_in0"] = np.ascontiguousarray(np.concatenate([w, w[:, 0:512][:, idx], w[:, 512:1024][:, idx]], axis=1))
    sh["w_out0"] = np.asarray(inp["even_w_out"][0])
    w1 = np.asarray(inp["odd_w_in"][0])
    cq, ckv, kr = w1[:, 0:256], w1[:, 256:384], w1[:, 384:416]
    rq, rk, rv, rg = w1[:, 416:672], w1[:, 672:928], w1[:, 928:1440], w1[:, 1440:1952]
    d32 = np.arange(32)
    sw32 = np.where((d32 % 16) < 8, d32 + 8, d32 - 8)
    z64 = np.zeros((D, 64), f32)
    z32 = np.zeros((D, 32), f32)
    kr_pad = np.concatenate([z64, kr, z32], axis=1)
    kr_sw_pad = np.concatenate([z64, kr[:, sw32], z32], axis=1)
    rq_dup = np.concatenate([np.concatenate([rq[:, h * 64:(h + 1) * 64]] * 2, axis=1) for h in range(4)], axis=1)
    rk_dup = np.concatenate([np.concatenate([rk[:, h * 64:(h + 1) * 64]] * 2, axis=1) for h in range(4)], axis=1)
    sh["w_in1"] = np.ascontiguousarray(np.concatenate([cq, ckv, rk, rv, rg, kr_pad, kr_sw_pad, rq_dup, rk_dup], axis=1))
    sh["w_out1"] = np.asarray(inp["odd_w_out"][0])
    wuq = np.asarray(inp["mla_w_uq"][0])
    ci = np.arange(768).reshape(8, 96).copy()
    ci[:, 64:] = ci[:, 64:][:, sw32]
    sh["w_uq"] = np.ascontiguousarray(np.concatenate([wuq, wuq[:, ci.reshape(-1)]], axis=1))
    wukv = np.asarray(inp["mla_w_ukv"][0]).reshape(128, 8, 128)
    sh["w_ukv"] = np.ascontiguousarray(np.concatenate([wukv[:, :, :64].reshape(128, 512), wukv[:, :, 64:].reshape(128, 512)], axis=1))
    for k in ("ada_w", "ada_b", "norm_mix", "norm_ffn", "final_norm", "diff_lambda", "diff_subln", "mla_q_norm",
              "mla_kv_norm", "ret_decay_logit", "moe_router", "moe_w1", "moe_w3", "moe_w2"):
        sh[k] = np.ascontiguousarray(np.asarray(inp[k], dtype=f32))
    sh["final_norm"] = sh["final_norm"].reshape(1, D)
    sh["diff_lambda"] = sh["diff_lambda"].reshape(1, 256)
    sh["ret_decay_logit"] = sh["ret_decay_logit"].reshape(1, 8)
    per = []
    c_ctx = np.asarray(inp["c_ctx"], f32)
    for b in range(inp["x"].shape[0]):
        cc = np.stack([np.asarray(inp["c"][b], f32).reshape(8, 128).T, c_ctx.reshape(8, 128).T], axis=-1)
        xa = np.concatenate([np.asarray(inp["x"][b], f32), np.asarray(inp["ctx"][b], f32)], axis=0)
        per.append({"xin": np.ascontiguousarray(xa), "cc": np.ascontiguousarray(cc)})
    return sh, per


def build(stop_after=None, debug=False, only=None):
    nc = bass.Bass("TRN2", target_bir_lowering=False)
    P = Prog(nc)
    EXT = "ExternalOutput" if debug else None

    def din(name, shape, dt=F32):
        return P.dram(name, shape, dt, kind="ExternalInput")

    xin = din("xin", [LT, D])
    cc = din("cc", [128, 8, 2])
    rope_d = din("rope_d", [2, 128, L])
    rope_m = din("rope_m", [2, 128, L])
    ident_d = din("ident", [128, 128])
    na_bias = din("na_bias", [21, 128, 8, 128])
    ramps_d = din("ramps", [128, 4, 128])
    dummy_idx = din("dummy_idx", [128, 16], I32)
    w_in0 = din("w_in0", [D, 4096])
    w_out0 = din("w_out0", [D, D])
    w_in1 = din("w_in1", [D, 2944])
    w_out1 = din("w_out1", [D, D])
    w_uq = din("w_uq", [256, 1536])
    w_ukv = din("w_ukv", [128, 1024])
    ada_w = din("ada_w", [2, D, 6 * D])
    ada_b = din("ada_b", [2, 6 * D])
    norm_mix = din("norm_mix", [2, D])
    norm_ffn = din("norm_ffn", [2, D])
    final_norm = din("final_norm", [1, D])
    diff_lambda = din("diff_lambda", [1, 256])
    diff_subln = din("diff_subln", [1, 128])
    mla_q_norm = din("mla_q_norm", [1, 256])
    mla_kv_norm = din("mla_kv_norm", [1, 128])
    ret_decay = din("ret_decay_logit", [1, 8])
    moe_router = din("moe_router", [2, D, 16])
    moe_w = [din("moe_w1", [2, 16, D, D]), din("moe_w3", [2, 16, D, D]), din("moe_w2", [2, 16, D, D])]
    out_d = P.dram("out", [L, D], F32, kind="ExternalOutput")

    mods_d = P.dram("mods_d", [2, 2, 6 * D], F32, kind=EXT)
    x1_d = P.dram("x1_d", [LT + 128, D], F32, kind=EXT)
    h2_d = P.dram("h2_d", [LT + 128, D], BF16, kind=EXT)
    ao_d = P.dram("ao_d", [LT, D], BF16, kind=EXT)
    qTd = P.dram("qTd", [4, 128, LT], BF16, kind=EXT)
    kTd = P.dram("kTd", [4, 128, LT], BF16, kind=EXT)
    qTn = P.dram("qTn", [4, 128, LT], BF16, kind=EXT)
    kTn = P.dram("kTn", [4, 128, LT], BF16, kind=EXT)
    vd = P.dram("vd", [LT, 512], BF16, kind=EXT)
    vn = P.dram("vn", [LT, 512], BF16, kind=EXT)
    cqn_d = P.dram("cqn_d", [3, 128, LT], BF16, kind=EXT)
    krT_d = P.dram("krT_d", [128, LT], BF16, kind=EXT)
    rqT_d = P.dram("rqT_d", [4, 128, LT], BF16, kind=EXT)
    rkT_d = P.dram("rkT_d", [4, 128, LT], BF16, kind=EXT)
    rk_d = P.dram("rk_d", [LT, 256], BF16, kind=EXT)
    rv_d = P.dram("rv_d", [LT, 512], BF16, kind=EXT)
    sg_d = P.dram("sg_d", [LT, 512], BF16, kind=EXT)
    dbg_aff = P.dram("dbg_aff", [16, LT], F32, kind=EXT) if debug else None
    dbg_idx = P.dram("dbg_idx", [128, 5, 16], I32, kind=EXT) if debug else None

    identf = P.sbuf("identf", [128, 128], F32)
    identb = P.sbuf("identb", [128, 128], BF16)
    gateT = P.sbuf("gateT", [128, 5, 16], F32)
    idxT = P.sbuf("idxT", [128, 5, 16], I32)
    P.dma("sp", identf.all(), ident_d.all())
    P.copy("dve", identb.all(), identf.all())
    eps_t = P.sbuf("eps_t", [128, 1], F32)
    P.memset("dve", eps_t.all(), 1e-6)

    MOD = {"sh1": 0, "sc1": 1, "g1": 2, "sh2": 3, "sc2": 4, "g2": 5}

    def modrow(l, who, name):
        k = MOD[name]
        v = mods_d[l, who, k * D:(k + 1) * D]
        return v.with_ap(v.ap.partition_broadcast(128))

    def brow(tt_d, idx, n):
        v = tt_d[idx, 0:n]
        return v.with_ap(v.ap.partition_broadcast(128))

    def stop(name):
        return stop_after == name

    P.push()
    zt = P.sbuf("zt", [128, D], F32)
    ztb = P.sbuf("ztb", [128, D], BF16)
    P.memset("dve", zt.all(), 0.0)
    P.memset("pool", ztb.all(), 0.0)
    P.dma("sp", x1_d[LT:LT + 128, :], zt.all())
    P.dma("sp", h2_d[LT:LT + 128, :], ztb.all())
    P.pop()
    P.push()
    ccs = P.sbuf("ccs", [128, 8, 2], F32)
    scs = P.sbuf("scs", [128, 8, 2], F32)
    P.dma("sp", ccs.all(), cc.all())
    P.act(scs.all(), ccs.all(), AF.Silu)
    adw = [P.sbuf("adw%d" % i, [128, 8, 512], F32) for i in range(2)]
    modsb = P.sbuf("modsb", [2, 6 * D], F32)
    adb = P.sbuf("adb", [2, 6 * D], F32)
    psm = [P.psum("psm%d" % i, [128, 512], F32) for i in range(2)]
    for l in range(2):
        P.dma("sp", adb[0:1, :], ada_b[l:l + 1, :])
        P.dma("sp", adb[1:2, :], ada_b[l:l + 1, :])
        for nb in range(12):
            w = adw[nb % 2]
            src = ada_w[l, :, nb * 512:(nb + 1) * 512]
            P.dma("sp" if nb % 2 == 0 else "act", w.all(), src.with_ap(src.ap.rearrange("(c p) n -> p c n", p=128)))
            P.mm(psm[nb % 2][0:2, :], [(scs[:, c, :], w[:, c, :]) for c in range(8)])
            P.tt("dve", modsb[:, nb * 512:(nb + 1) * 512], psm[nb % 2][0:2, :], adb[:, nb * 512:(nb + 1) * 512], ALU.add)
        P.dma("sp", mods_d[l], modsb.all())
    P.pop()
    if stop("mods"):
        P.finish()
        return nc

    def make_sb(l, who, which, s_b, sh_b, t1, t2):
        nrm = norm_mix if which == 1 else norm_ffn
        P.dma("sp", t1.all(), modrow(l, who, "sc%d" % which))
        P.dma("sp", t2.all(), brow(nrm, l, D))
        P.stt(s_b.all(), t1.all(), 1.0, t2.all(), ALU.add, ALU.mult)
        P.dma("sp", sh_b.all(), modrow(l, who, "sh%d" % which))

    def rstd_of(ss, rstd, n):
        P.act(rstd, ss, AF.Sqrt, scale=1.0 / n, bias=eps_t[:, 0:1])
        P.recip(rstd, rstd)


    def cast_rot(i):
        return ("dve", "pool", "act")[i % 3]

    def phase_A0():
        P.push()
        wb = P.sbuf("w0b", [128, 8, 4096], BF16)
        wst = [P.sbuf("w0st%d" % i, [128, 8, 256], F32) for i in range(2)]
        for nb in range(16):
            src = w_in0[:, nb * 256:(nb + 1) * 256]
            P.dma("sp" if nb % 2 == 0 else "act", wst[nb % 2].all(), src.with_ap(src.ap.rearrange("(c p) n -> p c n", p=128)))
            P.copy(cast_rot(nb), wb[:, :, nb * 256:(nb + 1) * 256], wst[nb % 2].all())
        sb = [P.sbuf("sb%d" % i, [128, D], F32) for i in range(2)]
        shb = [P.sbuf("shb%d" % i, [128, D], F32) for i in range(2)]
        xt = [P.sbuf("xt%d" % i, [128, D], F32) for i in range(2)]
        tmp32 = P.sbuf("tmp32", [128, D], F32)
        junk = P.sbuf("junk", [128, D], BF16)
        make_sb(0, 0, 1, sb[0], shb[0], xt[0], xt[1])
        make_sb(0, 1, 1, sb[1], shb[1], xt[0], xt[1])
        hb = [P.sbuf("hb%d" % i, [128, D], BF16) for i in range(2)]
        hTs = [P.sbuf("hT%d" % i, [128, 8, 512], BF16) for i in range(2)]
        cst = P.sbuf("cst", [128, 2, 512], F32)
        fst = P.sbuf("fst", [128, 16, 512], BF16)
        vst = P.sbuf("vst", [128, 4, 1024], BF16)
        t1 = P.sbuf("rt1", [128, 512], F32)
        t2 = P.sbuf("rt2", [128, 512], F32)
        ss = P.sbuf("ss", [128, 1], F32)
        rstd = P.sbuf("rstd", [128, 1], F32)
        pT = [P.psum("pT%d" % i, [128, 1024], BF16) for i in range(2)]
        pa = [P.psum("pa%d" % i, [128, 512], F32) for i in range(2)]
        pb = [P.psum("pb%d" % i, [128, 512], F32) for i in range(2)]
        pv = [P.psum("pv%d" % i, [128, 512], F32) for i in range(2)]
        def S1(blk):
            ntile = 4 if blk < 8 else 2
            who = 0 if blk < 8 else 1
            hT = hTs[blk % 2]
            for ti in range(ntile):
                t = blk * 4 + ti
                x = xt[ti % 2]
                P.dma("sp", x.all(), xin[t * 128:(t + 1) * 128, :])
                P.act(junk.all(), x.all(), AF.Square, accum=ss.all())
                rstd_of(ss.all(), rstd.all(), D)
                P.stt(tmp32.all(), x.all(), rstd.all(), sb[who].all(), ALU.mult, ALU.mult)
                P.tt("pool", hb[ti % 2].all(), tmp32.all(), shb[who].all(), ALU.add)
                P.transposes([(pT[ti % 2][:, c * 128:(c + 1) * 128], hb[ti % 2][:, c * 128:(c + 1) * 128]) for c in range(8)], identb.all())
                pv_ = pT[ti % 2].all()
                P.copy("act", hT[:, :, ti * 128:(ti + 1) * 128], pv_.with_ap(pv_.ap.rearrange("p (c t) -> p c t", c=8)))

        def S2(blk):
            ntile = 4 if blk < 8 else 2
            ntok = ntile * 128
            tok0 = blk * 512
            hT = hTs[blk % 2]
            if blk < 8:
                P.dma("sp", cst[:, 0, :], rope_d[0, :, tok0:tok0 + 512])
                P.dma("sp", cst[:, 1, :], rope_d[1, :, tok0:tok0 + 512])
            for g in range(16):
                col0 = [0, 512, 1536, 2048][g // 4] + (g % 4) * 128
                ps = pa[g % 2]
                P.mm(ps[:, 0:ntok], [(wb[:, c, col0:col0 + 128], hT[:, c, 0:ntok]) for c in range(8)])
                if g < 8 and blk < 8:
                    sc0 = 3072 + (g // 4) * 512 + (g % 4) * 128
                    ps2 = pb[g % 2]
                    P.mm(ps2[:, 0:ntok], [(wb[:, c, sc0:sc0 + 128], hT[:, c, 0:ntok]) for c in range(8)])
                    P.tt("dve", t1[:, 0:ntok], ps[:, 0:ntok], cst[:, 0, 0:ntok], ALU.mult)
                    P.tt("dve", t2[:, 0:ntok], ps2[:, 0:ntok], cst[:, 1, 0:ntok], ALU.mult)
                    P.tt("pool", fst[:, g, 0:ntok], t1[:, 0:ntok], t2[:, 0:ntok], ALU.add)
                else:
                    P.copy("act", fst[:, g, 0:ntok], ps[:, 0:ntok])
            for gi, dst in enumerate((qTd, kTd, qTn, kTn)):
                dv = dst[:, :, tok0:tok0 + ntok]
                P.dma("pool", dv.with_ap(dv.ap.rearrange("h p t -> p h t")), fst[:, gi * 4:(gi + 1) * 4, 0:ntok])
            for ti in range(ntile):
                for vi, c0 in enumerate((1024, 2560)):
                    psv = pv[vi]
                    P.mm(psv.all(), [(hT[:, c, ti * 128:(ti + 1) * 128], wb[:, c, c0:c0 + 512]) for c in range(8)])
                    P.copy("dve" if vi == 0 else "act", vst[:, ti, vi * 512:(vi + 1) * 512], psv.all())
            for vi, dst in enumerate((vd, vn)):
                dv = dst[tok0:tok0 + ntok, :]
                P.dma("pool", dv.with_ap(dv.ap.rearrange("(t p) c -> p t c", p=128)), vst[:, 0:ntile, vi * 512:(vi + 1) * 512])

        S1(0)
        for blk in range(9):
            if blk + 1 < 9:
                S1(blk + 1)
            S2(blk)
        P.pop()

    def phase_B0():
        lam_init = 0.2
        P.push()
        QT = [P.sbuf("QTd%d" % i, [128, LT], BF16) for i in range(2)]
        KT = [P.sbuf("KTd%d" % i, [128, LT], BF16) for i in range(2)]
        V = [P.sbuf("Vd%d" % i, [128, NT, 130], BF16) for i in range(2)]
        for i in range(2):
            P.memset("dve", V[i][:, :, 128:130], 1.0)
        pt = [P.sbuf("pt%d" % i, [128, 2, 512], BF16) for i in range(3)]
        dl = P.sbuf("dl", [128, 256], F32)
        dtmp = P.sbuf("dtmp", [128, 128], F32)
        sm = P.sbuf("lsm", [128, 2], F32)
        lam = P.sbuf("lam", [128, 1], F32)
        nlam = P.sbuf("nlam", [128, 1], F32)
        gsub = P.sbuf("gsub", [128, 128], F32)
        P.dma("sp", dl.all(), brow(diff_lambda, 0, 256))
        P.tt("dve", dtmp[:, 0:64], dl[:, 0:64], dl[:, 64:128], ALU.mult)
        P.tt("dve", dtmp[:, 64:128], dl[:, 128:192], dl[:, 192:256], ALU.mult)
        P.act(dtmp[:, 0:64], dtmp[:, 0:64], AF.Identity, accum=sm[:, 0:1])
        P.act(dtmp[:, 64:128], dtmp[:, 64:128], AF.Identity, accum=sm[:, 1:2])
        P.act(sm.all(), sm.all(), AF.Exp)
        P.tt("dve", lam.all(), sm[:, 0:1], sm[:, 1:2], ALU.subtract)
        P.ts("dve", nlam.all(), lam.all(), lam_init, -1.0, ALU.add, ALU.mult)
        P.dma("sp", gsub.all(), brow(diff_subln, 0, 128))
        P.ts("dve", gsub.all(), gsub.all(), 1.0 - lam_init, None, ALU.mult)
        osb = [P.sbuf("osb%d" % i, [128, 4, 2, 129], F32) for i in range(2)]
        rl = P.sbuf("rl", [128, 4, 2, 1], F32)
        aall = P.sbuf("aall", [128, 4, 128], F32)
        atmp = P.sbuf("atmp", [128, 4, 128], F32)
        junk = P.sbuf("junkd", [128, 128], F32)
        ss = P.sbuf("ssd", [128, 4], F32)
        rs = P.sbuf("rsd", [128, 4], F32)
        aost = [P.sbuf("aost%d" % i, [128, 4, 128], BF16) for i in range(2)]
        psS = [P.psum("psS%d" % b, [128, 1024], F32) for b in range(2)]
        psO = [P.psum("psO%d" % i, [128, 512], F32) for i in range(3)]
        it = 0
        for h in range(4):
            b = h % 2
            P.dma("sp", QT[b].all(), qTd[h])
            P.dma("sp", KT[b].all(), kTd[h])
            sv = vd[:, h * 128:(h + 1) * 128]
            P.dma("sp", V[b][:, :, 0:128], sv.with_ap(sv.ap.rearrange("(t p) c -> p t c", p=128)))
            for qb in range(9):
                nq = 4 if qb < 8 else 2
                nqt = nq * 128
                q0 = qb * 512
                kts = list(range(NT)) if qb < 8 else [32, 33]
                first = [True, True, True]

                def qk_ex(ki):
                    kt = kts[ki]
                    s_ = psS[ki % 2]
                    for m in range(2):
                        P.mm(s_[:, m * 512:m * 512 + nqt], [(KT[b][m * 64:(m + 1) * 64, kt * 128:(kt + 1) * 128],
                                                             QT[b][m * 64:(m + 1) * 64, q0:q0 + nqt])])
                    p = pt[ki % 3]
                    sv_ = s_.all()
                    sv3 = sv_.with_ap(sv_.ap.rearrange("p (m q) -> p m q", m=2)[:, :, 0:nqt])
                    P.act(p[:, :, 0:nqt], sv3, AF.Exp, scale=0.125)

                def pv(ki):
                    kt = kts[ki]
                    p = pt[ki % 3]
                    for qi in range(nq):
                        for m in range(2):
                            a = qi * 2 + m
                            bank, off = a // 3, (a % 3) * 129
                            P.mm(psO[bank][:, off:off + 129], [(p[:, m, qi * 128:(qi + 1) * 128], V[b][:, kt, 0:129])],
                                 start=first[bank], skip=True)
                            first[bank] = False

                qk_ex(0)
                for ki in range(len(kts)):
                    if ki + 1 < len(kts):
                        qk_ex(ki + 1)
                    pv(ki)
                ao = aost[it % 2]
                ob_ = osb[it % 2]
                it += 1
                na = nq * 2
                for bank in range((na + 2) // 3):
                    n_in = min(3, na - bank * 3)
                    ov = ob_.all()
                    o2 = ov.with_ap(ov.ap.rearrange("p a m c -> p (a m c)")[:, bank * 387:bank * 387 + n_in * 129])
                    P.copy("dve", o2, psO[bank][:, 0:n_in * 129])
                P.recip(rl[:, 0:nq], ob_[:, 0:nq, :, 128:129])
                P.ts("dve", rl[:, 0:nq, 1, :], rl[:, 0:nq, 1, :], nlam.all(), None, ALU.mult)
                r0 = rl[:, 0:nq, 0, :]
                r1_ = rl[:, 0:nq, 1, :]
                P.tt("dve", aall[:, 0:nq, :], ob_[:, 0:nq, 0, 0:128], r0.with_ap(r0.ap.to_broadcast([128, nq, 128])), ALU.mult)
                P.tt("pool", atmp[:, 0:nq, :], ob_[:, 0:nq, 1, 0:128], r1_.with_ap(r1_.ap.to_broadcast([128, nq, 128])), ALU.mult)
                P.tt("dve", aall[:, 0:nq, :], aall[:, 0:nq, :], atmp[:, 0:nq, :], ALU.add)
                for qi in range(nq):
                    P.stt(junk.all(), aall[:, qi, :], 1.0, aall[:, qi, :], ALU.mult, ALU.mult, accum=ss[:, qi:qi + 1])
                P.act(rs[:, 0:nq], ss[:, 0:nq], AF.Sqrt, scale=1.0 / 128, bias=eps_t[:, 0:1])
                P.recip(rs[:, 0:nq], rs[:, 0:nq])
                for qi in range(nq):
                    P.stt(ao[:, qi, :], aall[:, qi, :], rs[:, qi:qi + 1], gsub.all(), ALU.mult, ALU.mult)
                dv = ao_d[q0:q0 + nqt, h * 128:(h + 1) * 128]
                P.dma("pool", dv.with_ap(dv.ap.rearrange("(t p) c -> p t c", p=128)), ao[:, 0:nq, :])
        P.pop()

    def phase_C0():
        P.push()
        QTn_ = P.sbuf("QTn", [128, 4, LT], BF16)
        KTn_ = P.sbuf("KTn", [128, 4, LT], BF16)
        Vn_ = P.sbuf("Vn", [128, NT, 8, 66], BF16)
        P.memset("dve", Vn_[:, :, :, 64:66], 1.0)
        for hp in range(4):
            P.dma("sp", QTn_[:, hp, :], qTn[hp])
            P.dma("act", KTn_[:, hp, :], kTn[hp])
        for h in range(8):
            sv = vn[:, h * 64:(h + 1) * 64]
            P.dma("sp" if h % 2 == 0 else "act", Vn_[:, :, h, 0:64], sv.with_ap(sv.ap.rearrange("(t p) c -> p t c", p=128)))
        breg = P.sbuf("nab", [128, 5, 8, 128], F32)
        bedge = P.sbuf("nae", [128, 4, 8, 128], F32)
        for i in range(5):
            P.dma("sp", breg[:, i, :, :], na_bias[i])
        ssb = [P.sbuf("nas%d" % i, [128, 8, 128], F32) for i in range(2)]
        ptn = [P.sbuf("napt%d" % i, [128, 8, 128], BF16) for i in range(3)]
        onst = [P.sbuf("naost%d" % i, [128, 512], BF16) for i in range(2)]
        rr = P.sbuf("narr", [128, 8], F32)
        psS = [[P.psum("nS%d%d" % (b, k), [128, 512], F32) for k in range(2)] for b in range(2)]
        psO = [[P.psum("nO%d%d" % (b, k), [128, 512], F32) for k in range(2)] for b in range(2)]
        per_j, _ = _na_tiles()
        _js = [int(v) for v in os.environ.get('C0_J', '').split(',') if v and v != 'none'] or list(range(NT))
        if os.environ.get('C0_J') == 'none':
            _js = []
        _nopv = os.environ.get('C0_NOPV') == '1'
        _noexp = os.environ.get('C0_NOEXP') == '1'
        for j in _js:
            if j < 32:
                tiles = per_j[j] + [(32, None), (33, None)]
            else:
                tiles = [(32, None), (33, None)]
            base = {0: 5, 1: 9, 30: 13, 31: 17}.get(j)
            if base is not None:
                for i in range(4):
                    P.dma("sp", bedge[:, i, :, :], na_bias[base + i])
            first = [True, True]
            ob = psO[j % 2]
            def qk_ex(ki):
                kt, pid = tiles[ki]
                s_ = psS[ki % 2]
                for h in range(8):
                    hp, hh = h // 2, h % 2
                    P.mm(s_[hh][:, hp * 128:(hp + 1) * 128],
                         [(KTn_[hh * 64:(hh + 1) * 64, hp, kt * 128:(kt + 1) * 128], QTn_[hh * 64:(hh + 1) * 64, hp, j * 128:(j + 1) * 128])])
                p = ptn[ki % 3]
                sbuf_s = ssb[ki % 2]
                for half in range(2):
                    pv_ = s_[half].all()
                    pv3 = pv_.with_ap(pv_.ap.rearrange("p (h q) -> p h q", h=4))
                    hs = slice(half * 4, half * 4 + 4)
                    if pid is not None:
                        bt = breg[:, pid, hs, :] if pid < 5 else bedge[:, pid - base, hs, :]
                        P.stt(sbuf_s[:, hs, :], pv3, 0.125, bt, ALU.mult, ALU.add)
                        P.act(p[:, hs, :], sbuf_s[:, hs, :], AF.Exp)
                    else:
                        P.act(p[:, hs, :], pv3, AF.Exp, scale=0.125)

            def pv(ki):
                kt, pid = tiles[ki]
                p = ptn[ki % 3]
                for h in range(8):
                    bk, off = h // 4, (h % 4) * 65
                    P.mm(ob[bk][:, off:off + 65], [(p[:, (h % 2) * 4 + h // 2, :], Vn_[:, kt, h, 0:65])], start=first[bk], skip=True)
                    first[bk] = False

            qk_ex(0)
            for ki in range(len(tiles)):
                if ki + 1 < len(tiles):
                    qk_ex(ki + 1)
                pv(ki)
            if _nopv:
                continue
            for h in range(8):
                bk, off = h // 4, (h % 4) * 65
                P.recip(rr[:, h:h + 1], ob[bk][:, off + 64:off + 65])
                P.ts("dve", onst[j % 2][:, h * 64:(h + 1) * 64], ob[bk][:, off:off + 64], rr[:, h:h + 1], None, ALU.mult)
            P.dma("pool", ao_d[j * 128:(j + 1) * 128, 512:1024], onst[j % 2].all())
        P.pop()

    def phase_D(l, ntiles, affT):
        w_out = w_out0 if l == 0 else w_out1
        xsrc = xin if l == 0 else x1_d
        P.push()
        wob = P.sbuf("wob", [128, 8, D], BF16)
        wst = [P.sbuf("wost%d" % i, [128, 8, 256], F32) for i in range(2)]
        for nb in range(4):
            src = w_out[:, nb * 256:(nb + 1) * 256]
            P.dma("sp" if nb % 2 == 0 else "act", wst[nb % 2].all(), src.with_ap(src.ap.rearrange("(c p) n -> p c n", p=128)))
            P.copy(cast_rot(nb), wob[:, :, nb * 256:(nb + 1) * 256], wst[nb % 2].all())
        nwho = 2 if l == 0 else 1
        xt = [P.sbuf("dxt%d" % i, [128, D], F32) for i in range(2)]
        tmp32 = P.sbuf("dtmp32", [128, D], F32)
        g1b = [P.sbuf("g1b%d" % i, [128, D], F32) for i in range(nwho)]
        s2b = [P.sbuf("s2b%d" % i, [128, D], F32) for i in range(nwho)]
        sh2b = [P.sbuf("sh2b%d" % i, [128, D], F32) for i in range(nwho)]
        for who in range(nwho):
            P.dma("sp", g1b[who].all(), modrow(l, who, "g1"))
            make_sb(l, who, 2, s2b[who], sh2b[who], xt[0], xt[1])
        wr = P.sbuf("wr", [128, 8, 16], F32)
        srcr = moe_router[l]
        P.dma("sp", wr.all(), srcr.with_ap(srcr.ap.rearrange("(c p) e -> p c e", p=128)))
        aot = [P.sbuf("aot%d" % i, [128, D], BF16) for i in range(2)]
        aoT = [P.sbuf("aoT%d" % i, [128, 8, 128], BF16) for i in range(2)]
        xn = [P.sbuf("dxn%d" % i, [128, D], F32) for i in range(2)]
        h2 = [P.sbuf("dh2%d" % i, [128, D], F32) for i in range(2)]
        h2b = [P.sbuf("dh2b%d" % i, [128, D], BF16) for i in range(2)]
        h2T = P.sbuf("dh2T", [128, 8, 128], F32)
        junk = P.sbuf("djunk", [128, D], BF16)
        ss = P.sbuf("dss", [128, 1], F32)
        rstd = P.sbuf("drstd", [128, 1], F32)
        mx = P.sbuf("dmx", [128, 1], F32)
        sme = P.sbuf("dsme", [128, 1], F32)
        ex = P.sbuf("dex", [128, 16], F32)
        aff = P.sbuf("daff", [128, 16], F32)
        pT = P.psum("dpT", [128, 1024], BF16)
        pY = [P.psum("dpY%d" % i, [128, 512], F32) for i in range(2)]
        pH = [P.psum("dpH%d" % i, [128, 512], F32) for i in range(2)]
        pR = P.psum("dpR", [128, 512], F32)
        pR2 = P.psum("dpR2", [128, 512], F32)
        def S1(t):
            who = 0 if t < 32 else 1
            k = t % 2
            P.dma("sp", aot[k].all(), ao_d[t * 128:(t + 1) * 128, :])
            P.dma("act", xt[k].all(), xsrc[t * 128:(t + 1) * 128, :])
            P.transposes([(pT[:, c * 128:(c + 1) * 128], aot[k][:, c * 128:(c + 1) * 128]) for c in range(8)], identb.all())
            pv_ = pT.all()
            P.copy("act", aoT[k].all(), pv_.with_ap(pv_.ap.rearrange("p (c t) -> p c t", c=8)))
            for half in range(2):
                hs = slice(half * 512, (half + 1) * 512)
                P.mm(pY[half].all(), [(aoT[k][:, c, :], wob[:, c, hs]) for c in range(8)])
                P.tt("dve", tmp32[:, hs], pY[half].all(), g1b[who][:, hs], ALU.mult)
            P.tt("pool", xn[k].all(), xt[k].all(), tmp32.all(), ALU.add)
            P.dma("pool", x1_d[t * 128:(t + 1) * 128, :], xn[k].all())
            P.act(junk.all(), xn[k].all(), AF.Square, accum=ss.all())
            rstd_of(ss.all(), rstd.all(), D)
            P.stt(tmp32.all(), xn[k].all(), rstd.all(), s2b[who].all(), ALU.mult, ALU.mult)
            P.tt("pool", h2[k].all(), tmp32.all(), sh2b[who].all(), ALU.add)
            P.copy("act", h2b[k].all(), h2[k].all())
            P.dma("pool", h2_d[t * 128:(t + 1) * 128, :], h2b[k].all())

        def S2(t):
            k = t % 2
            P.transposes([(pH[c // 4][:, (c % 4) * 128:(c % 4 + 1) * 128], h2[k][:, c * 128:(c + 1) * 128]) for c in range(8)], identf.all())
            for hh in range(2):
                pv_ = pH[hh].all()
                P.copy("dve" if hh == 0 else "act", h2T[:, hh * 4:(hh + 1) * 4, :], pv_.with_ap(pv_.ap.rearrange("p (c t) -> p c t", c=4)))
            P.mm(pR[:, 0:16], [(h2T[:, c, :], wr[:, c, :]) for c in range(8)])
            P.op("dve", lambda e: e.reduce_max(out=mx.all().ap, in_=pR[:, 0:16].ap, axis=mybir.AxisListType.X), [pR[:, 0:16]], [mx.all()])
            P.ts("dve", mx.all(), mx.all(), -1.0, None, ALU.mult)
            P.act(ex.all(), pR[:, 0:16], AF.Exp, bias=mx.all(), accum=sme.all())
            P.recip(sme.all(), sme.all())
            P.ts("dve", aff.all(), ex.all(), sme.all(), None, ALU.mult)
            P.transposes([(pR2[0:16, 0:128], aff.all())], identf.all())
            P.copy("dve", affT[:, t * 128:(t + 1) * 128], pR2[0:16, 0:128])

        S1(0)
        for t in range(ntiles):
            if t + 1 < ntiles:
                S1(t + 1)
            S2(t)
        if debug:
            P.dma("sp", dbg_aff[:, 0:ntiles * 128], affT[:, 0:ntiles * 128])
        P.pop()

    def phase_topk(l, affT):
        P.push()
        work = P.sbuf("tkw", [16, L], F32)
        vals = P.sbuf("tkv", [16, 512], F32)
        idxu = P.sbuf("tki", [16, 512], U32)
        idxf = P.sbuf("tkf", [16, 512], F32)
        pX = [P.psum("tkp%d" % i, [128, 512], F32) for i in range(2)]
        tf = P.sbuf("tktf", [128, 16], F32)

        def topk(src0, wk, v, iu, rounds):
            src = src0
            for r in range(rounds):
                v8 = v[:, r * 8:(r + 1) * 8]
                i8 = iu[:, r * 8:(r + 1) * 8]
                P.op("dve", lambda e, s=src, a=v8: e.max(out=a.ap, in_=s.ap), [src], [v8])
                P.op("dve", lambda e, s=src, a=v8, b=i8: e.max_index(out=b.ap, in_max=a.ap, in_values=s.ap), [src, v8], [i8])
                if r < rounds - 1:
                    P.op("dve", lambda e, s=src, a=v8, w=wk: e.match_replace(out=w.ap, in_to_replace=a.ap, in_values=s.ap, imm_value=-1.0),
                         [src, v8], [wk])
                    src = wk
        topk(affT[:, 0:L], work.all(), vals, idxu, 64)
        P.copy("dve", idxf.all(), idxu.all())
        for st in range(4):
            P.transposes([(pX[0][:, 0:16], vals[:, st * 128:(st + 1) * 128])], identf[0:16, 0:16])
            P.copy("dve", gateT[:, st, :], pX[0][:, 0:16])
            P.transposes([(pX[1][:, 0:16], idxf[:, st * 128:(st + 1) * 128])], identf[0:16, 0:16])
            P.copy("dve", tf.all(), pX[1][:, 0:16])
            P.copy("dve", idxT[:, st, :], tf.all())
        if l == 0:
            workc = P.sbuf("tkwc", [16, LC], F32)
            valsc = P.sbuf("tkvc", [16, 32], F32)
            idxcu = P.sbuf("tkic", [16, 32], U32)
            idxcf = P.sbuf("tkfc", [16, 32], F32)
            topk(affT[:, L:LT], workc.all(), valsc, idxcu, 4)
            P.copy("dve", idxcf.all(), idxcu.all())
            P.ts("dve", idxcf.all(), idxcf.all(), float(L), None, ALU.add)
            P.memset("dve", gateT[:, 4, :], 0.0)
            P.dma("sp", idxT[:, 4, :], dummy_idx.all())
            P.transposes([(pX[0][0:32, 0:16], valsc.all())], identf[0:16, 0:16])
            P.copy("dve", gateT[0:32, 4, :], pX[0][0:32, 0:16])
            P.transposes([(pX[1][0:32, 0:16], idxcf.all())], identf[0:16, 0:16])
            P.copy("dve", tf[0:32, :], pX[1][0:32, 0:16])
            P.copy("dve", idxT[0:32, 4, :], tf[0:32, :])
        if debug:
            P.dma("sp", dbg_idx.all(), idxT.all())
        P.pop()

    def phase_experts(l, NS):
        P.push()
        wbf = [[P.sbuf("ew%d_%d" % (wi, i), [128, 8, D], BF16) for i in range(2)] for wi in range(3)]
        wst = [P.sbuf("est%d" % i, [128, 2, 1024], F32) for i in range(3)]
        xes = [P.sbuf("xe%d" % i, [128, NS, D], BF16) for i in range(2)]
        xeT = P.sbuf("xeT", [128, 8, NS * 128], BF16)
        hidT = P.sbuf("hidT", [128, 8, NS * 128], BF16)
        s1 = P.sbuf("es1", [128, NS * 128], F32)
        yst = [P.sbuf("yst%d" % i, [128, D], F32) for i in range(3)]
        nwho = 2 if NS == 5 else 1
        g2b = [P.sbuf("g2b%d" % i, [128, D], F32) for i in range(nwho)]
        for who in range(nwho):
            P.dma("sp", g2b[who].all(), modrow(l, who, "g2"))
        pT1 = P.psum("epT", [128, 1024], BF16)
        pT = [pT1, pT1]
        h1As = [P.psum("eh1A%d" % i, [128, 512], F32) for i in range(2)]
        h3As = [P.psum("eh3A%d" % i, [128, 512], F32) for i in range(2)]
        hB = P.psum("ehB", [128, 512], F32)
        hB3 = hB
        pYe = [P.psum("epY%d" % i, [128, 512], F32) for i in range(2)]
        NSL = NS * 128
        state = {"n": 0}

        def wsteps(e):
            buf = e % 2
            for wi in range(3):
                for cb in range(4):
                    yield (wi, cb, buf, e)

        def do_step(step):
            wi, cb, buf, e = step
            i = state["n"]
            state["n"] += 1
            st = wst[i % 3]
            src = moe_w[wi][l, e, cb * 256:(cb + 1) * 256, :]
            P.dma("sp" if i % 2 == 0 else "act", st.all(), src.with_ap(src.ap.rearrange("(c p) n -> p c n", p=128)))
            P.copy(("act", "dve", "act")[i % 3], wbf[wi][buf][:, 2 * cb:2 * cb + 2, :], st.all())

        for step in wsteps(0):
            do_step(step)
        for e in range(16):
            buf = e % 2
            nxt = wsteps(e + 1) if e + 1 < 16 else iter(())

            def pre(n=1):
                for _ in range(n):
                    s = next(nxt, None)
                    if s is not None:
                        do_step(s)
            def gather(ee):
                xg = xes[ee % 2]
                for st in range(NS):
                    iv = idxT[:, st, ee:ee + 1]
                    P.dma("pool", xg[:, st, :], h2_d.all(),
                          fn=lambda en, o=xg[:, st, :].ap, i_=iv.ap: en.indirect_dma_start(
                              out=o, out_offset=None, in_=h2_d.all().ap, in_offset=bass.IndirectOffsetOnAxis(ap=i_, axis=0)),
                          extra_reads=[iv])
            if e == 0:
                gather(0)
            if e + 1 < 16:
                gather(e + 1)
            xe = xes[e % 2]
            for c in range(8):
                P.transposes([(pT[c % 2][:, st * 128:(st + 1) * 128], xe[:, st, c * 128:(c + 1) * 128]) for st in range(NS)], identb.all())
                P.copy("act" if c % 2 == 0 else "dve", xeT[:, c, :], pT[c % 2][:, 0:NSL])
            w1, w3, w2 = wbf[0][buf], wbf[1][buf], wbf[2][buf]
            for fc in range(8):
                fs = slice(fc * 128, (fc + 1) * 128)
                h1A, h3A = h1As[fc % 2], h3As[fc % 2]
                P.mm(h1A.all(), [(w1[:, c, fs], xeT[:, c, 0:512]) for c in range(8)])
                P.mm(h3A.all(), [(w3[:, c, fs], xeT[:, c, 0:512]) for c in range(8)])
                if NS == 5:
                    P.mm(hB[:, 0:128], [(w1[:, c, fs], xeT[:, c, 512:640]) for c in range(8)])
                    P.mm(hB3[:, 128:256], [(w3[:, c, fs], xeT[:, c, 512:640]) for c in range(8)])
                P.act(s1[:, 0:512], h1A.all(), AF.Silu)
                P.tt("dve", hidT[:, fc, 0:512], s1[:, 0:512], h3A.all(), ALU.mult)
                if NS == 5:
                    P.act(s1[:, 512:640], hB[:, 0:128], AF.Silu)
                    P.tt("dve", hidT[:, fc, 512:640], s1[:, 512:640], hB3[:, 128:256], ALU.mult)
                pre(1)
            for st in range(NS):
                y = yst[(e * NS + st) % 3]
                who = 1 if st == 4 else 0
                for half in range(2):
                    hs = slice(half * 512, (half + 1) * 512)
                    P.mm(pYe[half].all(), [(hidT[:, fc, st * 128:(st + 1) * 128], w2[:, fc, hs]) for fc in range(8)])
                    P.stt(y[:, hs], pYe[half].all(), gateT[:, st, e:e + 1], g2b[who][:, hs], ALU.mult, ALU.mult)
                iv = idxT[:, st, e:e + 1]
                P.dma("pool", x1_d.all(), y.all(),
                      fn=lambda en, y_=y.all().ap, i_=iv.ap: en.indirect_dma_start(
                          out=x1_d.all().ap, out_offset=bass.IndirectOffsetOnAxis(ap=i_, axis=0), in_=y_, in_offset=None,
                          compute_op=ALU.add),
                      extra_reads=[iv])
                pre(1)
            pre(12)
        P.pop()

    def phase_final():
        P.push()
        fnb = P.sbuf("fnb", [128, D], F32)
        P.dma("sp", fnb.all(), brow(final_norm, 0, D))
        xt = [P.sbuf("fxt%d" % i, [128, D], F32) for i in range(2)]
        ot = [P.sbuf("fot%d" % i, [128, D], F32) for i in range(2)]
        junk = P.sbuf("fjunk", [128, D], BF16)
        ss = P.sbuf("fss", [128, 1], F32)
        rstd = P.sbuf("frstd", [128, 1], F32)
        for t in range(32):
            k = t % 2
            P.dma("sp", xt[k].all(), x1_d[t * 128:(t + 1) * 128, :])
            P.act(junk.all(), xt[k].all(), AF.Square, accum=ss.all())
            rstd_of(ss.all(), rstd.all(), D)
            P.stt(ot[k].all(), xt[k].all(), rstd.all(), fnb.all(), ALU.mult, ALU.mult)
            P.dma("act", out_d[t * 128:(t + 1) * 128, :], ot[k].all())
        P.pop()

    def phase_A1():
        P.push()
        NW = 2944
        wb = P.sbuf("w1b", [128, 8, NW], BF16)
        wst = [P.sbuf("w1st%d" % i, [128, 8, 256], F32) for i in range(2)]
        nb = 0
        c0 = 0
        while c0 < NW:
            w_ = min(256, NW - c0)
            src = w_in1[:, c0:c0 + w_]
            P.dma("sp" if nb % 2 == 0 else "act", wst[nb % 2][:, :, 0:w_], src.with_ap(src.ap.rearrange("(c p) n -> p c n", p=128)))
            P.copy(cast_rot(nb), wb[:, :, c0:c0 + w_], wst[nb % 2][:, :, 0:w_])
            c0 += w_
            nb += 1
        sb = [P.sbuf("sb%d" % i, [128, D], F32) for i in range(2)]
        shb = [P.sbuf("shb%d" % i, [128, D], F32) for i in range(2)]
        xt = [P.sbuf("xt%d" % i, [128, D], F32) for i in range(2)]
        tmp32 = P.sbuf("tmp32", [128, D], F32)
        junk = P.sbuf("junk", [128, D], BF16)
        make_sb(1, 0, 1, sb[0], shb[0], xt[0], xt[1])
        make_sb(1, 1, 1, sb[1], shb[1], xt[0], xt[1])
        qnb = P.sbuf("qnb", [128, 384], F32)
        P.dma("sp", qnb[:, 0:256], brow(mla_q_norm, 0, 256))
        P.dma("sp", qnb[:, 256:384], brow(mla_kv_norm, 0, 128))
        hb = [P.sbuf("hb%d" % i, [128, D], BF16) for i in range(2)]
        hTs = [P.sbuf("hT%d" % i, [128, 8, 512], BF16) for i in range(2)]
        cst = P.sbuf("cst", [128, 2, 512], F32)
        fst = P.sbuf("fst", [128, 9, 512], BF16)
        cqst = P.sbuf("cqst", [128, 3, 512], BF16)
        cqn = P.sbuf("cqn", [128, 384], BF16)
        tkst = P.sbuf("tkst", [128, 4, 1280], BF16)
        t1 = P.sbuf("rt1", [128, 512], F32)
        t2 = P.sbuf("rt2", [128, 512], F32)
        ss = P.sbuf("ss", [128, 2], F32)
        rstd = P.sbuf("rstd", [128, 2], F32)
        pT = [P.psum("pT%d" % i, [128, 1024], BF16) for i in range(2)]
        pa = [P.psum("pa%d" % i, [128, 512], F32) for i in range(2)]
        pb = P.psum("pb", [128, 512], F32)
        pv = [P.psum("pv%d" % i, [128, 512], F32) for i in range(2)]
        pq = P.psum("pq", [128, 1024], BF16)
        def S1(blk):
            ntile = 4 if blk < 8 else 2
            who = 0 if blk < 8 else 1
            hT = hTs[blk % 2]
            for ti in range(ntile):
                t = blk * 4 + ti
                x = xt[ti % 2]
                P.dma("sp", x.all(), x1_d[t * 128:(t + 1) * 128, :])
                P.act(junk.all(), x.all(), AF.Square, accum=ss[:, 0:1])
                rstd_of(ss[:, 0:1], rstd[:, 0:1], D)
                P.stt(tmp32.all(), x.all(), rstd[:, 0:1], sb[who].all(), ALU.mult, ALU.mult)
                P.tt("pool", hb[ti % 2].all(), tmp32.all(), shb[who].all(), ALU.add)
                P.transposes([(pT[ti % 2][:, c * 128:(c + 1) * 128], hb[ti % 2][:, c * 128:(c + 1) * 128]) for c in range(8)], identb.all())
                pv_ = pT[ti % 2].all()
                P.copy("act", hT[:, :, ti * 128:(ti + 1) * 128], pv_.with_ap(pv_.ap.rearrange("p (c t) -> p c t", c=8)))

        def S2(blk):
            ntile = 4 if blk < 8 else 2
            ntok = ntile * 128
            tok0 = blk * 512
            hT = hTs[blk % 2]
            for ti in range(ntile):
                hts = slice(ti * 128, (ti + 1) * 128)
                P.mm(pv[0][:, 0:384], [(hT[:, c, hts], wb[:, c, 0:384]) for c in range(8)])
                P.act(junk[:, 0:256], pv[0][:, 0:256], AF.Square, accum=ss[:, 0:1])
                P.act(junk[:, 256:384], pv[0][:, 256:384], AF.Square, accum=ss[:, 1:2])
                rstd_of(ss[:, 0:1], rstd[:, 0:1], 256)
                rstd_of(ss[:, 1:2], rstd[:, 1:2], 128)
                P.stt(cqn[:, 0:256], pv[0][:, 0:256], rstd[:, 0:1], qnb[:, 0:256], ALU.mult, ALU.mult)
                P.stt(cqn[:, 256:384], pv[0][:, 256:384], rstd[:, 1:2], qnb[:, 256:384], ALU.mult, ALU.mult)
                P.transposes([(pq[:, c * 128:(c + 1) * 128], cqn[:, c * 128:(c + 1) * 128]) for c in range(3)], identb.all())
                pq_ = pq[:, 0:384]
                P.copy("dve", cqst[:, :, hts], pq_.with_ap(pq_.ap.rearrange("p (c t) -> p c t", c=3)))
                P.mm(pv[1][:, 0:256], [(hT[:, c, hts], wb[:, c, 384:640]) for c in range(8)])
                P.copy("dve", tkst[:, ti, 0:256], pv[1][:, 0:256])
                P.mm(pv[0].all(), [(hT[:, c, hts], wb[:, c, 640:1152]) for c in range(8)])
                P.copy("act", tkst[:, ti, 256:768], pv[0].all())
                P.mm(pv[1].all(), [(hT[:, c, hts], wb[:, c, 1152:1664]) for c in range(8)])
                P.act(tkst[:, ti, 768:1280], pv[1].all(), AF.Silu)
            if blk < 8:
                P.dma("sp", cst[:, 0, :], rope_m[0, :, tok0:tok0 + 512])
                P.dma("sp", cst[:, 1, :], rope_m[1, :, tok0:tok0 + 512])
            P.mm(pa[0][:, 0:ntok], [(wb[:, c, 1664:1792], hT[:, c, 0:ntok]) for c in range(8)])
            if blk < 8:
                P.mm(pb[:, 0:ntok], [(wb[:, c, 1792:1920], hT[:, c, 0:ntok]) for c in range(8)])
                P.tt("dve", t1[:, 0:ntok], pa[0][:, 0:ntok], cst[:, 0, 0:ntok], ALU.mult)
                P.tt("dve", t2[:, 0:ntok], pb[:, 0:ntok], cst[:, 1, 0:ntok], ALU.mult)
                P.tt("pool", fst[:, 0, 0:ntok], t1[:, 0:ntok], t2[:, 0:ntok], ALU.add)
            else:
                P.copy("act", fst[:, 0, 0:ntok], pa[0][:, 0:ntok])
            for g in range(8):
                col0 = 1920 + g * 128
                ps = pa[(g + 1) % 2]
                P.mm(ps[:, 0:ntok], [(wb[:, c, col0:col0 + 128], hT[:, c, 0:ntok]) for c in range(8)])
                P.copy("act" if g % 2 == 0 else "dve", fst[:, 1 + g, 0:ntok], ps[:, 0:ntok])
            P.dma("pool", krT_d[:, tok0:tok0 + ntok], fst[:, 0, 0:ntok])
            for gi, dst in enumerate((rqT_d, rkT_d)):
                dv = dst[:, :, tok0:tok0 + ntok]
                P.dma("pool", dv.with_ap(dv.ap.rearrange("h p t -> p h t")), fst[:, 1 + gi * 4:5 + gi * 4, 0:ntok])
            dv = cqn_d[:, :, tok0:tok0 + ntok]
            P.dma("pool", dv.with_ap(dv.ap.rearrange("c p t -> p c t")), cqst[:, :, 0:ntok])
            for dst, lo, hi in ((rk_d, 0, 256), (rv_d, 256, 768), (sg_d, 768, 1280)):
                dv = dst[tok0:tok0 + ntok, :]
                P.dma("pool", dv.with_ap(dv.ap.rearrange("(t p) c -> p t c", p=128)), tkst[:, 0:ntile, lo:hi])

        S1(0)
        for blk in range(9):
            if blk + 1 < 9:
                S1(blk + 1)
            S2(blk)
        P.pop()

    def phase_B1():
        P.push()
        cqnT = P.sbuf("cqnT", [128, 3, LT], BF16)
        for c in range(3):
            P.dma("sp" if c % 2 == 0 else "act", cqnT[:, c, :], cqn_d[c])
        wuqf = P.sbuf("wuqf", [128, 2, 1536], F32)
        wuq = P.sbuf("wuq", [128, 2, 1536], BF16)
        P.dma("sp", wuqf.all(), w_uq.all().with_ap(w_uq.base.rearrange("(c p) n -> p c n", p=128)))
        P.copy("dve", wuq.all(), wuqf.all())
        wukvf = P.sbuf("wukvf", [128, 1024], F32)
        wukv = P.sbuf("wukv", [128, 1024], BF16)
        P.dma("sp", wukvf.all(), w_ukv.all())
        P.copy("pool", wukv.all(), wukvf.all())
        Vm = P.sbuf("Vm", [128, NT, 8, 66], BF16)
        P.memset("dve", Vm[:, :, :, 64:66], 1.0)
        KT = P.sbuf("KTm", [96, LT], BF16)
        QT = [P.sbuf("QTm%d" % i, [96, LT], BF16) for i in range(2)]
        P.dma("sp", KT[64:96, :], krT_d[64:96, :])
        csm = P.sbuf("csm", [96, 2, L], F32)
        P.dma("sp", csm[64:96, 0, :], rope_m[0, 64:96, :])
        P.dma("act", csm[64:96, 1, :], rope_m[1, 64:96, :])
        ptm = [P.sbuf("ptm%d" % i, [128, 1024], BF16) for i in range(3)]
        ost = [P.sbuf("most%d" % i, [128, 4, 64], BF16) for i in range(2)]
        rr = P.sbuf("mrr", [128, 4], F32)
        t1 = P.sbuf("mt1", [96, 512], F32)
        t2 = P.sbuf("mt2", [96, 512], F32)
        psV = [P.psum("mpV%d" % i, [128, 512], F32) for i in range(2)]
        psS = [P.psum("mpS%d" % i, [128, 1024], F32) for i in range(2)]
        psO = [P.psum("mpO%d" % i, [128, 512], F32) for i in range(2)]
        class _Half:
            def __init__(self, tt_, lo):
                self.tt_, self.lo = tt_, lo

            def __getitem__(self, idx):
                r, c = idx
                c0 = 0 if c.start is None else c.start
                c1 = 512 if c.stop is None else c.stop
                return self.tt_[r, self.lo + c0:self.lo + c1]
        psQs = [_Half(psS[0], 0), _Half(psS[1], 0)]
        psQ2s = [_Half(psS[0], 512), _Half(psS[1], 512)]
        for t in range(NT):
            P.mm(psV[t % 2].all(), [(cqnT[:, 2, t * 128:(t + 1) * 128], wukv[:, 512:1024])])
            pv_ = psV[t % 2].all()
            P.copy("act" if t % 2 == 0 else "dve", Vm[:, t, :, 0:64], pv_.with_ap(pv_.ap.rearrange("p (h e) -> p h e", h=8)))
        sc = 96.0 ** -0.5
        for h in range(8):
            q_ = QT[h % 2]
            for blk in range(9):
                ntok = 512 if blk < 8 else 256
                tok0 = blk * 512
                ts_ = slice(tok0, tok0 + ntok)
                psQ, psQ2 = psQs[blk % 2], psQ2s[blk % 2]
                P.mm(psV[blk % 2][0:64, 0:ntok], [(wukv[:, h * 64:(h + 1) * 64], cqnT[:, 2, ts_])])
                P.copy("pool" if False else "dve", KT[0:64, ts_], psV[blk % 2][0:64, 0:ntok])
                P.mm(psQ[0:96, 0:ntok], [(wuq[:, c, h * 96:(h + 1) * 96], cqnT[:, c, ts_]) for c in range(2)])
                P.copy("act", q_[0:64, ts_], psQ[0:64, 0:ntok])
                if blk < 8:
                    P.mm(psQ2[0:96, 0:ntok], [(wuq[:, c, 768 + h * 96:768 + (h + 1) * 96], cqnT[:, c, ts_]) for c in range(2)])
                    P.tt("dve", t1[64:96, 0:ntok], psQ[64:96, 0:ntok], csm[64:96, 0, ts_], ALU.mult)
                    P.tt("dve", t2[64:96, 0:ntok], psQ2[64:96, 0:ntok], csm[64:96, 1, ts_], ALU.mult)
                    P.tt("pool", q_[64:96, ts_], t1[64:96, 0:ntok], t2[64:96, 0:ntok], ALU.add)
                else:
                    P.copy("act", q_[64:96, ts_], psQ[64:96, 0:ntok])
            for qb in range(8):
                q0 = qb * 512
                ob = psO[qb % 2]

                def qk_ex(pi):
                    s_ = psS[pi % 2]
                    for u in range(2):
                        kt = 2 * pi + u
                        P.mm(s_[:, u * 512:(u + 1) * 512], [(KT[0:96, kt * 128:(kt + 1) * 128], q_[0:96, q0:q0 + 512])])
                    P.act(ptm[pi % 3].all(), s_.all(), AF.Exp, scale=sc)

                def pv(pi):
                    p = ptm[pi % 3]
                    for u in range(2):
                        kt = 2 * pi + u
                        for qi in range(4):
                            P.mm(ob[:, qi * 65:(qi + 1) * 65], [(p[:, u * 512 + qi * 128:u * 512 + (qi + 1) * 128], Vm[:, kt, h, 0:65])],
                                 start=(kt == 0 and qi == 0), skip=True)

                qk_ex(0)
                for pi in range(NT // 2):
                    if pi + 1 < NT // 2:
                        qk_ex(pi + 1)
                    pv(pi)
                o = ost[qb % 2]
                for qi in range(4):
                    P.recip(rr[:, qi:qi + 1], ob[:, qi * 65 + 64:qi * 65 + 65])
                    P.ts("dve", o[:, qi, :], ob[:, qi * 65:qi * 65 + 64], rr[:, qi:qi + 1], None, ALU.mult)
                dv = ao_d[q0:q0 + 512, h * 64:(h + 1) * 64]
                P.dma("pool", dv.with_ap(dv.ap.rearrange("(t p) c -> p t c", p=128)), o.all())
        P.pop()

    def phase_C1():
        P.push()
        dl = P.sbuf("rdl", [128, 8], F32)
        lg = P.sbuf("rlg", [128, 8], F32)
        P.dma("sp", dl.all(), brow(ret_decay, 0, 8))
        P.act(lg.all(), dl.all(), AF.Exp, scale=-1.0)
        P.ts("dve", lg.all(), lg.all(), 1.0, None, ALU.add)
        P.act(lg.all(), lg.all(), AF.Ln)
        P.ts("dve", lg.all(), lg.all(), -1.0, None, ALU.mult)
        rmp = P.sbuf("rmp", [128, 4, 128], F32)
        P.dma("sp", rmp.all(), ramps_d.all())
        lgfb = P.sbuf("lgfb", [128, 1], F32)
        dq = P.sbuf("rdq", [128, 128], F32)
        dk = P.sbuf("rdk", [128, 2], F32)
        gC = P.sbuf("rgC", [128, 1], F32)
        DT = P.sbuf("rDT", [128, 128], F32)
        dtmp = P.sbuf("rdtmp", [128, 128], F32)
        qT = P.sbuf("rqT", [128, L], BF16)
        kT = P.sbuf("rkT", [64, L], BF16)
        ktok = P.sbuf("rktok", [128, NT, 64], BF16)
        vtok = P.sbuf("rvtok", [128, NT, 128], BF16)
        sg = P.sbuf("rsg", [128, 32, 128], BF16)
        U = P.sbuf("rU", [128, NT, 128], F32)
        S = P.sbuf("rS", [128, 32, 128], F32)
        Sb = P.sbuf("rSb", [128, 32, 128], BF16)
        Kd = [P.sbuf("rKd%d" % i, [128, 128], BF16) for i in range(2)]
        AT = [P.sbuf("rAT%d" % i, [128, 128], BF16) for i in range(2)]
        Qd = [P.sbuf("rQd%d" % i, [128, 128], BF16) for i in range(2)]
        r = [P.sbuf("rr%d" % i, [128, 128], F32) for i in range(2)]
        junk = P.sbuf("rjunk", [128, 128], F32)
        ss = P.sbuf("rss", [128, 1], F32)
        rs = P.sbuf("rrs", [128, 1], F32)
        ost = [P.sbuf("rost%d" % i, [128, 4, 128], BF16) for i in range(2)]
        psU = [P.psum("rpU%d" % i, [128, 512], F32) for i in range(2)]
        psS = [P.psum("rpS%d" % i, [128, 512], F32) for i in range(2)]
        psO = [P.psum("rpO%d" % i, [128, 512], F32) for i in range(2)]
        for h in range(4):
            P.copy("dve", lgfb[0:64, :], lg[0:64, h:h + 1])
            P.copy("dve", lgfb[64:128, :], lg[64:128, 4 + h:5 + h])
            P.act(dq.all(), rmp[:, 0, :], AF.Exp, scale=lgfb.all())
            P.act(dk[:, 0:1], rmp[:, 3, 0:1], AF.Exp, scale=lg[:, h:h + 1])
            P.act(dk[:, 1:2], rmp[:, 3, 1:2], AF.Exp, scale=lg[:, 4 + h:5 + h])
            P.act(gC.all(), rmp[:, 3, 2:3], AF.Exp, scale=lgfb.all())
            P.ts("dve", dtmp.all(), rmp[:, 1, :], lg[:, h:h + 1], None, ALU.mult)
            P.stt(dtmp.all(), rmp[:, 2, :], lg[:, 4 + h:5 + h], dtmp.all(), ALU.mult, ALU.add)
            P.act(DT.all(), dtmp.all(), AF.Exp)
            P.dma("sp", qT.all(), rqT_d[h, :, 0:L])
            P.dma("act", kT.all(), rkT_d[h, 0:64, 0:L])
            sv = rk_d[:, h * 64:(h + 1) * 64]
            P.dma("sp", ktok.all(), sv.with_ap(sv.ap.rearrange("(t p) c -> p t c", p=128)))
            sv = rv_d[:, h * 128:(h + 1) * 128]
            P.dma("act", vtok.all(), sv.with_ap(sv.ap.rearrange("(t p) c -> p t c", p=128)))
            sv = sg_d[0:L, h * 128:(h + 1) * 128]
            P.dma("sp", sg.all(), sv.with_ap(sv.ap.rearrange("(t p) c -> p t c", p=128)))
            for t in range(NT):
                kd = Kd[t % 2]
                P.ts("dve", kd[:, 0:64], ktok[:, t, :], dk[:, 0:1], None, ALU.mult)
                P.ts("pool", kd[:, 64:128], ktok[:, t, :], dk[:, 1:2], None, ALU.mult)
                P.mm(psU[t % 2][:, 0:128], [(kd.all(), vtok[:, t, :])])
                P.copy("act", U[:, t, :], psU[t % 2][:, 0:128])
            P.stt(S[0:64, 0, :], U[0:64, 32, :], gC[0:64, :], U[0:64, 33, :], ALU.mult, ALU.add)
            for n in range(31):
                P.stt(S[0:64, n + 1, :], S[0:64, n, :], gC[0:64, :], U[0:64, n, :], ALU.mult, ALU.add)
            P.stt(S[64:128, 31, :], U[64:128, 33, :], gC[64:128, :], U[64:128, 32, :], ALU.mult, ALU.add)
            for n in range(31, 0, -1):
                P.stt(S[64:128, n - 1, :], S[64:128, n, :], gC[64:128, :], U[64:128, n, :], ALU.mult, ALU.add)
            P.copy("pool", Sb.all(), S.all())
            def pre(n):
                ns = slice(n * 128, (n + 1) * 128)
                P.mm(psS[n % 2][:, 0:128], [(kT[0:64, ns], qT[0:64, ns])])
                P.tt("dve", AT[n % 2].all(), psS[n % 2][:, 0:128], DT.all(), ALU.mult)
                P.tt("pool", Qd[n % 2].all(), qT[:, ns], dq.all(), ALU.mult)
            pre(0)
            for n in range(32):
                ns = slice(n * 128, (n + 1) * 128)
                if n + 1 < 32:
                    pre(n + 1)
                P.mm(psO[n % 2][:, 0:128], [(AT[n % 2].all(), vtok[:, n, :]), (Qd[n % 2].all(), Sb[:, n, :])])
                rr_ = r[n % 2]
                P.act(rr_.all(), psO[n % 2][:, 0:128], AF.Copy, scale=0.125)
                P.act(junk.all(), rr_.all(), AF.Square, accum=ss.all())
                rstd_of(ss.all(), rs.all(), 128)
                o = ost[(n // 4) % 2]
                P.stt(o[:, n % 4, :], rr_.all(), rs.all(), sg[:, n, :], ALU.mult, ALU.mult)
                if n % 4 == 3:
                    q0 = (n - 3) * 128
                    dv = ao_d[q0:q0 + 512, 512 + h * 128:512 + (h + 1) * 128]
                    P.dma("pool", dv.with_ap(dv.ap.rearrange("(t p) c -> p t c", p=128)), o.all())
        P.pop()

    seq = [
        ("A0", lambda: phase_A0()),
        ("B0", lambda: phase_B0()),
        ("C0", lambda: phase_C0()),
        ("D0", None), ("T0", None), ("E0", None),
        ("A1", lambda: phase_A1()),
        ("B1", lambda: phase_B1()),
        ("C1", lambda: phase_C1()),
        ("D1", None), ("T1", None), ("E1", None),
        ("F", lambda: phase_final()),
    ]
    affT = None
    for name, fn in seq:
        if only is not None and name not in only:
            continue
        if name in ("D0", "D1"):
            l = 0 if name == "D0" else 1
            P.push()
            affT = P.sbuf("affT", [16, LT], F32)
            phase_D(l, NT if l == 0 else 32, affT)
        elif name in ("T0", "T1"):
            l = 0 if name == "T0" else 1
            phase_topk(l, affT)
            P.pop()
        elif name in ("E0", "E1"):
            l = 0 if name == "E0" else 1
            phase_experts(l, 5 if l == 0 else 4)
        else:
            fn()
        if stop(name):
            break
    if stop_after in ("D0", "D1"):
        P.pop()
    P.finish()
    P.stats = dict(P.ninst)
    return nc


_CACHE = {}


def kernel(**inputs):
    sh, per = host_prep(inputs)
    if "nc" not in _CACHE:
        _CACHE["nc"] = build()
    nc = _CACHE["nc"]
    n = len(per)
    in_maps = []
    for b in range(n):
        m = dict(sh)
        m.update(per[b])
        in_maps.append(m)
    res = run_bass_kernel_spmd(nc, in_maps, core_ids=list(range(n)))
    return np.stack([np.asarray(r["out"], dtype=np.float32) for r in res.results], axis=0)
```

```python
import os
import numpy as np
from contextlib import ExitStack
import concourse.bass as bass
import concourse.mybir as mybir
from concourse.bass_utils import run_bass_kernel_spmd

F32 = mybir.dt.float32
BF16 = mybir.dt.bfloat16
I32 = mybir.dt.int32
U32 = mybir.dt.uint32
AF = mybir.ActivationFunctionType
ALU = mybir.AluOpType

D = 1024
L = 4096
LC = 256
LT = L + LC
NT = LT // 128
NEG = -30000.0


class View:
    __slots__ = ("ap", "tt", "reg")

    def __init__(self, ap, tt, reg):
        self.ap = ap
        self.tt = tt
        self.reg = reg

    def with_ap(self, ap):
        return View(ap, self.tt, self.reg)


class TT:
    def __init__(self, name, base_ap, shape, space):
        self.name = name
        self.base = base_ap
        self.shape = tuple(shape)
        self.space = space
        self.recs = []

    def __getitem__(self, idx):
        if not isinstance(idx, tuple):
            idx = (idx,)
        idx = tuple(idx) + (slice(None),) * (len(self.shape) - len(idx))
        reg = []
        for i, s in enumerate(idx):
            n = self.shape[i]
            if isinstance(s, int):
                assert 0 <= s < n, (self.name, idx, self.shape)
                reg.append((s, s + 1))
            else:
                lo = 0 if s.start is None else s.start
                hi = n if s.stop is None else s.stop
                assert 0 <= lo < hi <= n, (self.name, idx, self.shape)
                reg.append((lo, hi))
        return View(self.base[idx], self, tuple(reg))

    def all(self):
        return self[tuple(slice(None) for _ in self.shape)]


def _overlap(a, b):
    for (l1, h1), (l2, h2) in zip(a, b):
        if h1 <= l2 or h2 <= l1:
            return False
    return True


def _contains(a, b):
    for (l1, h1), (l2, h2) in zip(a, b):
        if l2 < l1 or h2 > h1:
            return False
    return True


class Prog:
    def __init__(self, nc, n_dma_sems=10):
        self.nc = nc
        self.eobj = {"pe": nc.tensor, "act": nc.scalar, "dve": nc.vector, "pool": nc.gpsimd, "sp": nc.sync}
        self.es = ExitStack()
        self.cnt = {e: 0 for e in self.eobj}
        self.ninst = {e: 0 for e in self.eobj}
        self.seen = {e: {} for e in self.eobj}
        self.esem = {}
        for e in ("pe", "act", "dve", "pool"):
            self.esem[e] = self.es.enter_context(nc.semaphore("es_" + e))
        self.dsem = {}
        self.dtot = {}
        self.dnext = {}
        self.semobj = {}
        for e in ("sp", "act", "pool"):
            self.dsem[e] = [self.es.enter_context(nc.semaphore("ds_%s%d" % (e, i))) for i in range(n_dma_sems)]
            self.dnext[e] = 0
            for s in self.dsem[e]:
                self.dtot[id(s)] = 0
                self.semobj[id(s)] = s
        for e, s in self.esem.items():
            self.semobj[id(s)] = s
        self.scopes = []

    def push(self):
        st = ExitStack()
        self.scopes.append(st)
        return st

    def pop(self):
        self.barrier()
        self.scopes.pop().close()

    def _stack(self):
        return self.scopes[-1] if self.scopes else self.es

    def sbuf(self, name, shape, dt):
        self.uid = getattr(self, "uid", 0) + 1
        name = "%s_%d" % (name, self.uid)
        h = self._stack().enter_context(self.nc.sbuf_tensor(name, list(shape), dt))
        return TT(name, h[tuple(slice(None) for _ in shape)], shape, "sbuf")

    def psum(self, name, shape, dt):
        self.uid = getattr(self, "uid", 0) + 1
        name = "%s_%d" % (name, self.uid)
        h = self._stack().enter_context(self.nc.psum_tensor(name, list(shape), dt))
        return TT(name, h[tuple(slice(None) for _ in shape)], shape, "psum")

    def dram(self, name, shape, dt, kind=None):
        if kind is None:
            h = self.nc.dram_tensor(name, list(shape), dt)
        else:
            h = self.nc.dram_tensor(name, list(shape), dt, kind=kind)
        return TT(name, h.ap(), shape, "dram")

    def _deps(self, reads, writes):
        deps = {}
        for v in reads:
            for r in v.tt.recs:
                if r[1] and _overlap(r[0], v.reg):
                    if deps.get(r[2], 0) < r[3]:
                        deps[r[2]] = r[3]
        for v in writes:
            for r in v.tt.recs:
                if _overlap(r[0], v.reg):
                    if deps.get(r[2], 0) < r[3]:
                        deps[r[2]] = r[3]
        return deps

    def _record(self, reads, writes, sk, val):
        for v in writes:
            recs = v.tt.recs
            recs[:] = [r for r in recs if not _contains(v.reg, r[0])]
            recs.append([v.reg, True, sk, val])
        for v in reads:
            recs = v.tt.recs
            for r in recs:
                if (not r[1]) and r[2] == sk and r[0] == v.reg:
                    if r[3] < val:
                        r[3] = val
                    break
            else:
                recs.append([v.reg, False, sk, val])

    def _psum_deps(self, eng, views, deps, val=None):
        mysk = id(self.esem[eng]) if eng in self.esem else None
        for v in views:
            if v.tt.space != "psum":
                continue
            last = v.tt.__dict__.setdefault("last", {})
            if val is None:
                for sk, lv in last.items():
                    if sk != mysk and deps.get(sk, 0) < lv:
                        deps[sk] = lv
            else:
                last[mysk] = val

    def _emit(self, eng, deps, fn, sk, inc):
        e = self.eobj[eng]
        seen = self.seen[eng]
        for wsk, wv in deps.items():
            if seen.get(wsk, 0) < wv:
                seen[wsk] = wv
                e.wait_ge(self.semobj[wsk], wv)
        fn(e).then_inc(self.semobj[sk], inc)
        self.ninst[eng] += 1

    def op(self, eng, fn, reads=(), writes=()):
        deps = self._deps(reads, writes)
        self._psum_deps(eng, list(reads) + list(writes), deps)
        self.cnt[eng] += 1
        sk = id(self.esem[eng])
        self._emit(eng, deps, fn, sk, 1)
        self._record(reads, writes, sk, self.cnt[eng])
        self._psum_deps(eng, list(reads) + list(writes), None, val=self.cnt[eng])

    def dma(self, eng, out, in_, fn=None, extra_reads=()):
        reads = [in_] + list(extra_reads)
        deps = self._deps(reads, [out])
        pool = self.dsem[eng]
        s = pool[self.dnext[eng] % len(pool)]
        self.dnext[eng] += 1
        sk = id(s)
        if self.dtot[sk] > 0 and deps.get(sk, 0) < self.dtot[sk]:
            deps[sk] = self.dtot[sk]
        self.dtot[sk] += 16
        if fn is None:
            oap, iap = out.ap, in_.ap
            fn = lambda e: e.dma_start(out=oap, in_=iap)
        self._emit(eng, deps, fn, sk, 16)
        self._record(reads, [out], sk, self.dtot[sk])

    def barrier(self):
        allev = {}
        for e in ("pe", "act", "dve", "pool"):
            if self.cnt[e] > 0:
                allev[id(self.esem[e])] = self.cnt[e]
        for sk, tot in self.dtot.items():
            if tot > 0:
                allev[sk] = tot
        for eng, e in self.eobj.items():
            seen = self.seen[eng]
            for wsk, wv in allev.items():
                if seen.get(wsk, 0) < wv:
                    seen[wsk] = wv
                    e.wait_ge(self.semobj[wsk], wv)

    def finish(self):
        self.barrier()
        while self.scopes:
            self.scopes.pop().close()
        self.es.close()

    def act(self, out, in_, func, scale=1.0, bias=None, accum=None, eng="act"):
        rd = [in_]
        wr = [out]
        kw = {}
        if isinstance(scale, View):
            rd.append(scale)
            kw["scale"] = scale.ap
        else:
            kw["scale"] = float(scale)
        if bias is not None:
            if isinstance(bias, View):
                rd.append(bias)
                kw["bias"] = bias.ap
            else:
                kw["bias"] = float(bias)
        if accum is not None:
            wr.append(accum)
            kw["accum_out"] = accum.ap
        oap, iap = out.ap, in_.ap
        self.op("act", lambda e: e.activation(out=oap, in_=iap, func=func, **kw), rd, wr)

    def ts(self, eng, out, in0, s1, s2=None, op0=ALU.mult, op1=None, accum=None):
        rd = [in0]
        wr = [out]
        a1 = s1
        if isinstance(s1, View):
            rd.append(s1)
            a1 = s1.ap
        a2 = s2
        if isinstance(s2, View):
            rd.append(s2)
            a2 = s2.ap
        kw = {}
        if op1 is not None:
            kw["op1"] = op1
        if accum is not None:
            kw["accum_out"] = accum.ap
            wr.append(accum)
        oap, iap = out.ap, in0.ap
        self.op(eng, lambda e: e.tensor_scalar(out=oap, in0=iap, scalar1=a1, scalar2=a2, op0=op0, **kw), rd, wr)

    def tt(self, eng, out, in0, in1, op):
        oap, a, b = out.ap, in0.ap, in1.ap
        self.op(eng, lambda e: e.tensor_tensor(out=oap, in0=a, in1=b, op=op), [in0, in1], [out])

    def stt(self, out, in0, scalar, in1, op0, op1, accum=None):
        rd = [in0, in1]
        wr = [out]
        sc = scalar
        if isinstance(scalar, View):
            rd.append(scalar)
            sc = scalar.ap
        kw = {}
        if accum is not None:
            kw["accum_out"] = accum.ap
            wr.append(accum)
        oap, a, b = out.ap, in0.ap, in1.ap
        self.op("dve", lambda e: e.scalar_tensor_tensor(out=oap, in0=a, scalar=sc, in1=b, op0=op0, op1=op1, **kw), rd, wr)

    def copy(self, eng, out, in_):
        oap, iap = out.ap, in_.ap
        if eng == "act":
            self.op("act", lambda e: e.activation(out=oap, in_=iap, func=AF.Copy), [in_], [out])
        else:
            self.op(eng, lambda e: e.tensor_copy(out=oap, in_=iap), [in_], [out])

    def memset(self, eng, out, val):
        oap = out.ap
        self.op(eng, lambda e: e.memset(oap, val), [], [out])

    def recip(self, out, in_):
        oap, iap = out.ap, in_.ap
        self.op("dve", lambda e: e.reciprocal(out=oap, in_=iap), [in_], [out])

    def mm(self, out, pairs, start=True, skip=False):
        rd = []
        for a, b in pairs:
            rd.append(a)
            rd.append(b)
        oap = out.ap
        aps = [(a.ap, b.ap) for a, b in pairs]
        n = len(aps)

        def fn(e):
            ins = None
            for i, (la, ra) in enumerate(aps):
                st = start and i == 0
                if skip:
                    ins = e.matmul(oap, lhsT=la, rhs=ra, start=st, stop=(i == n - 1), skip_group_check=True)
                else:
                    ins = e.matmul(oap, lhsT=la, rhs=ra, start=st, stop=(i == n - 1))
            return ins
        self.op("pe", fn, rd, [out])

    def transposes(self, items, ident, extra_writes=()):
        rd = [ident] + [b for _, b in items]
        wr = [a for a, _ in items]
        aps = [(a.ap, b.ap) for a, b in items]
        iap = ident.ap

        def fn(e):
            ins = None
            for oa, ia in aps:
                ins = e.transpose(out=oa, in_=ia, identity=iap)
            return ins
        self.op("pe", fn, rd, wr)


def bview(tt_dram, row_idx, lo, hi, reg_tt=None):
    v = tt_dram[row_idx + (slice(lo, hi),)] if isinstance(row_idx, tuple) else tt_dram[row_idx, lo:hi]
    return v.with_ap(v.ap.partition_broadcast(128))


def _rope_tables():
    f32 = np.float32
    tok = np.arange(L)
    rows = (tok // 64).astype(f32)
    cols = (tok % 64).astype(f32)
    fr = (np.float32(10000.0) ** (-np.arange(16, dtype=f32) / np.float32(16))).astype(f32)
    cs = np.zeros((2, 128, L), f32)
    for p in range(128):
        d = p % 64
        pos = rows if d < 32 else cols
        ang = (pos * fr[d % 16]).astype(f32)
        cs[0, p] = np.cos(ang)
        sg = -1.0 if (d % 32) < 16 else 1.0
        cs[1, p] = sg * np.sin(ang)
    fr8 = (np.float32(10000.0) ** (-np.arange(8, dtype=f32) / np.float32(8))).astype(f32)
    cm = np.zeros((2, 128, L), f32)
    for d in range(32):
        pos = rows if d < 16 else cols
        ang = (pos * fr8[d % 8]).astype(f32)
        cm[0, 64 + d] = np.cos(ang)
        sg = -1.0 if (d % 16) < 8 else 1.0
        cm[1, 64 + d] = sg * np.sin(ang)
    return cs, cm


def _na_tiles():
    pats = {}
    per_j = []
    for j in range(32):
        if 2 <= j <= 29:
            kts = list(range(j - 2, j + 3))
            ids = [kt - j + 2 for kt in kts]
        else:
            kts = [0, 1, 2, 3] if j < 2 else [28, 29, 30, 31]
            base = {0: 5, 1: 9, 30: 13, 31: 17}[j]
            ids = [base + i for i in range(4)]
        for kt, pid in zip(kts, ids):
            if pid not in pats:
                pats[pid] = (j, kt)
        per_j.append(list(zip(kts, ids)))
    return per_j, pats


def _na_bias(rpb):
    per_j, pats = _na_tiles()
    out = np.full((21, 128, 8, 128), NEG, np.float32)
    kl = np.arange(128)
    ql = np.arange(128)
    for pid, (j, kt) in pats.items():
        kr = 2 * kt + kl // 64
        kc = kl % 64
        qr = 2 * j + ql // 64
        qc = ql % 64
        rs = np.clip(qr - 4, 0, 56)
        cst = np.clip(qc - 8, 0, 48)
        inwin = ((kr[:, None] >= rs[None, :]) & (kr[:, None] < rs[None, :] + 8) &
                 (kc[:, None] >= cst[None, :]) & (kc[:, None] < cst[None, :] + 16))
        ro = np.clip(kr[:, None] - qr[None, :] + 7, 0, 14)
        co = np.clip(kc[:, None] - qc[None, :] + 15, 0, 30)
        for h in range(8):
            g = rpb[h][ro, co]
            out[pid, :, (h % 2) * 4 + h // 2, :] = np.where(inwin, g, np.float32(NEG))
    return out


def host_prep(inp):
    f32 = np.float32
    sh = {}
    cs, cm = _rope_tables()
    sh["rope_d"] = cs
    sh["rope_m"] = cm
    sh["ident"] = np.eye(128, dtype=f32)
    sh["na_bias"] = _na_bias(np.asarray(inp["na_rpb"][0]))
    pos = np.arange(128, dtype=f32)
    ramps = np.zeros((128, 4, 128), f32)
    ramps[:64, 0, :] = pos[None, :] + 1.0
    ramps[64:, 0, :] = 128.0 - pos[None, :]
    ramps[:, 1, :] = np.maximum(pos[None, :] - pos[:, None], 0.0)
    ramps[:, 2, :] = np.maximum(pos[:, None] - pos[None, :], 0.0)
    ramps[:, 3, 0] = 127.0 - pos
    ramps[:, 3, 1] = pos
    ramps[:, 3, 2] = 128.0
    sh["ramps"] = ramps
    sh["dummy_idx"] = np.ascontiguousarray(np.broadcast_to((LT + np.arange(128, dtype=np.int32))[:, None], (128, 16)))
    w = np.asarray(inp["even_w_in"][0])
    d = np.arange(64)
    sw = np.where((d % 32) < 16, d + 16, d - 16)
    idx = np.arange(512).reshape(4, 2, 64)[:, :, sw].reshape(-1)
    sh["w_in0"] = np.ascontiguousarray(np.concatenate([w, w[:, 0:512][:, idx], w[:, 512:1024][:, idx]], axis=1))
    sh["w_out0"] = np.asarray(inp["even_w_out"][0])
    w1 = np.asarray(inp["odd_w_in"][0])
    cq, ckv, kr = w1[:, 0:256], w1[:, 256:384], w1[:, 384:416]
    rq, rk, rv, rg = w1[:, 416:672], w1[:, 672:928], w1[:, 928:1440], w1[:, 1440:1952]
    d32 = np.arange(32)
    sw32 = np.where((d32 % 16) < 8, d32 + 8, d32 - 8)
    z64 = np.zeros((D, 64), f32)
    z32 = np.zeros((D, 32), f32)
    kr_pad = np.concatenate([z64, kr, z32], axis=1)
    kr_sw_pad = np.concatenate([z64, kr[:, sw32], z32], axis=1)
    rq_dup = np.concatenate([np.concatenate([rq[:, h * 64:(h + 1) * 64]] * 2, axis=1) for h in range(4)], axis=1)
    rk_dup = np.concatenate([np.concatenate([rk[:, h * 64:(h + 1) * 64]] * 2, axis=1) for h in range(4)], axis=1)
    sh["w_in1"] = np.ascontiguousarray(np.concatenate([cq, ckv, rk, rv, rg, kr_pad, kr_sw_pad, rq_dup, rk_dup], axis=1))
    sh["w_out1"] = np.asarray(inp["odd_w_out"][0])
    wuq = np.asarray(inp["mla_w_uq"][0])
    ci = np.arange(768).reshape(8, 96).copy()
    ci[:, 64:] = ci[:, 64:][:, sw32]
    sh["w_uq"] = np.ascontiguousarray(np.concatenate([wuq, wuq[:, ci.reshape(-1)]], axis=1))
    wukv = np.asarray(inp["mla_w_ukv"][0]).reshape(128, 8, 128)
    sh["w_ukv"] = np.ascontiguousarray(np.concatenate([wukv[:, :, :64].reshape(128, 512), wukv[:, :, 64:].reshape(128, 512)], axis=1))
    for k in ("ada_w", "ada_b", "norm_mix", "norm_ffn", "final_norm", "diff_lambda", "diff_subln", "mla_q_norm",
              "mla_kv_norm", "ret_decay_logit", "moe_router", "moe_w1", "moe_w3", "moe_w2"):
        sh[k] = np.ascontiguousarray(np.asarray(inp[k], dtype=f32))
    sh["final_norm"] = sh["final_norm"].reshape(1, D)
    sh["diff_lambda"] = sh["diff_lambda"].reshape(1, 256)
    sh["ret_decay_logit"] = sh["ret_decay_logit"].reshape(1, 8)
    per = []
    c_ctx = np.asarray(inp["c_ctx"], f32)
    for b in range(inp["x"].shape[0]):
        cc = np.stack([np.asarray(inp["c"][b], f32).reshape(8, 128).T, c_ctx.reshape(8, 128).T], axis=-1)
        xa = np.concatenate([np.asarray(inp["x"][b], f32), np.asarray(inp["ctx"][b], f32)], axis=0)
        per.append({"xin": np.ascontiguousarray(xa), "cc": np.ascontiguousarray(cc)})
    return sh, per


def build(stop_after=None, debug=False, only=None):
    nc = bass.Bass("TRN2", target_bir_lowering=False)
    P = Prog(nc)
    EXT = "ExternalOutput" if debug else None

    def din(name, shape, dt=F32):
        return P.dram(name, shape, dt, kind="ExternalInput")

    xin = din("xin", [LT, D])
    cc = din("cc", [128, 8, 2])
    rope_d = din("rope_d", [2, 128, L])
    rope_m = din("rope_m", [2, 128, L])
    ident_d = din("ident", [128, 128])
    na_bias = din("na_bias", [21, 128, 8, 128])
    ramps_d = din("ramps", [128, 4, 128])
    dummy_idx = din("dummy_idx", [128, 16], I32)
    w_in0 = din("w_in0", [D, 4096])
    w_out0 = din("w_out0", [D, D])
    w_in1 = din("w_in1", [D, 2944])
    w_out1 = din("w_out1", [D, D])
    w_uq = din("w_uq", [256, 1536])
    w_ukv = din("w_ukv", [128, 1024])
    ada_w = din("ada_w", [2, D, 6 * D])
    ada_b = din("ada_b", [2, 6 * D])
    norm_mix = din("norm_mix", [2, D])
    norm_ffn = din("norm_ffn", [2, D])
    final_norm = din("final_norm", [1, D])
    diff_lambda = din("diff_lambda", [1, 256])
    diff_subln = din("diff_subln", [1, 128])
    mla_q_norm = din("mla_q_norm", [1, 256])
    mla_kv_norm = din("mla_kv_norm", [1, 128])
    ret_decay = din("ret_decay_logit", [1, 8])
    moe_router = din("moe_router", [2, D, 16])
    moe_w = [din("moe_w1", [2, 16, D, D]), din("moe_w3", [2, 16, D, D]), din("moe_w2", [2, 16, D, D])]
    out_d = P.dram("out", [L, D], F32, kind="ExternalOutput")

    mods_d = P.dram("mods_d", [2, 2, 6 * D], F32, kind=EXT)
    x1_d = P.dram("x1_d", [LT + 128, D], F32, kind=EXT)
    h2_d = P.dram("h2_d", [LT + 128, D], BF16, kind=EXT)
    ao_d = P.dram("ao_d", [LT, D], BF16, kind=EXT)
    qTd = P.dram("qTd", [4, 128, LT], BF16, kind=EXT)
    kTd = P.dram("kTd", [4, 128, LT], BF16, kind=EXT)
    qTn = P.dram("qTn", [4, 128, LT], BF16, kind=EXT)
    kTn = P.dram("kTn", [4, 128, LT], BF16, kind=EXT)
    vd = P.dram("vd", [LT, 512], BF16, kind=EXT)
    vn = P.dram("vn", [LT, 512], BF16, kind=EXT)
    cqn_d = P.dram("cqn_d", [3, 128, LT], BF16, kind=EXT)
    krT_d = P.dram("krT_d", [128, LT], BF16, kind=EXT)
    rqT_d = P.dram("rqT_d", [4, 128, LT], BF16, kind=EXT)
    rkT_d = P.dram("rkT_d", [4, 128, LT], BF16, kind=EXT)
    rk_d = P.dram("rk_d", [LT, 256], BF16, kind=EXT)
    rv_d = P.dram("rv_d", [LT, 512], BF16, kind=EXT)
    sg_d = P.dram("sg_d", [LT, 512], BF16, kind=EXT)
    dbg_aff = P.dram("dbg_aff", [16, LT], F32, kind=EXT) if debug else None
    dbg_idx = P.dram("dbg_idx", [128, 5, 16], I32, kind=EXT) if debug else None

    identf = P.sbuf("identf", [128, 128], F32)
    identb = P.sbuf("identb", [128, 128], BF16)
    gateT = P.sbuf("gateT", [128, 5, 16], F32)
    idxT = P.sbuf("idxT", [128, 5, 16], I32)
    P.dma("sp", identf.all(), ident_d.all())
    P.copy("dve", identb.all(), identf.all())
    eps_t = P.sbuf("eps_t", [128, 1], F32)
    P.memset("dve", eps_t.all(), 1e-6)

    MOD = {"sh1": 0, "sc1": 1, "g1": 2, "sh2": 3, "sc2": 4, "g2": 5}

    def modrow(l, who, name):
        k = MOD[name]
        v = mods_d[l, who, k * D:(k + 1) * D]
        return v.with_ap(v.ap.partition_broadcast(128))

    def brow(tt_d, idx, n):
        v = tt_d[idx, 0:n]
        return v.with_ap(v.ap.partition_broadcast(128))

    def stop(name):
        return stop_after == name

    P.push()
    zt = P.sbuf("zt", [128, D], F32)
    ztb = P.sbuf("ztb", [128, D], BF16)
    P.memset("dve", zt.all(), 0.0)
    P.memset("pool", ztb.all(), 0.0)
    P.dma("sp", x1_d[LT:LT + 128, :], zt.all())
    P.dma("sp", h2_d[LT:LT + 128, :], ztb.all())
    P.pop()
    P.push()
    ccs = P.sbuf("ccs", [128, 8, 2], F32)
    scs = P.sbuf("scs", [128, 8, 2], F32)
    P.dma("sp", ccs.all(), cc.all())
    P.act(scs.all(), ccs.all(), AF.Silu)
    adw = [P.sbuf("adw%d" % i, [128, 8, 512], F32) for i in range(2)]
    modsb = P.sbuf("modsb", [2, 6 * D], F32)
    adb = P.sbuf("adb", [2, 6 * D], F32)
    psm = [P.psum("psm%d" % i, [128, 512], F32) for i in range(2)]
    for l in range(2):
        P.dma("sp", adb[0:1, :], ada_b[l:l + 1, :])
        P.dma("sp", adb[1:2, :], ada_b[l:l + 1, :])
        for nb in range(12):
            w = adw[nb % 2]
            src = ada_w[l, :, nb * 512:(nb + 1) * 512]
            P.dma("sp" if nb % 2 == 0 else "act", w.all(), src.with_ap(src.ap.rearrange("(c p) n -> p c n", p=128)))
            P.mm(psm[nb % 2][0:2, :], [(scs[:, c, :], w[:, c, :]) for c in range(8)])
            P.tt("dve", modsb[:, nb * 512:(nb + 1) * 512], psm[nb % 2][0:2, :], adb[:, nb * 512:(nb + 1) * 512], ALU.add)
        P.dma("sp", mods_d[l], modsb.all())
    P.pop()
    if stop("mods"):
        P.finish()
        return nc

    def make_sb(l, who, which, s_b, sh_b, t1, t2):
        nrm = norm_mix if which == 1 else norm_ffn
        P.dma("sp", t1.all(), modrow(l, who, "sc%d" % which))
        P.dma("sp", t2.all(), brow(nrm, l, D))
        P.stt(s_b.all(), t1.all(), 1.0, t2.all(), ALU.add, ALU.mult)
        P.dma("sp", sh_b.all(), modrow(l, who, "sh%d" % which))

    def rstd_of(ss, rstd, n):
        P.act(rstd, ss, AF.Ln, scale=1.0 / n, bias=eps_t[:, 0:1])
        P.act(rstd, rstd, AF.Exp, scale=-0.5)


    def cast_rot(i):
        return ("dve", "pool", "act")[i % 3]

    def phase_A0():
        P.push()
        wb = P.sbuf("w0b", [128, 8, 4096], BF16)
        wst = [P.sbuf("w0st%d" % i, [128, 8, 256], F32) for i in range(2)]
        for nb in range(16):
            src = w_in0[:, nb * 256:(nb + 1) * 256]
            P.dma("sp" if nb % 2 == 0 else "act", wst[nb % 2].all(), src.with_ap(src.ap.rearrange("(c p) n -> p c n", p=128)))
            P.copy(cast_rot(nb), wb[:, :, nb * 256:(nb + 1) * 256], wst[nb % 2].all())
        sb = [P.sbuf("sb%d" % i, [128, D], F32) for i in range(2)]
        shb = [P.sbuf("shb%d" % i, [128, D], F32) for i in range(2)]
        xt = [P.sbuf("xt%d" % i, [128, D], F32) for i in range(2)]
        tmp32 = P.sbuf("tmp32", [128, D], F32)
        junk = P.sbuf("junk", [128, D], BF16)
        make_sb(0, 0, 1, sb[0], shb[0], xt[0], xt[1])
        make_sb(0, 1, 1, sb[1], shb[1], xt[0], xt[1])
        hb = [P.sbuf("hb%d" % i, [128, D], BF16) for i in range(2)]
        hTs = [P.sbuf("hT%d" % i, [128, 8, 512], BF16) for i in range(2)]
        cst = P.sbuf("cst", [128, 2, 512], F32)
        fst = P.sbuf("fst", [128, 16, 512], BF16)
        vst = P.sbuf("vst", [128, 4, 1024], BF16)
        t1 = P.sbuf("rt1", [128, 512], F32)
        t2 = P.sbuf("rt2", [128, 512], F32)
        ss = P.sbuf("ss", [128, 1], F32)
        rstd = P.sbuf("rstd", [128, 1], F32)
        pT = [P.psum("pT%d" % i, [128, 1024], BF16) for i in range(2)]
        pa = [P.psum("pa%d" % i, [128, 512], F32) for i in range(2)]
        pb = [P.psum("pb%d" % i, [128, 512], F32) for i in range(2)]
        pv = [P.psum("pv%d" % i, [128, 512], F32) for i in range(2)]
        def S1(blk):
            ntile = 4 if blk < 8 else 2
            who = 0 if blk < 8 else 1
            hT = hTs[blk % 2]
            for ti in range(ntile):
                t = blk * 4 + ti
                x = xt[ti % 2]
                P.dma("sp", x.all(), xin[t * 128:(t + 1) * 128, :])
                P.act(junk.all(), x.all(), AF.Square, accum=ss.all())
                rstd_of(ss.all(), rstd.all(), D)
                P.stt(tmp32.all(), x.all(), rstd.all(), sb[who].all(), ALU.mult, ALU.mult)
                P.tt("pool", hb[ti % 2].all(), tmp32.all(), shb[who].all(), ALU.add)
                P.transposes([(pT[ti % 2][:, c * 128:(c + 1) * 128], hb[ti % 2][:, c * 128:(c + 1) * 128]) for c in range(8)], identb.all())
                pv_ = pT[ti % 2].all()
                P.copy("act", hT[:, :, ti * 128:(ti + 1) * 128], pv_.with_ap(pv_.ap.rearrange("p (c t) -> p c t", c=8)))

        def S2(blk):
            ntile = 4 if blk < 8 else 2
            ntok = ntile * 128
            tok0 = blk * 512
            hT = hTs[blk % 2]
            if blk < 8:
                P.dma("sp", cst[:, 0, :], rope_d[0, :, tok0:tok0 + 512])
                P.dma("sp", cst[:, 1, :], rope_d[1, :, tok0:tok0 + 512])
            for g in range(16):
                col0 = [0, 512, 1536, 2048][g // 4] + (g % 4) * 128
                ps = pa[g % 2]
                P.mm(ps[:, 0:ntok], [(wb[:, c, col0:col0 + 128], hT[:, c, 0:ntok]) for c in range(8)])
                if g < 8 and blk < 8:
                    sc0 = 3072 + (g // 4) * 512 + (g % 4) * 128
                    ps2 = pb[g % 2]
                    P.mm(ps2[:, 0:ntok], [(wb[:, c, sc0:sc0 + 128], hT[:, c, 0:ntok]) for c in range(8)])
                    P.tt("dve", t1[:, 0:ntok], ps[:, 0:ntok], cst[:, 0, 0:ntok], ALU.mult)
                    P.tt("dve", t2[:, 0:ntok], ps2[:, 0:ntok], cst[:, 1, 0:ntok], ALU.mult)
                    P.tt("pool", fst[:, g, 0:ntok], t1[:, 0:ntok], t2[:, 0:ntok], ALU.add)
                else:
                    P.copy("act", fst[:, g, 0:ntok], ps[:, 0:ntok])
            for gi, dst in enumerate((qTd, kTd, qTn, kTn)):
                dv = dst[:, :, tok0:tok0 + ntok]
                P.dma("pool", dv.with_ap(dv.ap.rearrange("h p t -> p h t")), fst[:, gi * 4:(gi + 1) * 4, 0:ntok])
            for ti in range(ntile):
                for vi, c0 in enumerate((1024, 2560)):
                    psv = pv[vi]
                    P.mm(psv.all(), [(hT[:, c, ti * 128:(ti + 1) * 128], wb[:, c, c0:c0 + 512]) for c in range(8)])
                    P.copy("dve" if vi == 0 else "act", vst[:, ti, vi * 512:(vi + 1) * 512], psv.all())
            for vi, dst in enumerate((vd, vn)):
                dv = dst[tok0:tok0 + ntok, :]
                P.dma("pool", dv.with_ap(dv.ap.rearrange("(t p) c -> p t c", p=128)), vst[:, 0:ntile, vi * 512:(vi + 1) * 512])

        S1(0)
        for blk in range(9):
            if blk + 1 < 9:
                S1(blk + 1)
            S2(blk)
        P.pop()

    def phase_B0():
        lam_init = 0.2
        P.push()
        QT = [P.sbuf("QTd%d" % i, [128, LT], BF16) for i in range(2)]
        KT = [P.sbuf("KTd%d" % i, [128, LT], BF16) for i in range(2)]
        V = [P.sbuf("Vd%d" % i, [128, NT, 130], BF16) for i in range(2)]
        for i in range(2):
            P.memset("dve", V[i][:, :, 128:130], 1.0)
        pt = [P.sbuf("pt%d" % i, [128, 2, 512], BF16) for i in range(3)]
        dl = P.sbuf("dl", [128, 256], F32)
        dtmp = P.sbuf("dtmp", [128, 128], F32)
        sm = P.sbuf("lsm", [128, 2], F32)
        lam = P.sbuf("lam", [128, 1], F32)
        nlam = P.sbuf("nlam", [128, 1], F32)
        gsub = P.sbuf("gsub", [128, 128], F32)
        P.dma("sp", dl.all(), brow(diff_lambda, 0, 256))
        P.tt("dve", dtmp[:, 0:64], dl[:, 0:64], dl[:, 64:128], ALU.mult)
        P.tt("dve", dtmp[:, 64:128], dl[:, 128:192], dl[:, 192:256], ALU.mult)
        P.act(dtmp[:, 0:64], dtmp[:, 0:64], AF.Identity, accum=sm[:, 0:1])
        P.act(dtmp[:, 64:128], dtmp[:, 64:128], AF.Identity, accum=sm[:, 1:2])
        P.act(sm.all(), sm.all(), AF.Exp)
        P.tt("dve", lam.all(), sm[:, 0:1], sm[:, 1:2], ALU.subtract)
        P.ts("dve", nlam.all(), lam.all(), lam_init, -1.0, ALU.add, ALU.mult)
        P.dma("sp", gsub.all(), brow(diff_subln, 0, 128))
        P.ts("dve", gsub.all(), gsub.all(), 1.0 - lam_init, None, ALU.mult)
        osb = [P.sbuf("osb%d" % i, [128, 4, 2, 129], F32) for i in range(2)]
        rl = P.sbuf("rl", [128, 4, 2, 1], F32)
        aall = P.sbuf("aall", [128, 4, 128], F32)
        atmp = P.sbuf("atmp", [128, 4, 128], F32)
        junk = P.sbuf("junkd", [128, 128], F32)
        ss = P.sbuf("ssd", [128, 4], F32)
        rs = P.sbuf("rsd", [128, 4], F32)
        aost = [P.sbuf("aost%d" % i, [128, 4, 128], BF16) for i in range(2)]
        psS = [P.psum("psS%d" % b, [128, 1024], F32) for b in range(2)]
        psO = [P.psum("psO%d" % i, [128, 512], F32) for i in range(3)]
        it = 0
        for h in range(4):
            b = h % 2
            P.dma("sp", QT[b].all(), qTd[h])
            P.dma("sp", KT[b].all(), kTd[h])
            sv = vd[:, h * 128:(h + 1) * 128]
            P.dma("sp", V[b][:, :, 0:128], sv.with_ap(sv.ap.rearrange("(t p) c -> p t c", p=128)))
            for qb in range(9):
                nq = 4 if qb < 8 else 2
                nqt = nq * 128
                q0 = qb * 512
                kts = list(range(NT)) if qb < 8 else [32, 33]
                first = [True, True, True]

                def qk_ex(ki):
                    kt = kts[ki]
                    s_ = psS[ki % 2]
                    for m in range(2):
                        P.mm(s_[:, m * 512:m * 512 + nqt], [(KT[b][m * 64:(m + 1) * 64, kt * 128:(kt + 1) * 128],
                                                             QT[b][m * 64:(m + 1) * 64, q0:q0 + nqt])])
                    p = pt[ki % 3]
                    sv_ = s_.all()
                    sv3 = sv_.with_ap(sv_.ap.rearrange("p (m q) -> p m q", m=2)[:, :, 0:nqt])
                    P.act(p[:, :, 0:nqt], sv3, AF.Exp, scale=0.125)

                def pv(ki):
                    kt = kts[ki]
                    p = pt[ki % 3]
                    for qi in range(nq):
                        for m in range(2):
                            a = qi * 2 + m
                            bank, off = a // 3, (a % 3) * 129
                            P.mm(psO[bank][:, off:off + 129], [(p[:, m, qi * 128:(qi + 1) * 128], V[b][:, kt, 0:129])],
                                 start=first[bank], skip=True)
                            first[bank] = False

                qk_ex(0)
                for ki in range(len(kts)):
                    if ki + 1 < len(kts):
                        qk_ex(ki + 1)
                    pv(ki)
                ao = aost[it % 2]
                ob_ = osb[it % 2]
                it += 1
                na = nq * 2
                for bank in range((na + 2) // 3):
                    n_in = min(3, na - bank * 3)
                    ov = ob_.all()
                    o2 = ov.with_ap(ov.ap.rearrange("p a m c -> p (a m c)")[:, bank * 387:bank * 387 + n_in * 129])
                    P.copy("dve", o2, psO[bank][:, 0:n_in * 129])
                P.recip(rl[:, 0:nq], ob_[:, 0:nq, :, 128:129])
                P.ts("dve", rl[:, 0:nq, 1, :], rl[:, 0:nq, 1, :], nlam.all(), None, ALU.mult)
                r0 = rl[:, 0:nq, 0, :]
                r1_ = rl[:, 0:nq, 1, :]
                P.tt("dve", aall[:, 0:nq, :], ob_[:, 0:nq, 0, 0:128], r0.with_ap(r0.ap.to_broadcast([128, nq, 128])), ALU.mult)
                P.tt("pool", atmp[:, 0:nq, :], ob_[:, 0:nq, 1, 0:128], r1_.with_ap(r1_.ap.to_broadcast([128, nq, 128])), ALU.mult)
                P.tt("dve", aall[:, 0:nq, :], aall[:, 0:nq, :], atmp[:, 0:nq, :], ALU.add)
                for qi in range(nq):
                    P.stt(junk.all(), aall[:, qi, :], 1.0, aall[:, qi, :], ALU.mult, ALU.mult, accum=ss[:, qi:qi + 1])
                P.act(rs[:, 0:nq], ss[:, 0:nq], AF.Ln, scale=1.0 / 128, bias=eps_t[:, 0:1])
                P.act(rs[:, 0:nq], rs[:, 0:nq], AF.Exp, scale=-0.5)
                for qi in range(nq):
                    P.stt(ao[:, qi, :], aall[:, qi, :], rs[:, qi:qi + 1], gsub.all(), ALU.mult, ALU.mult)
                dv = ao_d[q0:q0 + nqt, h * 128:(h + 1) * 128]
                P.dma("pool", dv.with_ap(dv.ap.rearrange("(t p) c -> p t c", p=128)), ao[:, 0:nq, :])
        P.pop()

    def phase_C0():
        P.push()
        QTn_ = P.sbuf("QTn", [128, 4, LT], BF16)
        KTn_ = P.sbuf("KTn", [128, 4, LT], BF16)
        Vn_ = P.sbuf("Vn", [128, NT, 8, 66], BF16)
        P.memset("dve", Vn_[:, :, :, 64:66], 1.0)
        for hp in range(4):
            P.dma("sp", QTn_[:, hp, :], qTn[hp])
            P.dma("act", KTn_[:, hp, :], kTn[hp])
        for h in range(8):
            sv = vn[:, h * 64:(h + 1) * 64]
            P.dma("sp" if h % 2 == 0 else "act", Vn_[:, :, h, 0:64], sv.with_ap(sv.ap.rearrange("(t p) c -> p t c", p=128)))
        breg = P.sbuf("nab", [128, 5, 8, 128], F32)
        bedge = P.sbuf("nae", [128, 4, 8, 128], F32)
        for i in range(5):
            P.dma("sp", breg[:, i, :, :], na_bias[i])
        ssb = [P.sbuf("nas%d" % i, [128, 8, 128], F32) for i in range(2)]
        ptn = [P.sbuf("napt%d" % i, [128, 8, 128], BF16) for i in range(3)]
        onst = [P.sbuf("naost%d" % i, [128, 512], BF16) for i in range(2)]
        rr = P.sbuf("narr", [128, 8], F32)
        psS = [[P.psum("nS%d%d" % (b, k), [128, 512], F32) for k in range(2)] for b in range(2)]
        psO = [[P.psum("nO%d%d" % (b, k), [128, 512], F32) for k in range(2)] for b in range(2)]
        per_j, _ = _na_tiles()
        _js = [int(v) for v in os.environ.get('C0_J', '').split(',') if v and v != 'none'] or list(range(NT))
        if os.environ.get('C0_J') == 'none':
            _js = []
        _nopv = os.environ.get('C0_NOPV') == '1'
        _noexp = os.environ.get('C0_NOEXP') == '1'
        for j in _js:
            if j < 32:
                tiles = per_j[j] + [(32, None), (33, None)]
            else:
                tiles = [(32, None), (33, None)]
            base = {0: 5, 1: 9, 30: 13, 31: 17}.get(j)
            if base is not None:
                for i in range(4):
                    P.dma("sp", bedge[:, i, :, :], na_bias[base + i])
            first = [True, True]
            ob = psO[j % 2]
            def qk_ex(ki):
                kt, pid = tiles[ki]
                s_ = psS[ki % 2]
                for h in range(8):
                    hp, hh = h // 2, h % 2
                    P.mm(s_[hh][:, hp * 128:(hp + 1) * 128],
                         [(KTn_[hh * 64:(hh + 1) * 64, hp, kt * 128:(kt + 1) * 128], QTn_[hh * 64:(hh + 1) * 64, hp, j * 128:(j + 1) * 128])])
                p = ptn[ki % 3]
                sbuf_s = ssb[ki % 2]
                for half in range(2):
                    pv_ = s_[half].all()
                    pv3 = pv_.with_ap(pv_.ap.rearrange("p (h q) -> p h q", h=4))
                    hs = slice(half * 4, half * 4 + 4)
                    if pid is not None:
                        bt = breg[:, pid, hs, :] if pid < 5 else bedge[:, pid - base, hs, :]
                        P.stt(sbuf_s[:, hs, :], pv3, 0.125, bt, ALU.mult, ALU.add)
                        P.act(p[:, hs, :], sbuf_s[:, hs, :], AF.Exp)
                    else:
                        P.act(p[:, hs, :], pv3, AF.Exp, scale=0.125)

            def pv(ki):
                kt, pid = tiles[ki]
                p = ptn[ki % 3]
                for h in range(8):
                    bk, off = h // 4, (h % 4) * 65
                    P.mm(ob[bk][:, off:off + 65], [(p[:, (h % 2) * 4 + h // 2, :], Vn_[:, kt, h, 0:65])], start=first[bk], skip=True)
                    first[bk] = False

            qk_ex(0)
            for ki in range(len(tiles)):
                if ki + 1 < len(tiles):
                    qk_ex(ki + 1)
                pv(ki)
            if _nopv:
                continue
            for h in range(8):
                bk, off = h // 4, (h % 4) * 65
                P.recip(rr[:, h:h + 1], ob[bk][:, off + 64:off + 65])
                P.ts("dve", onst[j % 2][:, h * 64:(h + 1) * 64], ob[bk][:, off:off + 64], rr[:, h:h + 1], None, ALU.mult)
            P.dma("pool", ao_d[j * 128:(j + 1) * 128, 512:1024], onst[j % 2].all())
        P.pop()

    def phase_D(l, ntiles, affT):
        w_out = w_out0 if l == 0 else w_out1
        xsrc = xin if l == 0 else x1_d
        P.push()
        wob = P.sbuf("wob", [128, 8, D], BF16)
        wst = [P.sbuf("wost%d" % i, [128, 8, 256], F32) for i in range(2)]
        for nb in range(4):
            src = w_out[:, nb * 256:(nb + 1) * 256]
            P.dma("sp" if nb % 2 == 0 else "act", wst[nb % 2].all(), src.with_ap(src.ap.rearrange("(c p) n -> p c n", p=128)))
            P.copy(cast_rot(nb), wob[:, :, nb * 256:(nb + 1) * 256], wst[nb % 2].all())
        nwho = 2 if l == 0 else 1
        xt = [P.sbuf("dxt%d" % i, [128, D], F32) for i in range(2)]
        tmp32 = P.sbuf("dtmp32", [128, D], F32)
        g1b = [P.sbuf("g1b%d" % i, [128, D], F32) for i in range(nwho)]
        s2b = [P.sbuf("s2b%d" % i, [128, D], F32) for i in range(nwho)]
        sh2b = [P.sbuf("sh2b%d" % i, [128, D], F32) for i in range(nwho)]
        for who in range(nwho):
            P.dma("sp", g1b[who].all(), modrow(l, who, "g1"))
            make_sb(l, who, 2, s2b[who], sh2b[who], xt[0], xt[1])
        wr = P.sbuf("wr", [128, 8, 16], F32)
        srcr = moe_router[l]
        P.dma("sp", wr.all(), srcr.with_ap(srcr.ap.rearrange("(c p) e -> p c e", p=128)))
        aot = [P.sbuf("aot%d" % i, [128, D], BF16) for i in range(2)]
        aoT = [P.sbuf("aoT%d" % i, [128, 8, 128], BF16) for i in range(2)]
        xn = [P.sbuf("dxn%d" % i, [128, D], F32) for i in range(2)]
        h2 = [P.sbuf("dh2%d" % i, [128, D], F32) for i in range(2)]
        h2b = [P.sbuf("dh2b%d" % i, [128, D], BF16) for i in range(2)]
        h2T = P.sbuf("dh2T", [128, 8, 128], F32)
        junk = P.sbuf("djunk", [128, D], BF16)
        ss = P.sbuf("dss", [128, 1], F32)
        rstd = P.sbuf("drstd", [128, 1], F32)
        mx = P.sbuf("dmx", [128, 1], F32)
        sme = P.sbuf("dsme", [128, 1], F32)
        ex = P.sbuf("dex", [128, 16], F32)
        aff = P.sbuf("daff", [128, 16], F32)
        pT = P.psum("dpT", [128, 1024], BF16)
        pY = [P.psum("dpY%d" % i, [128, 512], F32) for i in range(2)]
        pH = [P.psum("dpH%d" % i, [128, 512], F32) for i in range(2)]
        pR = P.psum("dpR", [128, 512], F32)
        pR2 = P.psum("dpR2", [128, 512], F32)
        def S1(t):
            who = 0 if t < 32 else 1
            k = t % 2
            P.dma("sp", aot[k].all(), ao_d[t * 128:(t + 1) * 128, :])
            P.dma("act", xt[k].all(), xsrc[t * 128:(t + 1) * 128, :])
            P.transposes([(pT[:, c * 128:(c + 1) * 128], aot[k][:, c * 128:(c + 1) * 128]) for c in range(8)], identb.all())
            pv_ = pT.all()
            P.copy("act", aoT[k].all(), pv_.with_ap(pv_.ap.rearrange("p (c t) -> p c t", c=8)))
            for half in range(2):
                hs = slice(half * 512, (half + 1) * 512)
                P.mm(pY[half].all(), [(aoT[k][:, c, :], wob[:, c, hs]) for c in range(8)])
                P.tt("dve", tmp32[:, hs], pY[half].all(), g1b[who][:, hs], ALU.mult)
            P.tt("pool", xn[k].all(), xt[k].all(), tmp32.all(), ALU.add)
            P.dma("pool", x1_d[t * 128:(t + 1) * 128, :], xn[k].all())
            P.act(junk.all(), xn[k].all(), AF.Square, accum=ss.all())
            rstd_of(ss.all(), rstd.all(), D)
            P.stt(tmp32.all(), xn[k].all(), rstd.all(), s2b[who].all(), ALU.mult, ALU.mult)
            P.tt("pool", h2[k].all(), tmp32.all(), sh2b[who].all(), ALU.add)
            P.copy("act", h2b[k].all(), h2[k].all())
            P.dma("pool", h2_d[t * 128:(t + 1) * 128, :], h2b[k].all())

        def S2(t):
            k = t % 2
            P.transposes([(pH[c // 4][:, (c % 4) * 128:(c % 4 + 1) * 128], h2[k][:, c * 128:(c + 1) * 128]) for c in range(8)], identf.all())
            for hh in range(2):
                pv_ = pH[hh].all()
                P.copy("dve" if hh == 0 else "act", h2T[:, hh * 4:(hh + 1) * 4, :], pv_.with_ap(pv_.ap.rearrange("p (c t) -> p c t", c=4)))
            P.mm(pR[:, 0:16], [(h2T[:, c, :], wr[:, c, :]) for c in range(8)])
            P.op("dve", lambda e: e.reduce_max(out=mx.all().ap, in_=pR[:, 0:16].ap, axis=mybir.AxisListType.X), [pR[:, 0:16]], [mx.all()])
            P.ts("dve", mx.all(), mx.all(), -1.0, None, ALU.mult)
            P.act(ex.all(), pR[:, 0:16], AF.Exp, bias=mx.all(), accum=sme.all())
            P.recip(sme.all(), sme.all())
            P.ts("dve", aff.all(), ex.all(), sme.all(), None, ALU.mult)
            P.transposes([(pR2[0:16, 0:128], aff.all())], identf.all())
            P.copy("dve", affT[:, t * 128:(t + 1) * 128], pR2[0:16, 0:128])

        S1(0)
        for t in range(ntiles):
            if t + 1 < ntiles:
                S1(t + 1)
            S2(t)
        if debug:
            P.dma("sp", dbg_aff[:, 0:ntiles * 128], affT[:, 0:ntiles * 128])
        P.pop()

    def phase_topk(l, affT):
        P.push()
        work = P.sbuf("tkw", [16, L], F32)
        vals = P.sbuf("tkv", [16, 512], F32)
        idxu = P.sbuf("tki", [16, 512], U32)
        idxf = P.sbuf("tkf", [16, 512], F32)
        pX = [P.psum("tkp%d" % i, [128, 512], F32) for i in range(2)]
        tf = P.sbuf("tktf", [128, 16], F32)

        def topk(src0, wk, v, iu, rounds):
            src = src0
            for r in range(rounds):
                v8 = v[:, r * 8:(r + 1) * 8]
                i8 = iu[:, r * 8:(r + 1) * 8]
                P.op("dve", lambda e, s=src, a=v8: e.max(out=a.ap, in_=s.ap), [src], [v8])
                P.op("dve", lambda e, s=src, a=v8, b=i8: e.max_index(out=b.ap, in_max=a.ap, in_values=s.ap), [src, v8], [i8])
                if r < rounds - 1:
                    P.op("dve", lambda e, s=src, a=v8, w=wk: e.match_replace(out=w.ap, in_to_replace=a.ap, in_values=s.ap, imm_value=-1.0),
                         [src, v8], [wk])
                    src = wk
        topk(affT[:, 0:L], work.all(), vals, idxu, 64)
        P.copy("dve", idxf.all(), idxu.all())
        for st in range(4):
            P.transposes([(pX[0][:, 0:16], vals[:, st * 128:(st + 1) * 128])], identf[0:16, 0:16])
            P.copy("dve", gateT[:, st, :], pX[0][:, 0:16])
            P.transposes([(pX[1][:, 0:16], idxf[:, st * 128:(st + 1) * 128])], identf[0:16, 0:16])
            P.copy("dve", tf.all(), pX[1][:, 0:16])
            P.copy("dve", idxT[:, st, :], tf.all())
        if l == 0:
            workc = P.sbuf("tkwc", [16, LC], F32)
            valsc = P.sbuf("tkvc", [16, 32], F32)
            idxcu = P.sbuf("tkic", [16, 32], U32)
            idxcf = P.sbuf("tkfc", [16, 32], F32)
            topk(affT[:, L:LT], workc.all(), valsc, idxcu, 4)
            P.copy("dve", idxcf.all(), idxcu.all())
            P.ts("dve", idxcf.all(), idxcf.all(), float(L), None, ALU.add)
            P.memset("dve", gateT[:, 4, :], 0.0)
            P.dma("sp", idxT[:, 4, :], dummy_idx.all())
            P.transposes([(pX[0][0:32, 0:16], valsc.all())], identf[0:16, 0:16])
            P.copy("dve", gateT[0:32, 4, :], pX[0][0:32, 0:16])
            P.transposes([(pX[1][0:32, 0:16], idxcf.all())], identf[0:16, 0:16])
            P.copy("dve", tf[0:32, :], pX[1][0:32, 0:16])
            P.copy("dve", idxT[0:32, 4, :], tf[0:32, :])
        if debug:
            P.dma("sp", dbg_idx.all(), idxT.all())
        P.pop()

    def phase_experts(l, NS):
        P.push()
        wbf = [[P.sbuf("ew%d_%d" % (wi, i), [128, 8, D], BF16) for i in range(2)] for wi in range(3)]
        wst = [P.sbuf("est%d" % i, [128, 2, 1024], F32) for i in range(3)]
        xes = [P.sbuf("xe%d" % i, [128, NS, D], BF16) for i in range(2)]
        xeT = P.sbuf("xeT", [128, 8, NS * 128], BF16)
        hidT = P.sbuf("hidT", [128, 8, NS * 128], BF16)
        s1 = P.sbuf("es1", [128, NS * 128], F32)
        yst = [P.sbuf("yst%d" % i, [128, D], F32) for i in range(3)]
        nwho = 2 if NS == 5 else 1
        g2b = [P.sbuf("g2b%d" % i, [128, D], F32) for i in range(nwho)]
        for who in range(nwho):
            P.dma("sp", g2b[who].all(), modrow(l, who, "g2"))
        pT1 = P.psum("epT", [128, 1024], BF16)
        pT = [pT1, pT1]
        h1As = [P.psum("eh1A%d" % i, [128, 512], F32) for i in range(2)]
        h3As = [P.psum("eh3A%d" % i, [128, 512], F32) for i in range(2)]
        hB = P.psum("ehB", [128, 512], F32)
        hB3 = hB
        pYe = [P.psum("epY%d" % i, [128, 512], F32) for i in range(2)]
        NSL = NS * 128
        state = {"n": 0}

        def wsteps(e):
            buf = e % 2
            for wi in range(3):
                for cb in range(4):
                    yield (wi, cb, buf, e)

        def do_step(step):
            wi, cb, buf, e = step
            i = state["n"]
            state["n"] += 1
            st = wst[i % 3]
            src = moe_w[wi][l, e, cb * 256:(cb + 1) * 256, :]
            P.dma("sp" if i % 2 == 0 else "act", st.all(), src.with_ap(src.ap.rearrange("(c p) n -> p c n", p=128)))
            P.copy(("act", "dve", "act")[i % 3], wbf[wi][buf][:, 2 * cb:2 * cb + 2, :], st.all())

        for step in wsteps(0):
            do_step(step)
        for e in range(16):
            buf = e % 2
            nxt = wsteps(e + 1) if e + 1 < 16 else iter(())

            def pre(n=1):
                for _ in range(n):
                    s = next(nxt, None)
                    if s is not None:
                        do_step(s)
            def gather(ee):
                xg = xes[ee % 2]
                for st in range(NS):
                    iv = idxT[:, st, ee:ee + 1]
                    P.dma("pool", xg[:, st, :], h2_d.all(),
                          fn=lambda en, o=xg[:, st, :].ap, i_=iv.ap: en.indirect_dma_start(
                              out=o, out_offset=None, in_=h2_d.all().ap, in_offset=bass.IndirectOffsetOnAxis(ap=i_, axis=0)),
                          extra_reads=[iv])
            if e == 0:
                gather(0)
            if e + 1 < 16:
                gather(e + 1)
            xe = xes[e % 2]
            for c in range(8):
                P.transposes([(pT[c % 2][:, st * 128:(st + 1) * 128], xe[:, st, c * 128:(c + 1) * 128]) for st in range(NS)], identb.all())
                P.copy("act" if c % 2 == 0 else "dve", xeT[:, c, :], pT[c % 2][:, 0:NSL])
            w1, w3, w2 = wbf[0][buf], wbf[1][buf], wbf[2][buf]
            for fc in range(8):
                fs = slice(fc * 128, (fc + 1) * 128)
                h1A, h3A = h1As[fc % 2], h3As[fc % 2]
                P.mm(h1A.all(), [(w1[:, c, fs], xeT[:, c, 0:512]) for c in range(8)])
                P.mm(h3A.all(), [(w3[:, c, fs], xeT[:, c, 0:512]) for c in range(8)])
                if NS == 5:
                    P.mm(hB[:, 0:128], [(w1[:, c, fs], xeT[:, c, 512:640]) for c in range(8)])
                    P.mm(hB3[:, 128:256], [(w3[:, c, fs], xeT[:, c, 512:640]) for c in range(8)])
                P.act(s1[:, 0:512], h1A.all(), AF.Silu)
                P.tt("dve", hidT[:, fc, 0:512], s1[:, 0:512], h3A.all(), ALU.mult)
                if NS == 5:
                    P.act(s1[:, 512:640], hB[:, 0:128], AF.Silu)
                    P.tt("dve", hidT[:, fc, 512:640], s1[:, 512:640], hB3[:, 128:256], ALU.mult)
                pre(1)
            for st in range(NS):
                y = yst[(e * NS + st) % 3]
                who = 1 if st == 4 else 0
                for half in range(2):
                    hs = slice(half * 512, (half + 1) * 512)
                    P.mm(pYe[half].all(), [(hidT[:, fc, st * 128:(st + 1) * 128], w2[:, fc, hs]) for fc in range(8)])
                    P.stt(y[:, hs], pYe[half].all(), gateT[:, st, e:e + 1], g2b[who][:, hs], ALU.mult, ALU.mult)
                iv = idxT[:, st, e:e + 1]
                P.dma("pool", x1_d.all(), y.all(),
                      fn=lambda en, y_=y.all().ap, i_=iv.ap: en.indirect_dma_start(
                          out=x1_d.all().ap, out_offset=bass.IndirectOffsetOnAxis(ap=i_, axis=0), in_=y_, in_offset=None,
                          compute_op=ALU.add),
                      extra_reads=[iv])
                pre(1)
            pre(12)
        P.pop()

    def phase_final():
        P.push()
        fnb = P.sbuf("fnb", [128, D], F32)
        P.dma("sp", fnb.all(), brow(final_norm, 0, D))
        xt = [P.sbuf("fxt%d" % i, [128, D], F32) for i in range(2)]
        ot = [P.sbuf("fot%d" % i, [128, D], F32) for i in range(2)]
        junk = P.sbuf("fjunk", [128, D], BF16)
        ss = P.sbuf("fss", [128, 1], F32)
        rstd = P.sbuf("frstd", [128, 1], F32)
        for t in range(32):
            k = t % 2
            P.dma("sp", xt[k].all(), x1_d[t * 128:(t + 1) * 128, :])
            P.act(junk.all(), xt[k].all(), AF.Square, accum=ss.all())
            rstd_of(ss.all(), rstd.all(), D)
            P.stt(ot[k].all(), xt[k].all(), rstd.all(), fnb.all(), ALU.mult, ALU.mult)
            P.dma("act", out_d[t * 128:(t + 1) * 128, :], ot[k].all())
        P.pop()

    def phase_A1():
        P.push()
        NW = 2944
        wb = P.sbuf("w1b", [128, 8, NW], BF16)
        wst = [P.sbuf("w1st%d" % i, [128, 8, 256], F32) for i in range(2)]
        nb = 0
        c0 = 0
        while c0 < NW:
            w_ = min(256, NW - c0)
            src = w_in1[:, c0:c0 + w_]
            P.dma("sp" if nb % 2 == 0 else "act", wst[nb % 2][:, :, 0:w_], src.with_ap(src.ap.rearrange("(c p) n -> p c n", p=128)))
            P.copy(cast_rot(nb), wb[:, :, c0:c0 + w_], wst[nb % 2][:, :, 0:w_])
            c0 += w_
            nb += 1
        sb = [P.sbuf("sb%d" % i, [128, D], F32) for i in range(2)]
        shb = [P.sbuf("shb%d" % i, [128, D], F32) for i in range(2)]
        xt = [P.sbuf("xt%d" % i, [128, D], F32) for i in range(2)]
        tmp32 = P.sbuf("tmp32", [128, D], F32)
        junk = P.sbuf("junk", [128, D], BF16)
        make_sb(1, 0, 1, sb[0], shb[0], xt[0], xt[1])
        make_sb(1, 1, 1, sb[1], shb[1], xt[0], xt[1])
        qnb = P.sbuf("qnb", [128, 384], F32)
        P.dma("sp", qnb[:, 0:256], brow(mla_q_norm, 0, 256))
        P.dma("sp", qnb[:, 256:384], brow(mla_kv_norm, 0, 128))
        hb = [P.sbuf("hb%d" % i, [128, D], BF16) for i in range(2)]
        hTs = [P.sbuf("hT%d" % i, [128, 8, 512], BF16) for i in range(2)]
        cst = P.sbuf("cst", [128, 2, 512], F32)
        fst = P.sbuf("fst", [128, 9, 512], BF16)
        cqst = P.sbuf("cqst", [128, 3, 512], BF16)
        cqn = P.sbuf("cqn", [128, 384], BF16)
        tkst = P.sbuf("tkst", [128, 4, 1280], BF16)
        t1 = P.sbuf("rt1", [128, 512], F32)
        t2 = P.sbuf("rt2", [128, 512], F32)
        ss = P.sbuf("ss", [128, 2], F32)
        rstd = P.sbuf("rstd", [128, 2], F32)
        pT = [P.psum("pT%d" % i, [128, 1024], BF16) for i in range(2)]
        pa = [P.psum("pa%d" % i, [128, 512], F32) for i in range(2)]
        pb = P.psum("pb", [128, 512], F32)
        pv = [P.psum("pv%d" % i, [128, 512], F32) for i in range(2)]
        pq = P.psum("pq", [128, 1024], BF16)
        def S1(blk):
            ntile = 4 if blk < 8 else 2
            who = 0 if blk < 8 else 1
            hT = hTs[blk % 2]
            for ti in range(ntile):
                t = blk * 4 + ti
                x = xt[ti % 2]
                P.dma("sp", x.all(), x1_d[t * 128:(t + 1) * 128, :])
                P.act(junk.all(), x.all(), AF.Square, accum=ss[:, 0:1])
                rstd_of(ss[:, 0:1], rstd[:, 0:1], D)
                P.stt(tmp32.all(), x.all(), rstd[:, 0:1], sb[who].all(), ALU.mult, ALU.mult)
                P.tt("pool", hb[ti % 2].all(), tmp32.all(), shb[who].all(), ALU.add)
                P.transposes([(pT[ti % 2][:, c * 128:(c + 1) * 128], hb[ti % 2][:, c * 128:(c + 1) * 128]) for c in range(8)], identb.all())
                pv_ = pT[ti % 2].all()
                P.copy("act", hT[:, :, ti * 128:(ti + 1) * 128], pv_.with_ap(pv_.ap.rearrange("p (c t) -> p c t", c=8)))

        def S2(blk):
            ntile = 4 if blk < 8 else 2
            ntok = ntile * 128
            tok0 = blk * 512
            hT = hTs[blk % 2]
            for ti in range(ntile):
                hts = slice(ti * 128, (ti + 1) * 128)
                P.mm(pv[0][:, 0:384], [(hT[:, c, hts], wb[:, c, 0:384]) for c in range(8)])
                P.act(junk[:, 0:256], pv[0][:, 0:256], AF.Square, accum=ss[:, 0:1])
                P.act(junk[:, 256:384], pv[0][:, 256:384], AF.Square, accum=ss[:, 1:2])
                rstd_of(ss[:, 0:1], rstd[:, 0:1], 256)
                rstd_of(ss[:, 1:2], rstd[:, 1:2], 128)
                P.stt(cqn[:, 0:256], pv[0][:, 0:256], rstd[:, 0:1], qnb[:, 0:256], ALU.mult, ALU.mult)
                P.stt(cqn[:, 256:384], pv[0][:, 256:384], rstd[:, 1:2], qnb[:, 256:384], ALU.mult, ALU.mult)
                P.transposes([(pq[:, c * 128:(c + 1) * 128], cqn[:, c * 128:(c + 1) * 128]) for c in range(3)], identb.all())
                pq_ = pq[:, 0:384]
                P.copy("dve", cqst[:, :, hts], pq_.with_ap(pq_.ap.rearrange("p (c t) -> p c t", c=3)))
                P.mm(pv[1][:, 0:256], [(hT[:, c, hts], wb[:, c, 384:640]) for c in range(8)])
                P.copy("dve", tkst[:, ti, 0:256], pv[1][:, 0:256])
                P.mm(pv[0].all(), [(hT[:, c, hts], wb[:, c, 640:1152]) for c in range(8)])
                P.copy("act", tkst[:, ti, 256:768], pv[0].all())
                P.mm(pv[1].all(), [(hT[:, c, hts], wb[:, c, 1152:1664]) for c in range(8)])
                P.act(tkst[:, ti, 768:1280], pv[1].all(), AF.Silu)
            if blk < 8:
                P.dma("sp", cst[:, 0, :], rope_m[0, :, tok0:tok0 + 512])
                P.dma("sp", cst[:, 1, :], rope_m[1, :, tok0:tok0 + 512])
            P.mm(pa[0][:, 0:ntok], [(wb[:, c, 1664:1792], hT[:, c, 0:ntok]) for c in range(8)])
            if blk < 8:
                P.mm(pb[:, 0:ntok], [(wb[:, c, 1792:1920], hT[:, c, 0:ntok]) for c in range(8)])
                P.tt("dve", t1[:, 0:ntok], pa[0][:, 0:ntok], cst[:, 0, 0:ntok], ALU.mult)
                P.tt("dve", t2[:, 0:ntok], pb[:, 0:ntok], cst[:, 1, 0:ntok], ALU.mult)
                P.tt("pool", fst[:, 0, 0:ntok], t1[:, 0:ntok], t2[:, 0:ntok], ALU.add)
            else:
                P.copy("act", fst[:, 0, 0:ntok], pa[0][:, 0:ntok])
            for g in range(8):
                col0 = 1920 + g * 128
                ps = pa[(g + 1) % 2]
                P.mm(ps[:, 0:ntok], [(wb[:, c, col0:col0 + 128], hT[:, c, 0:ntok]) for c in range(8)])
                P.copy("act" if g % 2 == 0 else "dve", fst[:, 1 + g, 0:ntok], ps[:, 0:ntok])
            P.dma("pool", krT_d[:, tok0:tok0 + ntok], fst[:, 0, 0:ntok])
            for gi, dst in enumerate((rqT_d, rkT_d)):
                dv = dst[:, :, tok0:tok0 + ntok]
                P.dma("pool", dv.with_ap(dv.ap.rearrange("h p t -> p h t")), fst[:, 1 + gi * 4:5 + gi * 4, 0:ntok])
            dv = cqn_d[:, :, tok0:tok0 + ntok]
            P.dma("pool", dv.with_ap(dv.ap.rearrange("c p t -> p c t")), cqst[:, :, 0:ntok])
            for dst, lo, hi in ((rk_d, 0, 256), (rv_d, 256, 768), (sg_d, 768, 1280)):
                dv = dst[tok0:tok0 + ntok, :]
                P.dma("pool", dv.with_ap(dv.ap.rearrange("(t p) c -> p t c", p=128)), tkst[:, 0:ntile, lo:hi])

        S1(0)
        for blk in range(9):
            if blk + 1 < 9:
                S1(blk + 1)
            S2(blk)
        P.pop()

    def phase_B1():
        P.push()
        cqnT = P.sbuf("cqnT", [128, 3, LT], BF16)
        for c in range(3):
            P.dma("sp" if c % 2 == 0 else "act", cqnT[:, c, :], cqn_d[c])
        wuqf = P.sbuf("wuqf", [128, 2, 1536], F32)
        wuq = P.sbuf("wuq", [128, 2, 1536], BF16)
        P.dma("sp", wuqf.all(), w_uq.all().with_ap(w_uq.base.rearrange("(c p) n -> p c n", p=128)))
        P.copy("dve", wuq.all(), wuqf.all())
        wukvf = P.sbuf("wukvf", [128, 1024], F32)
        wukv = P.sbuf("wukv", [128, 1024], BF16)
        P.dma("sp", wukvf.all(), w_ukv.all())
        P.copy("pool", wukv.all(), wukvf.all())
        Vm = P.sbuf("Vm", [128, NT, 8, 66], BF16)
        P.memset("dve", Vm[:, :, :, 64:66], 1.0)
        KT = P.sbuf("KTm", [96, LT], BF16)
        QT = [P.sbuf("QTm%d" % i, [96, LT], BF16) for i in range(2)]
        P.dma("sp", KT[64:96, :], krT_d[64:96, :])
        csm = P.sbuf("csm", [96, 2, L], F32)
        P.dma("sp", csm[64:96, 0, :], rope_m[0, 64:96, :])
        P.dma("act", csm[64:96, 1, :], rope_m[1, 64:96, :])
        ptm = [P.sbuf("ptm%d" % i, [128, 1024], BF16) for i in range(3)]
        ost = [P.sbuf("most%d" % i, [128, 4, 64], BF16) for i in range(2)]
        rr = P.sbuf("mrr", [128, 4], F32)
        t1 = P.sbuf("mt1", [96, 512], F32)
        t2 = P.sbuf("mt2", [96, 512], F32)
        psV = [P.psum("mpV%d" % i, [128, 512], F32) for i in range(2)]
        psS = [P.psum("mpS%d" % i, [128, 1024], F32) for i in range(2)]
        psO = [P.psum("mpO%d" % i, [128, 512], F32) for i in range(2)]
        class _Half:
            def __init__(self, tt_, lo):
                self.tt_, self.lo = tt_, lo

            def __getitem__(self, idx):
                r, c = idx
                c0 = 0 if c.start is None else c.start
                c1 = 512 if c.stop is None else c.stop
                return self.tt_[r, self.lo + c0:self.lo + c1]
        psQs = [_Half(psS[0], 0), _Half(psS[1], 0)]
        psQ2s = [_Half(psS[0], 512), _Half(psS[1], 512)]
        for t in range(NT):
            P.mm(psV[t % 2].all(), [(cqnT[:, 2, t * 128:(t + 1) * 128], wukv[:, 512:1024])])
            pv_ = psV[t % 2].all()
            P.copy("act" if t % 2 == 0 else "dve", Vm[:, t, :, 0:64], pv_.with_ap(pv_.ap.rearrange("p (h e) -> p h e", h=8)))
        sc = 96.0 ** -0.5
        for h in range(8):
            q_ = QT[h % 2]
            for blk in range(9):
                ntok = 512 if blk < 8 else 256
                tok0 = blk * 512
                ts_ = slice(tok0, tok0 + ntok)
                psQ, psQ2 = psQs[blk % 2], psQ2s[blk % 2]
                P.mm(psV[blk % 2][0:64, 0:ntok], [(wukv[:, h * 64:(h + 1) * 64], cqnT[:, 2, ts_])])
                P.copy("pool" if False else "dve", KT[0:64, ts_], psV[blk % 2][0:64, 0:ntok])
                P.mm(psQ[0:96, 0:ntok], [(wuq[:, c, h * 96:(h + 1) * 96], cqnT[:, c, ts_]) for c in range(2)])
                P.copy("act", q_[0:64, ts_], psQ[0:64, 0:ntok])
                if blk < 8:
                    P.mm(psQ2[0:96, 0:ntok], [(wuq[:, c, 768 + h * 96:768 + (h + 1) * 96], cqnT[:, c, ts_]) for c in range(2)])
                    P.tt("dve", t1[64:96, 0:ntok], psQ[64:96, 0:ntok], csm[64:96, 0, ts_], ALU.mult)
                    P.tt("dve", t2[64:96, 0:ntok], psQ2[64:96, 0:ntok], csm[64:96, 1, ts_], ALU.mult)
                    P.tt("pool", q_[64:96, ts_], t1[64:96, 0:ntok], t2[64:96, 0:ntok], ALU.add)
                else:
                    P.copy("act", q_[64:96, ts_], psQ[64:96, 0:ntok])
            for qb in range(8):
                q0 = qb * 512
                ob = psO[qb % 2]

                def qk_ex(pi):
                    s_ = psS[pi % 2]
                    for u in range(2):
                        kt = 2 * pi + u
                        P.mm(s_[:, u * 512:(u + 1) * 512], [(KT[0:96, kt * 128:(kt + 1) * 128], q_[0:96, q0:q0 + 512])])
                    P.act(ptm[pi % 3].all(), s_.all(), AF.Exp, scale=sc)

                def pv(pi):
                    p = ptm[pi % 3]
                    for u in range(2):
                        kt = 2 * pi + u
                        for qi in range(4):
                            P.mm(ob[:, qi * 65:(qi + 1) * 65], [(p[:, u * 512 + qi * 128:u * 512 + (qi + 1) * 128], Vm[:, kt, h, 0:65])],
                                 start=(kt == 0 and qi == 0), skip=True)

                qk_ex(0)
                for pi in range(NT // 2):
                    if pi + 1 < NT // 2:
                        qk_ex(pi + 1)
                    pv(pi)
                o = ost[qb % 2]
                for qi in range(4):
                    P.recip(rr[:, qi:qi + 1], ob[:, qi * 65 + 64:qi * 65 + 65])
                    P.ts("dve", o[:, qi, :], ob[:, qi * 65:qi * 65 + 64], rr[:, qi:qi + 1], None, ALU.mult)
                dv = ao_d[q0:q0 + 512, h * 64:(h + 1) * 64]
                P.dma("pool", dv.with_ap(dv.ap.rearrange("(t p) c -> p t c", p=128)), o.all())
        P.pop()

    def phase_C1():
        P.push()
        dl = P.sbuf("rdl", [128, 8], F32)
        lg = P.sbuf("rlg", [128, 8], F32)
        P.dma("sp", dl.all(), brow(ret_decay, 0, 8))
        P.act(lg.all(), dl.all(), AF.Exp, scale=-1.0)
        P.ts("dve", lg.all(), lg.all(), 1.0, None, ALU.add)
        P.act(lg.all(), lg.all(), AF.Ln)
        P.ts("dve", lg.all(), lg.all(), -1.0, None, ALU.mult)
        rmp = P.sbuf("rmp", [128, 4, 128], F32)
        P.dma("sp", rmp.all(), ramps_d.all())
        lgfb = P.sbuf("lgfb", [128, 1], F32)
        dq = P.sbuf("rdq", [128, 128], F32)
        dk = P.sbuf("rdk", [128, 2], F32)
        gC = P.sbuf("rgC", [128, 1], F32)
        DT = P.sbuf("rDT", [128, 128], F32)
        dtmp = P.sbuf("rdtmp", [128, 128], F32)
        qT = P.sbuf("rqT", [128, L], BF16)
        kT = P.sbuf("rkT", [64, L], BF16)
        ktok = P.sbuf("rktok", [128, NT, 64], BF16)
        vtok = P.sbuf("rvtok", [128, NT, 128], BF16)
        sg = P.sbuf("rsg", [128, 32, 128], BF16)
        U = P.sbuf("rU", [128, NT, 128], F32)
        S = P.sbuf("rS", [128, 32, 128], F32)
        Sb = P.sbuf("rSb", [128, 32, 128], BF16)
        Kd = [P.sbuf("rKd%d" % i, [128, 128], BF16) for i in range(2)]
        AT = [P.sbuf("rAT%d" % i, [128, 128], BF16) for i in range(2)]
        Qd = [P.sbuf("rQd%d" % i, [128, 128], BF16) for i in range(2)]
        r = [P.sbuf("rr%d" % i, [128, 128], F32) for i in range(2)]
        junk = P.sbuf("rjunk", [128, 128], F32)
        ss = P.sbuf("rss", [128, 1], F32)
        rs = P.sbuf("rrs", [128, 1], F32)
        ost = [P.sbuf("rost%d" % i, [128, 4, 128], BF16) for i in range(2)]
        psU = [P.psum("rpU%d" % i, [128, 512], F32) for i in range(2)]
        psS = [P.psum("rpS%d" % i, [128, 512], F32) for i in range(2)]
        psO = [P.psum("rpO%d" % i, [128, 512], F32) for i in range(2)]
        for h in range(4):
            P.copy("dve", lgfb[0:64, :], lg[0:64, h:h + 1])
            P.copy("dve", lgfb[64:128, :], lg[64:128, 4 + h:5 + h])
            P.act(dq.all(), rmp[:, 0, :], AF.Exp, scale=lgfb.all())
            P.act(dk[:, 0:1], rmp[:, 3, 0:1], AF.Exp, scale=lg[:, h:h + 1])
            P.act(dk[:, 1:2], rmp[:, 3, 1:2], AF.Exp, scale=lg[:, 4 + h:5 + h])
            P.act(gC.all(), rmp[:, 3, 2:3], AF.Exp, scale=lgfb.all())
            P.ts("dve", dtmp.all(), rmp[:, 1, :], lg[:, h:h + 1], None, ALU.mult)
            P.stt(dtmp.all(), rmp[:, 2, :], lg[:, 4 + h:5 + h], dtmp.all(), ALU.mult, ALU.add)
            P.act(DT.all(), dtmp.all(), AF.Exp)
            P.dma("sp", qT.all(), rqT_d[h, :, 0:L])
            P.dma("act", kT.all(), rkT_d[h, 0:64, 0:L])
            sv = rk_d[:, h * 64:(h + 1) * 64]
            P.dma("sp", ktok.all(), sv.with_ap(sv.ap.rearrange("(t p) c -> p t c", p=128)))
            sv = rv_d[:, h * 128:(h + 1) * 128]
            P.dma("act", vtok.all(), sv.with_ap(sv.ap.rearrange("(t p) c -> p t c", p=128)))
            sv = sg_d[0:L, h * 128:(h + 1) * 128]
            P.dma("sp", sg.all(), sv.with_ap(sv.ap.rearrange("(t p) c -> p t c", p=128)))
            for t in range(NT):
                kd = Kd[t % 2]
                P.ts("dve", kd[:, 0:64], ktok[:, t, :], dk[:, 0:1], None, ALU.mult)
                P.ts("pool", kd[:, 64:128], ktok[:, t, :], dk[:, 1:2], None, ALU.mult)
                P.mm(psU[t % 2][:, 0:128], [(kd.all(), vtok[:, t, :])])
                P.copy("act", U[:, t, :], psU[t % 2][:, 0:128])
            P.stt(S[0:64, 0, :], U[0:64, 32, :], gC[0:64, :], U[0:64, 33, :], ALU.mult, ALU.add)
            for n in range(31):
                P.stt(S[0:64, n + 1, :], S[0:64, n, :], gC[0:64, :], U[0:64, n, :], ALU.mult, ALU.add)
            P.stt(S[64:128, 31, :], U[64:128, 33, :], gC[64:128, :], U[64:128, 32, :], ALU.mult, ALU.add)
            for n in range(31, 0, -1):
                P.stt(S[64:128, n - 1, :], S[64:128, n, :], gC[64:128, :], U[64:128, n, :], ALU.mult, ALU.add)
            P.copy("pool", Sb.all(), S.all())
            def pre(n):
                ns = slice(n * 128, (n + 1) * 128)
                P.mm(psS[n % 2][:, 0:128], [(kT[0:64, ns], qT[0:64, ns])])
                P.tt("dve", AT[n % 2].all(), psS[n % 2][:, 0:128], DT.all(), ALU.mult)
                P.tt("pool", Qd[n % 2].all(), qT[:, ns], dq.all(), ALU.mult)
            pre(0)
            for n in range(32):
                ns = slice(n * 128, (n + 1) * 128)
                if n + 1 < 32:
                    pre(n + 1)
                P.mm(psO[n % 2][:, 0:128], [(AT[n % 2].all(), vtok[:, n, :]), (Qd[n % 2].all(), Sb[:, n, :])])
                rr_ = r[n % 2]
                P.act(rr_.all(), psO[n % 2][:, 0:128], AF.Copy, scale=0.125)
                P.act(junk.all(), rr_.all(), AF.Square, accum=ss.all())
                rstd_of(ss.all(), rs.all(), 128)
                o = ost[(n // 4) % 2]
                P.stt(o[:, n % 4, :], rr_.all(), rs.all(), sg[:, n, :], ALU.mult, ALU.mult)
                if n % 4 == 3:
                    q0 = (n - 3) * 128
                    dv = ao_d[q0:q0 + 512, 512 + h * 128:512 + (h + 1) * 128]
                    P.dma("pool", dv.with_ap(dv.ap.rearrange("(t p) c -> p t c", p=128)), o.all())
        P.pop()

    seq = [
        ("A0", lambda: phase_A0()),
        ("B0", lambda: phase_B0()),
        ("C0", lambda: phase_C0()),
        ("D0", None), ("T0", None), ("E0", None),
        ("A1", lambda: phase_A1()),
        ("B1", lambda: phase_B1()),
        ("C1", lambda: phase_C1()),
        ("D1", None), ("T1", None), ("E1", None),
        ("F", lambda: phase_final()),
    ]
    affT = None
    for name, fn in seq:
        if only is not None and name not in only:
            continue
        if name in ("D0", "D1"):
            l = 0 if name == "D0" else 1
            P.push()
            affT = P.sbuf("affT", [16, LT], F32)
            phase_D(l, NT if l == 0 else 32, affT)
        elif name in ("T0", "T1"):
            l = 0 if name == "T0" else 1
            phase_topk(l, affT)
            P.pop()
        elif name in ("E0", "E1"):
            l = 0 if name == "E0" else 1
            phase_experts(l, 5 if l == 0 else 4)
        else:
            fn()
        if stop(name):
            break
    if stop_after in ("D0", "D1"):
        P.pop()
    P.finish()
    P.stats = dict(P.ninst)
    return nc


_CACHE = {}


def kernel(**inputs):
    sh, per = host_prep(inputs)
    if "nc" not in _CACHE:
        _CACHE["nc"] = build()
    nc = _CACHE["nc"]
    n = len(per)
    in_maps = []
    for b in range(n):
        m = dict(sh)
        m.update(per[b])
        in_maps.append(m)
    res = run_bass_kernel_spmd(nc, in_maps, core_ids=list(range(n)))
    return np.stack([np.asarray(r["out"], dtype=np.float32) for r in res.results], axis=0)
```
